# Optimizing a Trainium2 kernel written in Bass

```python
import math
import jax, jax.numpy as jnp
from jax import lax
import numpy as np

D_MODEL = 1024
BATCH = 4
SEQ = 8192
DEPTH = 2

N_A_LAYERS = DEPTH // 2
N_B_LAYERS = DEPTH - N_A_LAYERS
HEAD_DIM = 64
A_HEADS = D_MODEL // HEAD_DIM
A_WIDTH = A_HEADS * HEAD_DIM
B_WINDOWS = (128, 512, 2048)
B_DILATIONS = (1, 4, 16)
B_GROUPS = len(B_DILATIONS)
B_HEADS_PER_GROUP = D_MODEL // (2 * HEAD_DIM)
B_HEADS = B_GROUPS * B_HEADS_PER_GROUP
B_WIDTH = B_HEADS * HEAD_DIM
B_OUT_WIDTH = B_HEADS_PER_GROUP * HEAD_DIM
BLOCK = 128
NUM_BUCKETS = 32
MAX_DISTANCE = 2048
N_GROUPS = 4
EXPERTS_PER_GROUP = 4
N_EXPERTS = N_GROUPS * EXPERTS_PER_GROUP
TOP_K = 2
D_EXPERT = D_MODEL // 2
EPS = 1e-6
NEG = -1e30
SCALE = HEAD_DIM ** -0.5

kernel_name = "fox_dilated_yoco_hmoe"


def rms_norm(x, g):
    xf = x.astype(jnp.float32)
    y = xf * lax.rsqrt(jnp.mean(xf * xf, axis=-1, keepdims=True) + EPS)
    return (y * g.astype(jnp.float32)).astype(x.dtype)


def fox_mixer(xn, w_in, b_f, q_gain, k_gain, w_out):
    B, S, _ = xn.shape
    nb = S // BLOCK
    proj = jnp.einsum('bsd,de->bse', xn, w_in)
    q = rms_norm(proj[..., :A_WIDTH].reshape(B, S, A_HEADS, HEAD_DIM), q_gain)
    k = rms_norm(proj[..., A_WIDTH:2 * A_WIDTH].reshape(B, S, A_HEADS, HEAD_DIM), k_gain)
    v = proj[..., 2 * A_WIDTH:3 * A_WIDTH].reshape(B, S, A_HEADS, HEAD_DIM)
    log_f = jax.nn.log_sigmoid((proj[..., 3 * A_WIDTH:] + b_f).astype(jnp.float32))
    cum = jnp.transpose(jnp.cumsum(log_f, axis=1), (0, 2, 1))
    qb = jnp.moveaxis(q.reshape(B, nb, BLOCK, A_HEADS, HEAD_DIM), 1, 0)
    cum_q = jnp.moveaxis(cum.reshape(B, A_HEADS, nb, BLOCK), 2, 0)
    key_pos = jnp.arange(S)

    def attend_block(args):
        blk, q_blk, cq = args
        s = jnp.einsum('bqhd,bkhd->bhqk', q_blk, k, preferred_element_type=jnp.float32) * SCALE
        s = s + cq[..., :, None] - cum[:, :, None, :]
        q_pos = blk * BLOCK + jnp.arange(BLOCK)
        s = jnp.where(key_pos[None, :] <= q_pos[:, None], s, NEG)
        p = jax.nn.softmax(s, axis=-1)
        return jnp.einsum('bhqk,bkhd->bqhd', p.astype(v.dtype), v)

    out = lax.map(attend_block, (jnp.arange(nb), qb, cum_q))
    out = jnp.moveaxis(out, 0, 1).reshape(B, S, A_WIDTH)
    return jnp.einsum('bse,ed->bsd', out, w_out)


def hier_moe(xn, w_group, b_group, w_expert, b_expert, w_gate, w_up, w_down):
    B, S, D = xn.shape
    N = B * S
    xt = xn.reshape(N, D)
    g_prob = jax.nn.softmax((xt @ w_group + b_group).astype(jnp.float32), axis=-1)
    g_val, g_idx = lax.top_k(g_prob, 1)
    e_logits = (xt @ w_expert + b_expert).astype(jnp.float32).reshape(N, N_GROUPS, EXPERTS_PER_GROUP)
    sel = jnp.broadcast_to(g_idx[:, :, None], (N, 1, EXPERTS_PER_GROUP))
    e_logits = jnp.take_along_axis(e_logits, sel, axis=1)[:, 0]
    e_val, e_idx = lax.top_k(jax.nn.softmax(e_logits, axis=-1), TOP_K)
    e_val = e_val / jnp.sum(e_val, axis=-1, keepdims=True)
    expert_id = g_idx * EXPERTS_PER_GROUP + e_idx
    gates = jnp.sum(jax.nn.one_hot(expert_id, N_EXPERTS, dtype=jnp.float32)
                    * (g_val * e_val)[..., None], axis=1)
    out = jnp.zeros((N, D), jnp.float32)
    for e in range(N_EXPERTS):
        hid = jax.nn.silu(xt @ w_gate[e]) * (xt @ w_up[e])
        out = out + gates[:, e:e + 1] * (hid @ w_down[e]).astype(jnp.float32)
    return out.astype(xn.dtype).reshape(B, S, D)


def to_strided_blocks(x, d):
    B, S = x.shape[:2]
    rest = x.shape[2:]
    span = d * BLOCK
    s_pad = -(-S // span) * span
    x = jnp.pad(x, [(0, 0), (0, s_pad - S)] + [(0, 0)] * len(rest))
    x = jnp.moveaxis(x.reshape(B, s_pad // d, d, *rest), 2, 1)
    return x.reshape(B, d, s_pad // span, BLOCK, *rest)


def from_strided_blocks(xb, S):
    B, d, nb = xb.shape[:3]
    rest = xb.shape[4:]
    x = jnp.moveaxis(xb.reshape(B, d, nb * BLOCK, *rest), 1, 2)
    return x.reshape(B, nb * BLOCK * d, *rest)[:, :S]


def with_prev_block(xb):
    prev = jnp.concatenate([jnp.zeros_like(xb[:, :, :1]), xb[:, :, :-1]], axis=2)
    return jnp.concatenate([prev, xb], axis=3)


def t5_bucket(dist):
    max_exact = NUM_BUCKETS // 2
    d_f = jnp.maximum(dist, max_exact).astype(jnp.float32)
    large = max_exact + (jnp.log(d_f / max_exact) / math.log(MAX_DISTANCE / max_exact)
                         * (NUM_BUCKETS - max_exact)).astype(jnp.int32)
    large = jnp.minimum(large, NUM_BUCKETS - 1)
    return jnp.where(dist < max_exact, dist, large)


def branch_bias(rel_bias, g, d):
    a = jnp.arange(BLOCK)[:, None]
    b = jnp.arange(2 * BLOCK)[None, :]
    n = BLOCK + a - b
    band = (n >= 0) & (n <= B_WINDOWS[g] // d)
    bucket = t5_bucket(jnp.maximum(n, 0) * d)
    bias = rel_bias[bucket][:, :, g * B_HEADS_PER_GROUP:(g + 1) * B_HEADS_PER_GROUP]
    return jnp.moveaxis(bias, 2, 0).astype(jnp.float32), band


def shared_kv(h, kv_norm, kv_w, k_gain):
    B, S, _ = h.shape
    kv = jnp.einsum('bsd,de->bse', rms_norm(h, kv_norm), kv_w)
    k = rms_norm(kv[..., :B_WIDTH].reshape(B, S, B_HEADS, HEAD_DIM), k_gain)
    v = kv[..., B_WIDTH:].reshape(B, S, B_HEADS, HEAD_DIM)
    branches = []
    for g, d in enumerate(B_DILATIONS):
        sl = slice(g * B_HEADS_PER_GROUP, (g + 1) * B_HEADS_PER_GROUP)
        branches.append((with_prev_block(to_strided_blocks(k[:, :, sl], d)),
                         with_prev_block(to_strided_blocks(v[:, :, sl], d))))
    return branches


def dilated_mixer(xn, w_q, q_gain, w_out, branches, biases):
    B, S, _ = xn.shape
    q = rms_norm(jnp.einsum('bsd,de->bse', xn, w_q).reshape(B, S, B_HEADS, HEAD_DIM), q_gain)
    outs, lses = [], []
    for g, d in enumerate(B_DILATIONS):
        kb, vb = branches[g]
        bias, band = biases[g]
        qb = to_strided_blocks(q[:, :, g * B_HEADS_PER_GROUP:(g + 1) * B_HEADS_PER_GROUP], d)
        nb = qb.shape[2]
        valid = band[None] & ((jnp.arange(nb)[:, None, None] > 0)
                              | (jnp.arange(2 * BLOCK) >= BLOCK)[None, None, :])
        s = jnp.einsum('brnqhd,brnkhd->brnhqk', qb, kb, preferred_element_type=jnp.float32) * SCALE
        s = jnp.where(valid[None, None, :, None], s + bias[None, None, None], NEG)
        m = jnp.max(s, axis=-1, keepdims=True)
        ex = jnp.exp(s - m)
        den = jnp.sum(ex, axis=-1, keepdims=True)
        o = jnp.einsum('brnhqk,brnkhd->brnqhd', (ex / den).astype(vb.dtype), vb)
        lse = jnp.swapaxes((m + jnp.log(den))[..., 0], 3, 4)
        outs.append(from_strided_blocks(o, S))
        lses.append(from_strided_blocks(lse, S))
    o = jnp.stack(outs, axis=0).astype(jnp.float32)
    alpha = jax.nn.softmax(jnp.stack(lses, axis=0), axis=0)
    merged = jnp.sum(alpha[..., None] * o, axis=0).astype(xn.dtype).reshape(B, S, B_OUT_WIDTH)
    return jnp.einsum('bse,ed->bsd', merged, w_out)


def setup_inputs(seed: int = 0) -> dict:
    key = jax.random.key(seed)
    ks = jax.random.split(key, 24)
    f32 = jnp.float32

    def nrm(k, shape, scale):
        return scale * jax.random.normal(k, shape, f32)

    def gain(k, shape):
        return 1.0 + 0.05 * jax.random.normal(k, shape, f32)

    res = (2 * DEPTH) ** -0.5
    return {
        "x": jax.random.normal(ks[0], (BATCH, SEQ, D_MODEL), f32),
        "a_norm": gain(ks[1], (N_A_LAYERS, D_MODEL)),
        "a_w_in": nrm(ks[2], (N_A_LAYERS, D_MODEL, 3 * A_WIDTH + A_HEADS), D_MODEL ** -0.5),
        "a_b_f": 2.0 + 0.5 * jax.random.normal(ks[3], (N_A_LAYERS, A_HEADS), f32),
        "a_q_gain": gain(ks[4], (N_A_LAYERS, HEAD_DIM)),
        "a_k_gain": gain(ks[5], (N_A_LAYERS, HEAD_DIM)),
        "a_w_out": nrm(ks[6], (N_A_LAYERS, A_WIDTH, D_MODEL), A_WIDTH ** -0.5 * res),
        "kv_norm": gain(ks[7], (D_MODEL,)),
        "kv_w": nrm(ks[8], (D_MODEL, 2 * B_WIDTH), D_MODEL ** -0.5),
        "kv_k_gain": gain(ks[9], (HEAD_DIM,)),
        "rel_bias": nrm(ks[10], (NUM_BUCKETS, B_HEADS), 0.5),
        "b_norm": gain(ks[11], (N_B_LAYERS, D_MODEL)),
        "b_w_q": nrm(ks[12], (N_B_LAYERS, D_MODEL, B_WIDTH), D_MODEL ** -0.5),
        "b_q_gain": gain(ks[13], (N_B_LAYERS, HEAD_DIM)),
        "b_w_out": nrm(ks[14], (N_B_LAYERS, B_OUT_WIDTH, D_MODEL), B_OUT_WIDTH ** -0.5 * res),
        "ffn_norm": gain(ks[15], (DEPTH, D_MODEL)),
        "moe_w_group": nrm(ks[16], (DEPTH, D_MODEL, N_GROUPS), D_MODEL ** -0.5),
        "moe_b_group": nrm(ks[17], (DEPTH, N_GROUPS), 0.01),
        "moe_w_expert": nrm(ks[18], (DEPTH, D_MODEL, N_EXPERTS), D_MODEL ** -0.5),
        "moe_b_expert": nrm(ks[19], (DEPTH, N_EXPERTS), 0.01),
        "moe_w_gate": nrm(ks[20], (DEPTH, N_EXPERTS, D_MODEL, D_EXPERT), D_MODEL ** -0.5),
        "moe_w_up": nrm(ks[21], (DEPTH, N_EXPERTS, D_MODEL, D_EXPERT), D_MODEL ** -0.5),
        "moe_w_down": nrm(ks[22], (DEPTH, N_EXPERTS, D_EXPERT, D_MODEL), D_EXPERT ** -0.5 * res),
    }


def reference(x, a_norm, a_w_in, a_b_f, a_q_gain, a_k_gain, a_w_out, kv_norm, kv_w, kv_k_gain,
              rel_bias, b_norm, b_w_q, b_q_gain, b_w_out, ffn_norm, moe_w_group, moe_b_group,
              moe_w_expert, moe_b_expert, moe_w_gate, moe_w_up, moe_w_down):
    h = x
    branches = None
    biases = [branch_bias(rel_bias, g, d) for g, d in enumerate(B_DILATIONS)]
    for layer in range(DEPTH):
        if layer < N_A_LAYERS:
            i = layer
            h = h + fox_mixer(rms_norm(h, a_norm[i]), a_w_in[i], a_b_f[i], a_q_gain[i],
                              a_k_gain[i], a_w_out[i])
        else:
            i = layer - N_A_LAYERS
            h = h + dilated_mixer(rms_norm(h, b_norm[i]), b_w_q[i], b_q_gain[i], b_w_out[i],
                                  branches, biases)
        h = h + hier_moe(rms_norm(h, ffn_norm[layer]), moe_w_group[layer], moe_b_group[layer],
                         moe_w_expert[layer], moe_b_expert[layer], moe_w_gate[layer],
                         moe_w_up[layer], moe_w_down[layer])
        if layer == N_A_LAYERS - 1:
            branches = shared_kv(h, kv_norm, kv_w, kv_k_gain)
    return h
```

```python
import math
import os
from contextlib import ExitStack

import numpy as np
import ml_dtypes

import concourse.bass as bass
import concourse.mybir as mybir
from concourse.bass_utils import run_bass_kernel_spmd

F32 = mybir.dt.float32
BF16 = mybir.dt.bfloat16
AF = mybir.ActivationFunctionType
ALU = mybir.AluOpType
AX = mybir.AxisListType

D = 1024
SPAN = 2048
NH_A = 16
HD = 64
NE = 16
DE = 512
EPS = 1e-6
SCALE = HD ** -0.5
NEGV = -30000.0
B_DIL = (1, 4, 16)


class Cfg:
    def __init__(self, KS=4, QS=(0, 1, 2, 3), BS=((0, None), (1, 0), (2, 1), (3, 2)),
                 stages=("p1", "p2", "m0", "p5", "p5b", "p6", "m1"), expose=(), inject=(), n_cores=4, masks=False):
        self.KS = KS
        self.QS = tuple(QS)
        self.BS = tuple(BS)
        self.SK = KS * SPAN
        self.NQ = len(QS) * SPAN
        self.NB = len(BS) * SPAN
        self.stages = tuple(stages)
        self.expose = set(expose)
        self.inject = set(inject)
        self.n_cores = n_cores
        self.masks = masks


class Sem:
    def __init__(self, nc, es, name):
        self.h = es.enter_context(nc.semaphore(name))
        self.n = 0
        self.name = name
        self.kind = None

    def mark(self, kind):
        assert self.kind in (None, kind), (self.name, self.kind, kind)
        self.kind = kind


class FW:
    def __init__(self, nc, es):
        self.nc = nc
        self.es = es
        self.eng = dict(pe=nc.tensor, act=nc.scalar, dve=nc.vector, pool=nc.gpsimd, sp=nc.sync)
        self.seen = {k: {} for k in self.eng}
        self.nsem = 0
        self.rings = {}
        self.ring_i = {}

    def sem(self, name):
        self.nsem += 1
        return Sem(self.nc, self.es, f"{name}_{self.nsem}")

    def _waits(self, e, waits):
        for w in waits:
            if w is None:
                continue
            if isinstance(w, list):
                self._waits(e, w)
                continue
            s, v = w
            if v is None or v <= 0:
                continue
            if self.seen[e].get(s.name, 0) >= v:
                continue
            self.seen[e][s.name] = v
            self.eng[e].wait_ge(s.h, v)

    def op(self, e, fn, waits=(), inc=None):
        self._waits(e, waits)
        ins = fn(self.eng[e])
        if inc is not None:
            inc.mark('eng')
            inc.n += 1
            ins.then_inc(inc.h, 1)
            return (inc, inc.n)
        return None

    def _ring_next(self, e):
        if e not in self.rings:
            self.rings[e] = [self.sem(f"dq{e}{i}") for i in range(8)]
            self.ring_i[e] = 0
        s = self.rings[e][self.ring_i[e] % len(self.rings[e])]
        self.ring_i[e] += 1
        self._waits(e, [(s, s.n)])
        return s

    def group(self, e):
        return DmaGroup(self, e)

    def dma(self, e, out, in_, waits=(), inc=None):
        if isinstance(inc, DmaGroup):
            assert inc.e == e
            s = inc.sem
        else:
            s = self._ring_next(e)
        self._waits(e, waits)
        ins = self.eng[e].dma_start(out=out, in_=in_)
        s.n += 16
        ins.then_inc(s.h, 16)
        return (s, s.n)

    def dma_all(self):
        return [(s, s.n) for r in self.rings.values() for s in r]


class DmaGroup:
    def __init__(self, fw, e):
        self.e = e
        self.sem = fw._ring_next(e)

    def token(self):
        return (self.sem, self.sem.n)


def _t5_bucket_np(dist):
    dist = np.asarray(dist, np.int64)
    max_exact = 16
    d_f = np.maximum(dist, max_exact).astype(np.float32)
    large = max_exact + (np.log(d_f / np.float32(max_exact)) / np.float32(math.log(2048 / max_exact))
                         * np.float32(32 - max_exact)).astype(np.int32)
    large = np.minimum(large, 31)
    return np.where(dist < max_exact, dist, large)


def make_consts():
    c = {}
    c["c_ident"] = np.eye(128, dtype=np.float32)
    k = np.arange(128)[:, None]
    q = np.arange(128)[None, :]
    c["c_blk"] = ((k // 64) == (q // 64)).astype(np.float32)
    c["c_triu"] = np.where(k <= q, -1.0, 0.0).astype(np.float32)
    qq = np.arange(512)[None, None, :]
    jj = np.arange(4)[None, :, None]
    kk = np.arange(128)[:, None, None]
    c["c_negm"] = np.where(qq < 128 * jj + kk, NEGV, 0.0).astype(np.float32).reshape(128, 2048)
    c["c_anti"] = np.eye(128, dtype=np.float32)[::-1].copy()
    oh = np.zeros((3, 32, 384), np.float32)
    ng = np.zeros((3, 1, 384), np.float32)
    for g, d in enumerate(B_DIL):
        for m in range(384):
            n = m - 127
            if 0 <= n <= 128:
                oh[g, int(_t5_bucket_np(n * d)), m] = 1.0
            else:
                ng[g, 0, m] = NEGV
    c["c_oh"] = oh
    c["c_stri"] = (k < q).astype(np.float32)
    ee = np.arange(16)
    c["c_lt"] = (ee[None, :] < ee[:, None]).astype(np.float32).reshape(256)
    c["c_ng"] = ng
    return c


def build(cfg):
    nc = bass.Bass("TRN2", target_bir_lowering=False)
    es = ExitStack()
    fw = FW(nc, es)
    SK, NQ, NB = cfg.SK, cfg.NQ, cfg.NB

    def ext_in(name, shape, dtype=F32):
        return nc.dram_tensor(name, list(shape), dtype, kind="ExternalInput").ap()

    def scratch(name, shape, dtype):
        if name in cfg.inject:
            kind = "ExternalInput"
        elif name in cfg.expose:
            kind = "ExternalOutput"
        else:
            kind = "Internal"
        return nc.dram_tensor(name, list(shape), dtype, kind=kind).ap()

    SHAPES = dict([("x", [SK, D]), ("a_norm", [D]), ("a_w_in", [D, 3088]), ("a_b_f", [16]), ("a_q_gain", [64]), ("a_k_gain", [64]),
                   ("a_w_out", [D, D]), ("kv_norm", [D]), ("kv_w", [D, 3072]), ("kv_k_gain", [64]),
                   ("rel_bias", [32, 24]), ("b_norm", [D]), ("b_w_q", [D, 1536]), ("b_q_gain", [64]),
                   ("b_w_out", [512, D]), ("ffn_norm", [2, D]), ("moe_w_group", [2, D, 4]), ("moe_b_group", [2, 4]),
                   ("moe_w_expert", [2, D, 16]), ("moe_b_expert", [2, 16]), ("moe_w_gate", [2, NE, D, DE]),
                   ("moe_w_up", [2, NE, D, DE]), ("moe_w_down", [2, NE, DE, D]),
                   ("c_ident", [128, 128]), ("c_blk", [128, 128]), ("c_triu", [128, 128]), ("c_negm", [128, 2048]),
                   ("c_anti", [128, 128]), ("c_oh", [3, 32, 384]), ("c_ng", [3, 1, 384]), ("c_stri", [128, 128]), ("c_lt", [256]), ("c_big", [SK]), ("c_kbias", [len(cfg.QS)])])

    class LazyIn(dict):
        def __missing__(self, k):
            v = ext_in(k, SHAPES[k])
            self[k] = v
            return v
    I = LazyIn()
    nc._used_inputs = I

    QT = scratch("QT", [NH_A, 70, NQ], BF16)
    KT = scratch("KT", [NH_A, 70, SK], BF16)
    VP = scratch("VP", [NH_A, 128, SK // 128, 65], BF16)
    AT0 = scratch("AT0", [D, NQ], BF16)
    H1 = scratch("H1", [NQ, D], F32)
    KB = scratch("KB", [1536, NQ], BF16)
    VB = scratch("VB", [3, NQ, 8 * 65], BF16)
    QB = scratch("QB", [1536, NB], BF16)
    FV = scratch("FV", [24, 384], F32)
    AT1 = scratch("AT1", [512, NB], BF16)
    Y = nc.dram_tensor("y", [NB, D], F32, kind="ExternalOutput").ap()

    s_fin = fw.sem("fin")
    fin_waits = []

    cst = es.enter_context(nc.sbuf_tensor("cst_ident_bf", [128, 128], BF16))
    ident_bf = cst
    ident_f = es.enter_context(nc.sbuf_tensor("cst_ident_f", [128, 128], F32))
    blk_bf = es.enter_context(nc.sbuf_tensor("cst_blk", [128, 128], BF16))
    triu_f = es.enter_context(nc.sbuf_tensor("cst_triu", [128, 128], F32))
    negm_bf = es.enter_context(nc.sbuf_tensor("cst_negm", [128, 2048], BF16))
    ones_f = es.enter_context(nc.sbuf_tensor("cst_ones", [128, 128], F32))
    s_c = fw.group('sp')
    fw.dma('sp', ident_f[:], I["c_ident"][:, :], inc=s_c)
    fw.dma('sp', triu_f[:], I["c_triu"][:, :], inc=s_c)
    s_cp = fw.group('pool')
    fw.dma('pool', ident_bf[:], I["c_ident"][:, :], inc=s_cp)
    fw.dma('pool', blk_bf[:], I["c_blk"][:, :], inc=s_cp)
    fw.dma('pool', negm_bf[:], I["c_negm"][:, :], inc=s_cp)
    s_c1 = fw.sem("cst1")
    w_ones = fw.op('pool', lambda e: e.memset(ones_f[:], 1.0), inc=s_c1)
    W_CST = [s_c.token(), s_cp.token(), w_ones]

    def rms_stats(xs_ap, junk_ap, st_tile, col, n, wait_in, s_act):
        t1 = fw.op('act', lambda e: e.activation(out=junk_ap, in_=xs_ap, func=AF.Square,
                                                  accum_out=st_tile[:, col:col + 1]), waits=wait_in, inc=s_act)
        t2 = fw.op('act', lambda e: e.activation(out=st_tile[:, col + 1:col + 2], in_=st_tile[:, col:col + 1],
                                                  func=AF.Ln, scale=1.0 / n, bias=EPS), waits=[t1], inc=s_act)
        t3 = fw.op('act', lambda e: e.activation(out=st_tile[:, col + 2:col + 3], in_=st_tile[:, col + 1:col + 2],
                                                  func=AF.Exp, scale=-0.5), waits=[t2], inc=s_act)
        return t3

    def phase1():
        NT = SK // 128
        NBLK = SK // 512
        def qcol_of_block(tb):
            span = (tb * 512) // SPAN
            if span in cfg.QS:
                return cfg.QS.index(span) * SPAN + (tb * 512) % SPAN
            return None
        with ExitStack() as ps:
            sb = lambda name, shape, dt: ps.enter_context(nc.sbuf_tensor("p1_" + name, shape, dt))
            pm = lambda name, shape, dt: ps.enter_context(nc.psum_tensor("p1P_" + name, shape, dt))
            w_in = sb("w_in", [128, 8, 3088], BF16)
            g_bc = sb("g_bc", [128, D], F32)
            bf_bc = sb("bf_bc", [128, 16], F32)
            gq = sb("gq", [128, 2], F32)
            xs = [sb(f"xs{i}", [128, D], F32) for i in range(2)]
            junk = sb("junk", [128, D], BF16)
            xn = [sb(f"xn{i}", [128, D], BF16) for i in range(2)]
            xnT = [sb(f"xnT{i}", [128, 8, 512], BF16) for i in range(2)]
            st = sb("st", [128, 3 * NT], F32)
            sqk = [sb(f"sqk{i}", [128, 512], BF16) for i in range(2)]
            lnv = [sb(f"lnv{i}", [128, 512], F32) for i in range(2)]
            rstd = [sb(f"rstd{i}", [128, 512], F32) for i in range(2)]
            kst = [sb(f"kst{i}", [128, 512], BF16) for i in range(3)]
            vst = [sb(f"vst{i}", [128, 16, 4, 65], BF16) for i in range(2)]
            fl = [sb(f"fl{i}", [128, 16], F32) for i in range(2)]
            spv = [sb(f"spv{i}", [128, 16], F32) for i in range(2)]
            cum = sb("cum", [16, SK], F32)
            psA = [pm(f"psA{i}", [128, 512], F32) for i in range(3)]
            ps2 = [pm(f"ps2{i}", [128, 512], F32) for i in range(2)]
            pt = [pm("pt0", [128, 8, 128], BF16)] * 2
            psf = pm("psf", [128, 512], F32)
            pscum = pm("pscum", [128, 512], F32)

            s_w = fw.group('sp'); s_x = None; s_a = fw.sem("p1a"); s_v = fw.sem("p1v")
            s_p = fw.sem("p1p"); s_g = fw.sem("p1g"); s_st = None

            s_wp = fw.group('pool')
            for c in range(8):
                fw.dma('pool', w_in[:, c, :], I["a_w_in"][c * 128:(c + 1) * 128, :], inc=s_wp)
            fw.dma('sp', g_bc[:], I["a_norm"].partition_broadcast(128), inc=s_w)
            fw.dma('sp', bf_bc[:], I["a_b_f"].partition_broadcast(128), inc=s_w)
            for hh in range(2):
                fw.dma('sp', gq[hh * 64:(hh + 1) * 64, 0:1], I["a_q_gain"].rearrange("(d o) -> d o", o=1), inc=s_w)
                fw.dma('sp', gq[hh * 64:(hh + 1) * 64, 1:2], I["a_k_gain"].rearrange("(d o) -> d o", o=1), inc=s_w)
            W_W = [s_w.token(), s_wp.token()]
            t = fw.op('pool', lambda e: e.memset(st[:], 0.0), inc=s_g)
            for i in range(2):
                t = fw.op('pool', lambda e: e.memset(vst[i][:], 1.0), inc=s_g)
            W_INIT = t
            t_gq = fw.op('dve', lambda e: e.tensor_scalar(out=gq[:, 0:1], in0=gq[:, 0:1], scalar1=SCALE, scalar2=None,
                                                            op0=ALU.mult), waits=[W_W], inc=s_v)

            xs_free = [[], []]
            xn_free = [[], []]
            pt_free_l = [[]]
            xnT_free = [[], []]
            psA_free = [[], [], []]
            ps2_free = [[], []]
            sqk_free = [[], []]
            lnv_free = [[], []]
            rstd_free = [[], []]
            kst_free = [[], [], []]
            vst_free = [[], []]
            fl_free = [[], []]
            spv_free = [[], []]
            psf_free = []
            pscum_free = []
            cnt = dict(a=0, k=0, j=0)
            last_cum = [None]

            def emit_tile_front(tb, ti):
                tg = tb * 4 + ti
                b = tg % 2
                bb = tb % 2
                tx = fw.dma('sp', xs[b][:], I["x"][tg * 128:(tg + 1) * 128, :], waits=xs_free[b], inc=s_x)
                t3 = rms_stats(xs[b][:], junk[:], st, 3 * tg, D, [tx, W_INIT], s_a)
                tn = fw.op('dve', lambda e: e.scalar_tensor_tensor(out=xn[b][:], in0=xs[b][:], scalar=st[:, 3 * tg + 2:3 * tg + 3],
                                                                    in1=g_bc[:], op0=ALU.mult, op1=ALU.mult),
                           waits=[t3, W_W] + xn_free[b], inc=s_v)
                xs_free[b] = [tn]
                tt = None
                for c in range(8):
                    tt = fw.op('pe', lambda e: e.transpose(pt[b][:, c, :], xn[b][:, c * 128:(c + 1) * 128], ident_bf[:]),
                               waits=[tn] + W_CST + pt_free_l[0], inc=(s_p if c == 7 else None))
                xn_free[b] = [tt]
                te = fw.op('dve', lambda e: e.tensor_copy(xnT[bb][:, :, ti * 128:(ti + 1) * 128], pt[b][:]),
                           waits=[tt] + xnT_free[bb], inc=s_v)
                pt_free_l[0] = [te]
                return te

            def emit_block(tb):
                bb = tb % 2
                tes = [emit_tile_front(tb, ti) for ti in range(4)]
                t_xnT = tes[-1]
                qcol = qcol_of_block(tb)
                jobs = []
                for kc in range(8):
                    jobs.append(("k", kc))
                if qcol is not None:
                    for qc in range(8):
                        jobs.append(("q", qc))
                for ti in range(4):
                    for half in range(2):
                        jobs.append(("v", ti, half))
                for ti in range(4):
                    jobs.append(("f", ti))
                vb = tb % 2
                pend = []
                last_pe = [None]

                def stage1(job):
                    kind = job[0]
                    if kind in ("k", "q"):
                        ch = job[1]
                        a = cnt['a'] % 3; cnt['a'] += 1
                        col0 = (1024 if kind == "k" else 0) + ch * 128
                        tm = None
                        for c in range(8):
                            tm = fw.op('pe', lambda e: e.matmul(psA[a][:], w_in[:, c, col0:col0 + 128], xnT[bb][:, c, :],
                                                                start=(c == 0), stop=(c == 7)),
                                       waits=[t_xnT, W_W] + psA_free[a], inc=(s_p if c == 7 else None))
                        last_pe[0] = tm
                        k2 = cnt['k'] % 2; cnt['k'] += 1
                        tsq = fw.op('act', lambda e: e.activation(out=sqk[k2][:], in_=psA[a][:], func=AF.Square),
                                    waits=[tm] + sqk_free[k2], inc=s_a)
                        return ("norm", kind, ch, a, k2, tm, tsq)
                    if kind == "v":
                        ti, half = job[1], job[2]
                        a = cnt['a'] % 3; cnt['a'] += 1
                        tm = None
                        for c in range(8):
                            tm = fw.op('pe', lambda e: e.matmul(psA[a][:], xnT[bb][:, c, ti * 128:(ti + 1) * 128],
                                                                w_in[:, c, 2048 + half * 512:2048 + (half + 1) * 512],
                                                                start=(c == 0), stop=(c == 7)),
                                       waits=[t_xnT, W_W] + psA_free[a], inc=(s_p if c == 7 else None))
                        last_pe[0] = tm
                        tev = fw.op('act', lambda e: e.copy(vst[vb][:, half * 8:(half + 1) * 8, ti, 0:64],
                                                            psA[a][:].rearrange("p (h d) -> p h d", d=64)),
                                    waits=[tm, W_INIT] + vst_free[vb], inc=s_a)
                        psA_free[a] = [tev]
                        return ("vdone", tev)
                    if kind == "f":
                        ti = job[1]
                        tg = tb * 4 + ti
                        f2 = tg % 2
                        tm = None
                        for c in range(8):
                            tm = fw.op('pe', lambda e: e.matmul(psf[:, 0:16], xnT[bb][:, c, ti * 128:(ti + 1) * 128],
                                                                w_in[:, c, 3072:3088], start=(c == 0), stop=(c == 7)),
                                       waits=[t_xnT, W_W] + psf_free, inc=(s_p if c == 7 else None))
                        last_pe[0] = tm
                        t1 = fw.op('dve', lambda e: e.tensor_tensor(out=fl[f2][:], in0=psf[:, 0:16], in1=bf_bc[:], op=ALU.add),
                                   waits=[tm] + fl_free[f2], inc=s_v)
                        psf_free[:] = [t1]
                        t2 = fw.op('act', lambda e: e.activation(out=fl[f2][:], in_=fl[f2][:], func=AF.Exp, scale=-1.0),
                                   waits=[t1], inc=s_a)
                        t3 = fw.op('act', lambda e: e.activation(out=spv[f2][:], in_=fl[f2][:], func=AF.Ln, bias=1.0, scale=1.0),
                                   waits=[t2] + spv_free[f2], inc=s_a)
                        fl_free[f2] = [t3]
                        return ("cum", ti, tg, f2, t3)
                    raise ValueError

                def stage2(rec):
                    if rec[0] == "norm":
                        _, kind, ch, a, k2, tm, tsq = rec
                        j = cnt['j'] % 2; cnt['j'] += 1
                        t2 = fw.op('pe', lambda e: e.matmul(ps2[j][:], blk_bf[:], sqk[k2][:], start=True, stop=True),
                                   waits=[tsq] + W_CST + ps2_free[j], inc=s_p)
                        sqk_free[k2] = [t2]
                        tl = fw.op('act', lambda e: e.activation(out=lnv[j][:], in_=ps2[j][:], func=AF.Ln, scale=1.0 / 64, bias=EPS),
                                   waits=[t2] + lnv_free[j], inc=s_a)
                        ps2_free[j] = [tl]
                        tr = fw.op('act', lambda e: e.activation(out=rstd[j][:], in_=lnv[j][:], func=AF.Exp, scale=-0.5),
                                   waits=[tl] + rstd_free[j], inc=s_a)
                        lnv_free[j] = [tr]
                        ks = cnt.setdefault('ks', 0) % 3; cnt['ks'] = cnt.get('ks', 0) + 1
                        gcol = 1 if kind == "k" else 0
                        tk = fw.op('dve', lambda e: e.scalar_tensor_tensor(out=kst[ks][:], in0=psA[a][:], scalar=gq[:, gcol:gcol + 1],
                                                                            in1=rstd[j][:], op0=ALU.mult, op1=ALU.mult),
                                   waits=[tr, t_gq, tm] + kst_free[ks], inc=s_v)
                        psA_free[a] = [tk]
                        rstd_free[j] = [tk]
                        tdl = []
                        for hh in range(2):
                            h = 2 * ch + hh
                            if kind == "k":
                                dst = KT[h, 0:64, tb * 512:(tb + 1) * 512]
                            else:
                                dst = QT[h, 0:64, qcol:qcol + 512]
                            tdl.append(fw.dma('pool', dst, kst[ks][hh * 64:(hh + 1) * 64, :], waits=[tk], inc=s_st))
                        kst_free[ks] = tdl
                    elif rec[0] == "cum":
                        _, ti, tg, f2, t3 = rec
                        tc = fw.op('pe', lambda e: e.matmul(pscum[0:16, 0:128], spv[f2][:], triu_f[:], start=True, stop=True),
                                   waits=[t3] + W_CST + pscum_free, inc=s_p)
                        spv_free[f2] = [tc]
                        if tg == 0:
                            td = fw.op('dve', lambda e: e.tensor_copy(cum[0:16, 0:128], pscum[0:16, 0:128]), waits=[tc], inc=s_v)
                        else:
                            td = fw.op('dve', lambda e: e.tensor_scalar(out=cum[0:16, tg * 128:(tg + 1) * 128], in0=pscum[0:16, 0:128],
                                                                         scalar1=cum[0:16, tg * 128 - 1:tg * 128], scalar2=None, op0=ALU.add),
                                       waits=[tc, last_cum[0]], inc=s_v)
                        last_cum[0] = td
                        pscum_free[:] = [td]

                prev = None
                for job in jobs:
                    rec = stage1(job)
                    if prev is not None:
                        stage2(prev)
                    prev = rec if rec[0] in ("norm", "cum") else None
                    if rec[0] == "vdone":
                        pass
                if prev is not None:
                    stage2(prev)
                tv = (s_a, s_a.n)
                td = fw.dma('pool', VP.rearrange("h p t c -> p h t c")[:, :, tb * 4:(tb + 1) * 4, :], vst[vb][:], waits=[tv], inc=s_st)
                vst_free[vb] = [td]
                xnT_free[bb] = [(s_p, s_p.n)]

            if "wci" in cfg.stages:
                weight_convert_prepare()
            for tb in range(NBLK):
                emit_block(tb)

            t_cum = last_cum[0]
            with ExitStack() as ps2c:
                sb2 = lambda name, shape, dt: ps2c.enter_context(nc.sbuf_tensor("p1c_" + name, shape, dt))
                cq = [sb2("cq0", [16, 6, 1024], BF16)]
                ck = [sb2("ck0", [16, 6, 1024], BF16)]
                r1 = sb2("r1", [16, 1024], F32)
                r2 = sb2("r2", [16, 1024], F32)
                csum = sb2("csum", [16, 1024], F32)
                bigb = [sb2("bigb0", [16, 1024], F32)]
                csum_free = []
                tinit = None
                for i in range(1):
                    fw.op('pool', lambda e: e.memset(cq[i][:], 1.0), inc=s_g)
                    tinit = fw.op('pool', lambda e: e.memset(ck[i][:], 1.0), inc=s_g)
                c_free = [[], []]
                for cc in range(SK // 1024):
                    b = 0
                    src = cum[0:16, cc * 1024:(cc + 1) * 1024]
                    w0 = [t_cum, tinit] + c_free[b]
                    if cfg.masks:
                        tbb = fw.dma('sp', bigb[b][:], I["c_big"][cc * 1024:(cc + 1) * 1024].partition_broadcast(16), waits=c_free[b])
                        tsum = fw.op('dve', lambda e: e.tensor_tensor(out=csum[:], in0=src, in1=bigb[b][:], op=ALU.add), waits=[t_cum, tbb] + csum_free, inc=s_v)
                        src = csum[:]
                        w0 = [tsum, tinit] + c_free[b]
                    t = fw.op('dve', lambda e: e.tensor_copy(cq[b][:, 0, :], src), waits=w0, inc=s_v)
                    t = fw.op('dve', lambda e: e.tensor_tensor(out=r1[:], in0=src, in1=cq[b][:, 0, :], op=ALU.subtract), waits=[t], inc=s_v)
                    t = fw.op('dve', lambda e: e.tensor_copy(cq[b][:, 1, :], r1[:]), waits=[t], inc=s_v)
                    t = fw.op('dve', lambda e: e.tensor_tensor(out=r2[:], in0=r1[:], in1=cq[b][:, 1, :], op=ALU.subtract), waits=[t], inc=s_v)
                    t = fw.op('dve', lambda e: e.tensor_copy(cq[b][:, 2, :], r2[:]), waits=[t], inc=s_v)
                    t = fw.op('dve', lambda e: e.tensor_scalar(out=ck[b][:, 3:6, :], in0=cq[b][:, 0:3, :], scalar1=-1.0, scalar2=None,
                                                                op0=ALU.mult), waits=[t], inc=s_v)
                    csum_free = [t]
                    tdl = [fw.dma('pool', KT[:, 64:70, cc * 1024:(cc + 1) * 1024], ck[b][:], waits=[t], inc=s_st)]
                    span = (cc * 1024) // SPAN
                    if span in cfg.QS:
                        qc0 = cfg.QS.index(span) * SPAN + (cc * 1024) % SPAN
                        tdl.append(fw.dma('pool', QT[:, 64:70, qc0:qc0 + 1024], cq[b][:], waits=[t], inc=s_st))
                    c_free[b] = tdl
                done = fw.dma_all()
                for e in ('pe', 'act', 'dve', 'pool', 'sp'):
                    fw._waits(e, [done, (s_p, s_p.n), (s_a, s_a.n), (s_v, s_v.n), (s_g, s_g.n)])
            return done

    def phase2():
        with ExitStack() as ps:
            sb = lambda name, shape, dt: ps.enter_context(nc.sbuf_tensor("p2_" + name, shape, dt))
            pm = lambda name, shape, dt: ps.enter_context(nc.psum_tensor("p2P_" + name, shape, dt))
            kT = [sb(f"kT{i}", [70, SK], BF16) for i in range(2)]
            qT = [sb(f"qT{i}", [70, NQ], BF16) for i in range(2)]
            vS = [sb(f"vS{i}", [128, SK // 128, 65], BF16) for i in range(2)]
            NPB = 4
            pT = [sb(f"pT{i}", [128, 512], BF16) for i in range(NPB)]
            osb = [sb(f"osb{i}", [65, 512], F32) for i in range(2)]
            rc = [sb(f"rc{i}", [65, 512], F32) for i in range(2)]
            ost = [sb(f"ost{i}", [64, 512], BF16) for i in range(2)]
            ps_s = [pm(f"s{i}", [128, 512], F32) for i in range(NPB)]
            ps_o = [pm(f"o{i}", [65, 512], F32) for i in range(2)]
            ps_bc = pm("bc", [64, 512], F32)

            s_ld = None; s_S = fw.sem("p2S"); s_E = fw.sem("p2E"); s_PV = fw.sem("p2PV")
            s_oc = fw.sem("p2oc"); s_v = fw.sem("p2v"); s_bc = fw.sem("p2bc"); s_st = None

            qblocks = []
            for j, dspan in enumerate(cfg.QS):
                for qb in range(4):
                    nt = dspan * 16 + 4 * qb + 4
                    qblocks.append((j * SPAN + qb * 512, nt))

            head_free = [[], []]
            n = 0
            nqb = 0
            ld_tok = {}
            osb_free = [[], []]
            rc_free = [[], []]
            ost_free = [[], []]
            psbc_free = []
            pso_free = [[], []]

            def load_head(h):
                b = h % 2
                w = head_free[b]
                gl = fw.group('sp')
                fw.dma('sp', kT[b][:], KT[h, :, :], waits=w, inc=gl)
                fw.dma('sp', qT[b][:], QT[h, :, :], waits=w, inc=gl)
                t = fw.dma('sp', vS[b][:], VP[h, :, :, :], waits=w, inc=gl)
                ld_tok[h] = t

            load_head(0)
            for h in range(NH_A):
                b = h % 2
                if h + 1 < NH_A:
                    load_head(h + 1)
                if "wci" in cfg.stages:
                    wc_issue(6)
                W_LD = [ld_tok[h]] + W_CST
                pairs = []
                for qi, (qc0, nt) in enumerate(qblocks):
                    for kt in range(nt):
                        jj = kt - (nt - 4) if kt >= nt - 4 else None
                        pairs.append((qi, qc0, kt, jj, kt == 0, kt == nt - 1))
                NP_ = len(pairs)
                deferred = {}

                def S_job(idx):
                    nonlocal n
                    qi, qc0, kt, jj, first, last = pairs[idx]
                    g = n + idx
                    sbuf = g % NPB
                    wfree = [(s_E, g - NPB + 1)] if g - NPB + 1 > 0 else []
                    if jj is not None:
                        c0 = 128 * jj if not first else 0
                        fw.op('pe', lambda e: e.matmul(ps_s[sbuf][:, c0:512], ident_bf[:], negm_bf[:, jj * 512 + c0:(jj + 1) * 512],
                                                       start=True, stop=False), waits=W_LD + wfree)
                        fw.op('pe', lambda e: e.matmul(ps_s[sbuf][:, c0:512], kT[b][:, kt * 128:(kt + 1) * 128], qT[b][:, qc0 + c0:qc0 + 512],
                                                       start=False, stop=True), inc=s_S)
                    else:
                        fw.op('pe', lambda e: e.matmul(ps_s[sbuf][:], kT[b][:, kt * 128:(kt + 1) * 128], qT[b][:, qc0:qc0 + 512],
                                                       start=True, stop=True), waits=W_LD + wfree, inc=s_S)

                def E_job(idx):
                    g = n + idx
                    sbuf = g % NPB
                    jj_ = pairs[idx][3]
                    c0 = 128 * jj_ if (jj_ is not None and not pairs[idx][4]) else 0
                    wfree = [(s_PV, g - NPB + 1)] if g - NPB + 1 > 0 else []
                    fw.op('act', lambda e: e.activation(out=pT[sbuf][:, c0:512], in_=ps_s[sbuf][:, c0:512], func=AF.Exp),
                          waits=[(s_S, g + 1)] + wfree, inc=s_E)

                def PV_job(idx):
                    qi, qc0, kt, jj, first, last = pairs[idx]
                    g = n + idx
                    sbuf = g % NPB
                    ob = (nqb + qi) % 2
                    w = [(s_E, g + 1)]
                    if first:
                        w = w + pso_free[ob]
                    c0 = 128 * jj if (jj is not None and not first) else 0
                    fw.op('pe', lambda e: e.matmul(ps_o[ob][:, c0:512], vS[b][:, kt, :], pT[sbuf][:, c0:512], start=first, stop=last),
                          waits=w, inc=s_PV)

                def epi_act(qi, idx_last):
                    ob = (nqb + qi) % 2
                    g = n + idx_last
                    t = fw.op('dve', lambda e: e.tensor_copy(osb[ob][:], ps_o[ob][:]), waits=[(s_PV, g + 1)] + osb_free[ob], inc=s_v)
                    pso_free[ob] = [t]
                    t2 = fw.op('dve', lambda e: e.reciprocal(rc[ob][64:65, :], osb[ob][64:65, :]), waits=[t] + rc_free[ob], inc=s_v)
                    return t2

                def epi_pe(qi, t2):
                    ob = (nqb + qi) % 2
                    qc0 = qblocks[qi][0]
                    t3 = fw.op('pe', lambda e: e.matmul(ps_bc[:], ones_f[64:65, 0:64], rc[ob][64:65, :], start=True, stop=True),
                               waits=[t2] + W_CST + psbc_free, inc=s_bc)
                    rc_free[ob] = [t3]
                    t4 = fw.op('dve', lambda e: e.tensor_tensor(out=ost[ob][:], in0=osb[ob][0:64, :], in1=ps_bc[:], op=ALU.mult),
                               waits=[t3] + ost_free[ob], inc=s_v)
                    psbc_free[:] = [t4]
                    osb_free[ob] = [t4]
                    t5 = fw.dma('pool', AT0[h * 64:(h + 1) * 64, qc0:qc0 + 512], ost[ob][:], waits=[t4], inc=s_st)
                    ost_free[ob] = [t5]

                epi_state = {}
                for step in range(NP_ + 8):
                    if step < NP_:
                        S_job(step)
                    if step - 1 >= 0 and step - 1 < NP_:
                        E_job(step - 1)
                    if step - 2 >= 0 and step - 2 < NP_:
                        PV_job(step - 2)
                        qi, qc0, kt, jj, first, last = pairs[step - 2]
                        if last:
                            deferred.setdefault(step + 2, []).append(("act", qi, step - 2))
                    for item in deferred.pop(step, []):
                        if item[0] == "act":
                            t2 = epi_act(item[1], item[2])
                            deferred.setdefault(step + 3, []).append(("pe", item[1], t2))
                        else:
                            epi_pe(item[1], item[2])
                assert not deferred
                n += NP_
                nqb += len(qblocks)
                head_free[b] = [(s_PV, s_PV.n), (s_S, s_S.n)]
            done = fw.dma_all()
            for e in ('pe', 'act', 'dve', 'pool', 'sp'):
                fw._waits(e, [done, (s_PV, s_PV.n), (s_E, s_E.n), (s_v, s_v.n), (s_bc, s_bc.n), (s_oc, s_oc.n), (s_S, s_S.n)])
            return done


    def moe_phase(layer, n_pass, resid_fn, AT, w_out_name, nch, out_fn, tag):
        with ExitStack() as ps:
            sb = lambda name, shape, dt: ps.enter_context(nc.sbuf_tensor(f"{tag}_" + name, shape, dt))
            acc = sb("acc", [128, 16, D], F32)
            xnT = sb("xnT", [128, 8, SPAN], BF16)
            gates = sb("gates", [128, 16, NE], F32)
            s_ld = None; s_a = fw.sem(tag + "a"); s_v = fw.sem(tag + "v"); s_p = fw.sem(tag + "p")
            s_g = fw.sem(tag + "g"); s_st = None
            pass_free = []
            for pz in range(n_pass):
                resid = resid_fn(pz)
                outp = out_fn(pz)
                with ExitStack() as pa:
                    sba = lambda name, shape, dt: pa.enter_context(nc.sbuf_tensor(f"{tag}a{pz}_" + name, shape, dt))
                    pma = lambda name, shape, dt: pa.enter_context(nc.psum_tensor(f"{tag}aP{pz}_" + name, shape, dt))
                    w_out = sba("w_out", [128, nch, D], BF16)
                    at_sb = [sba(f"at{i}", [128, nch, 512], BF16) for i in range(2)]
                    g_bc = sba("g_bc", [128, D], F32)
                    xn = [sba(f"xn{i}", [128, D], F32) for i in range(2)]
                    junk = sba("junk", [128, D], BF16)
                    xnTf = [sba(f"xnTf{i}", [128, 8, 128], F32) for i in range(2)]
                    wr = sba("wr", [128, 8, 20], F32)
                    rb = sba("rb", [128, 20], F32)
                    st = sba("st", [128, 3 * 16], F32)
                    rt = [sba(f"rt{i}", [128, 96], F32) for i in range(2)]
                    ps_op = [pma(f"op{i}", [128, 512], F32) for i in range(2)]
                    ptfa = pma("ptfa", [128, 4, 128], F32)
                    ptfb = pma("ptfb", [128, 4, 128], F32)
                    ps_r = pma("r", [128, 512], F32)

                    s_w = fw.group('sp'); s_wp = fw.group('pool')
                    for c in range(nch):
                        fw.dma('pool', w_out[:, c, :], I[w_out_name][c * 128:(c + 1) * 128, :], waits=pass_free, inc=s_wp)
                    fw.dma('sp', g_bc[:], I["ffn_norm"][layer, :].partition_broadcast(128), waits=pass_free, inc=s_w)
                    fw.dma('sp', wr[:, :, 0:4], I["moe_w_group"][layer].rearrange("(c p) g -> p c g", p=128), inc=s_w)
                    fw.dma('sp', wr[:, :, 4:20], I["moe_w_expert"][layer].rearrange("(c p) g -> p c g", p=128), inc=s_w)
                    fw.dma('sp', rb[:, 0:4], I["moe_b_group"][layer, :].partition_broadcast(128), inc=s_w)
                    fw.dma('sp', rb[:, 4:20], I["moe_b_expert"][layer, :].partition_broadcast(128), inc=s_w)
                    W_W = [s_w.token(), s_wp.token()]
                    W_INIT = fw.op('pool', lambda e: e.memset(st[:], 0.0), waits=pass_free, inc=s_g)
                    at_free = [[], []]
                    op_free = [[], []]
                    xn_free = [[], []]
                    ptf_free = []
                    xnTf_free = [[], []]
                    psr_free = []
                    rt_free = [[], []]
                    last_gate = None
                    for blk in range(4):
                        ab = blk % 2
                        t_at = fw.dma('sp', at_sb[ab][:], AT[:, pz * SPAN + blk * 512: pz * SPAN + (blk + 1) * 512]
                                      .rearrange("(c p) n -> p c n", p=128), waits=at_free[ab] + pass_free, inc=s_ld)
                        t_rs = fw.dma('sp', acc[:, blk * 4:(blk + 1) * 4, :],
                                      resid[blk * 512:(blk + 1) * 512, :].rearrange("(t p) d -> p t d", p=128),
                                      waits=pass_free, inc=s_ld)
                        t_last_op = None
                        for ti in range(4):
                            t = blk * 4 + ti
                            xb = t % 2
                            tadd = None
                            for half in range(2):
                                tm = None
                                for c in range(nch):
                                    tm = fw.op('pe', lambda e: e.matmul(ps_op[half][:], at_sb[ab][:, c, ti * 128:(ti + 1) * 128],
                                                                        w_out[:, c, half * 512:(half + 1) * 512],
                                                                        start=(c == 0), stop=(c == nch - 1)),
                                               waits=[t_at, W_W] + op_free[half], inc=(s_p if c == nch - 1 else None))
                                t_last_op = tm
                                tadd = fw.op('dve', lambda e: e.tensor_tensor(out=acc[:, t, half * 512:(half + 1) * 512],
                                                                               in0=acc[:, t, half * 512:(half + 1) * 512],
                                                                               in1=ps_op[half][:], op=ALU.add),
                                             waits=[tm, t_rs], inc=s_v)
                                op_free[half] = [tadd]
                            KSTOP = int(os.environ.get('KSTOP', 9))
                            if KSTOP <= 1:
                                continue
                            t3 = rms_stats(acc[:, t, :], junk[:], st, 3 * t, D, [tadd, W_INIT], s_a)
                            tn = fw.op('dve', lambda e: e.scalar_tensor_tensor(out=xn[xb][:], in0=acc[:, t, :], scalar=st[:, 3 * t + 2:3 * t + 3],
                                                                                in1=g_bc[:], op0=ALU.mult, op1=ALU.mult),
                                       waits=[t3, W_W] + xn_free[xb], inc=s_v)
                            if KSTOP <= 2:
                                continue
                            tt = None
                            for c in range(8):
                                pdst = (ptfa if c < 4 else ptfb)[:, c % 4, :]
                                tt = fw.op('pe', lambda e: e.matmul(pdst, xn[xb][:, c * 128:(c + 1) * 128], ident_f[:], start=True, stop=True),
                                           waits=[tn] + W_CST + ptf_free, inc=(s_p if c == 7 else None))
                            xn_free[xb] = [tt]
                            fw.op('dve', lambda e: e.tensor_copy(xnTf[xb][:, 0:4, :], ptfa[:]), waits=[tt] + xnTf_free[xb], inc=s_v)
                            te2 = fw.op('dve', lambda e: e.tensor_copy(xnTf[xb][:, 4:8, :], ptfb[:]), inc=s_v)
                            te1 = fw.op('pool', lambda e: e.tensor_copy(xnT[:, :, t * 128:(t + 1) * 128], xnTf[xb][:]), waits=[te2] + pass_free, inc=s_g)
                            ptf_free = [te2]
                            if KSTOP <= 3:
                                continue
                            tr = None
                            for c in range(8):
                                tr = fw.op('pe', lambda e: e.matmul(ps_r[:, 0:20], xnTf[xb][:, c, :], wr[:, c, :], start=(c == 0), stop=(c == 7)),
                                           waits=[te2, W_W] + psr_free, inc=(s_p if c == 7 else None))
                            xnTf_free[xb] = [tr, te1]
                            if KSTOP <= 4:
                                continue
                            R = rt[xb]
                            lg = R[:, 0:20]; gmax = R[:, 20:21]; ngmax = R[:, 21:22]; ge = R[:, 22:26]; gsum = R[:, 26:27]
                            gval = R[:, 27:28]; goh = R[:, 28:32]; pen = R[:, 32:36]; elm = R[:, 36:52]; m1 = R[:, 52:53]
                            oh1 = R[:, 53:69]; elm2 = R[:, 69:85]; m2 = R[:, 85:86]; dm = R[:, 86:87]; ex = R[:, 87:88]
                            den = R[:, 88:89]; e1 = R[:, 89:90]; e2 = R[:, 90:91]; w1 = R[:, 91:92]; w2 = R[:, 92:93]
                            oh2 = xn[xb][:, 0:16]
                            oh2 = R[:, 93:96]
                            V = lambda fn, w: fw.op('dve', fn, waits=w, inc=s_v)
                            A = lambda fn, w: fw.op('act', fn, waits=w, inc=s_a)
                            q = V(lambda e: e.tensor_tensor(out=lg, in0=ps_r[:, 0:20], in1=rb[:], op=ALU.add), [tr, W_W] + rt_free[xb])
                            psr_free = [q]
                            q = V(lambda e: e.memset(gsum, 0.0), [q])
                            q = V(lambda e: e.reduce_max(out=gmax, in_=lg[:, 0:4], axis=AX.X), [q])
                            q = V(lambda e: e.tensor_scalar(out=ngmax, in0=gmax, scalar1=-1.0, scalar2=None, op0=ALU.mult), [q])
                            qa = A(lambda e: e.activation(out=ge, in_=lg[:, 0:4], func=AF.Exp, bias=ngmax, scale=1.0, accum_out=gsum), [q])
                            q = V(lambda e: e.reciprocal(gval, gsum), [qa])
                            q = V(lambda e: e.tensor_scalar(out=goh, in0=lg[:, 0:4], scalar1=gmax, scalar2=None, op0=ALU.is_equal), [q])
                            q = V(lambda e: e.tensor_scalar(out=pen, in0=goh, scalar1=-1.0, scalar2=-NEGV, op0=ALU.add, op1=ALU.mult), [q])
                            q = V(lambda e: e.tensor_tensor(out=elm.rearrange("p (g k) -> p g k", k=4), in0=lg[:, 4:20].rearrange("p (g k) -> p g k", k=4),
                                                            in1=pen.unsqueeze(2).to_broadcast([128, 4, 4]), op=ALU.add), [q])
                            q = V(lambda e: e.reduce_max(out=m1, in_=elm, axis=AX.X), [q])
                            q = V(lambda e: e.tensor_scalar(out=oh1, in0=elm, scalar1=m1, scalar2=None, op0=ALU.is_equal), [q])
                            q = V(lambda e: e.scalar_tensor_tensor(out=elm2, in0=oh1, scalar=NEGV, in1=elm, op0=ALU.mult, op1=ALU.add), [q])
                            q = V(lambda e: e.reduce_max(out=m2, in_=elm2, axis=AX.X), [q])
                            q = V(lambda e: e.tensor_tensor(out=dm, in0=m2, in1=m1, op=ALU.subtract), [q])
                            qa = A(lambda e: e.activation(out=ex, in_=dm, func=AF.Exp), [q])
                            q = V(lambda e: e.tensor_scalar(out=den, in0=ex, scalar1=1.0, scalar2=None, op0=ALU.add), [qa])
                            q = V(lambda e: e.reciprocal(e1, den), [q])
                            q = V(lambda e: e.tensor_tensor(out=e2, in0=ex, in1=e1, op=ALU.mult), [q])
                            q = V(lambda e: e.tensor_tensor(out=w1, in0=e1, in1=gval, op=ALU.mult), [q])
                            q = V(lambda e: e.tensor_tensor(out=w2, in0=e2, in1=gval, op=ALU.mult), [q])
                            q = V(lambda e: e.tensor_scalar(out=elm, in0=elm2, scalar1=m2, scalar2=w2, op0=ALU.is_equal, op1=ALU.mult), [q])
                            q = V(lambda e: e.scalar_tensor_tensor(out=gates[:, t, :], in0=oh1, scalar=w1, in1=elm, op0=ALU.mult, op1=ALU.add),
                                  [q] + pass_free)
                            rt_free[xb] = [q]
                            last_gate = q
                        at_free[ab] = [t_last_op]
                    W_A = [last_gate, (s_a, s_a.n), (s_p, s_p.n), (s_g, s_g.n)]
                    for e_ in ('pe', 'act', 'dve', 'pool', 'sp'):
                        fw._waits(e_, W_A + [(s_v, s_v.n), (s_g, s_g.n)] + fw.dma_all())
                with ExitStack() as pb:
                    sbb = lambda name, shape, dt: pb.enter_context(nc.sbuf_tensor(f"{tag}b{pz}_" + name, shape, dt))
                    pmb = lambda name, shape, dt: pb.enter_context(nc.psum_tensor(f"{tag}bP{pz}_" + name, shape, dt))
                    wg = [sbb(f"wg{i}", [128, 8, DE], BF16) for i in range(2)]
                    wu = [sbb(f"wu{i}", [128, 8, DE], BF16) for i in range(2)]
                    wd = [sbb(f"wd{i}", [128, 4, D], BF16) for i in range(2)]
                    hidT = [sbb(f"hid{i}", [128, 4, 512], BF16) for i in range(2)]
                    sg = [sbb(f"sg{i}", [128, 512], F32) for i in range(2)]
                    ps_g = [pmb(f"g{i}", [128, 512], F32) for i in range(2)]
                    ps_u = [pmb(f"u{i}", [128, 512], F32) for i in range(2)]
                    ps_d = [pmb(f"d{i}", [128, 512], F32) for i in range(3)]
                    w_free = [[], []]
                    w_tok = {}

                    def load_w(e_):
                        b = e_ % 2
                        s_wp = fw.group('pool')
                        for c in range(8):
                            fw.dma('pool', wg[b][:, c, :], I["moe_w_gate"][layer, e_, c * 128:(c + 1) * 128, :], waits=w_free[b], inc=s_wp)
                            fw.dma('pool', wu[b][:, c, :], I["moe_w_up"][layer, e_, c * 128:(c + 1) * 128, :], waits=w_free[b], inc=s_wp)
                        for c in range(4):
                            fw.dma('pool', wd[b][:, c, :], I["moe_w_down"][layer, e_, c * 128:(c + 1) * 128, :], waits=w_free[b], inc=s_wp)
                        w_tok[e_] = s_wp.token()

                    g_free = [[], []]; u_free = [[], []]; d_free = [[], [], []]
                    hid_free = [[], []]; sg_free = [[], []]
                    cntb = dict(gu=0, d=0, u=0)
                    units = [(e_, blk) for e_ in range(int(os.environ.get('KNEXP', NE))) for blk in range(4)]
                    hid_ready = {}

                    def stageA(ui):
                        e_, blk = units[ui]
                        b = e_ % 2
                        hb = ui % 2
                        toks = []
                        for fc in range(4):
                            gb = cntb['gu'] % 2; cntb['gu'] += 1
                            tg = None
                            for c in range(8):
                                tg = fw.op('pe', lambda e: e.matmul(ps_g[gb][:], wg[b][:, c, fc * 128:(fc + 1) * 128], xnT[:, c, blk * 512:(blk + 1) * 512],
                                                                    start=(c == 0), stop=(c == 7)),
                                           waits=[w_tok[e_]] + g_free[gb], inc=(s_p if c == 7 else None))
                            tu = None
                            for c in range(8):
                                tu = fw.op('pe', lambda e: e.matmul(ps_u[gb][:], wu[b][:, c, fc * 128:(fc + 1) * 128], xnT[:, c, blk * 512:(blk + 1) * 512],
                                                                    start=(c == 0), stop=(c == 7)),
                                           waits=u_free[gb], inc=(s_p if c == 7 else None))
                            ts = fw.op('act', lambda e: e.activation(out=sg[gb][:], in_=ps_g[gb][:], func=AF.Silu),
                                       waits=[tg] + sg_free[gb], inc=s_a)
                            g_free[gb] = [ts]
                            th = fw.op('dve', lambda e: e.tensor_tensor(out=hidT[hb][:, fc, :], in0=sg[gb][:], in1=ps_u[gb][:], op=ALU.mult),
                                       waits=[ts, tu] + (hid_free[hb] if fc == 0 else []), inc=s_v)
                            u_free[gb] = [th]
                            sg_free[gb] = [th]
                            toks.append(th)
                        hid_ready[ui] = toks[-1]

                    def stageB(ui):
                        e_, blk = units[ui]
                        b = e_ % 2
                        hb = ui % 2
                        tm = None
                        for tt in range(4):
                            t = blk * 4 + tt
                            for dh in range(2):
                                db = cntb['d'] % 3; cntb['d'] += 1
                                for fc in range(4):
                                    tm = fw.op('pe', lambda e: e.matmul(ps_d[db][:], hidT[hb][:, fc, tt * 128:(tt + 1) * 128],
                                                                        wd[b][:, fc, dh * 512:(dh + 1) * 512], start=(fc == 0), stop=(fc == 3)),
                                               waits=[hid_ready[ui]] + d_free[db], inc=(s_p if fc == 3 else None))
                                ta = fw.op('dve', lambda e: e.scalar_tensor_tensor(out=acc[:, t, dh * 512:(dh + 1) * 512], in0=ps_d[db][:],
                                                                                    scalar=gates[:, t, e_:e_ + 1], in1=acc[:, t, dh * 512:(dh + 1) * 512],
                                                                                    op0=ALU.mult, op1=ALU.add),
                                           waits=[tm], inc=s_v)
                                d_free[db] = [ta]
                        hid_free[hb] = [tm]
                        if blk == 3:
                            w_free[b] = [tm]
                            if e_ + 2 < len(units) // 4:
                                load_w(e_ + 2)

                    if len(units) > 0:
                        load_w(0)
                    if len(units) > 4:
                        load_w(1)
                    for ui in range(len(units) + 1):
                        if ui < len(units):
                            stageA(ui)
                        if ui - 1 >= 0:
                            stageB(ui - 1)
                    t_fin = (s_v, s_v.n)
                    td = fw.dma('sp', outp.rearrange("(t p) d -> p t d", p=128), acc[:], waits=[t_fin], inc=s_st)
                    pass_free = [td, (s_p, s_p.n)]
                    for e_ in ('pe', 'act', 'dve', 'pool', 'sp'):
                        fw._waits(e_, [td, (s_p, s_p.n), (s_a, s_a.n), (s_v, s_v.n)] + fw.dma_all())
            return pass_free[0]


    def proj_phase(tag, src, N, norm_ap, w_ap, WC, gain_aps, njobs, vgroups):
        NBLK = N // 512
        with ExitStack() as ps:
            sb = lambda name, shape, dt: ps.enter_context(nc.sbuf_tensor(f"{tag}_" + name, shape, dt))
            pm = lambda name, shape, dt: ps.enter_context(nc.psum_tensor(f"{tag}P_" + name, shape, dt))
            w_sb = sb("w", [128, 8, WC], BF16)
            g_bc = sb("g_bc", [128, D], F32)
            gq = sb("gq", [128, max(1, len(gain_aps))], F32)
            xs = [sb(f"xs{i}", [128, D], F32) for i in range(2)]
            junk = sb("junk", [128, D], BF16)
            xn = [sb(f"xn{i}", [128, D], BF16) for i in range(2)]
            xnT = [sb(f"xnT{i}", [128, 8, 512], BF16) for i in range(2)]
            st = sb("st", [128, 3 * (N // 128)], F32)
            sqk = [sb(f"sqk{i}", [128, 512], BF16) for i in range(2)]
            lnv = [sb(f"lnv{i}", [128, 512], F32) for i in range(2)]
            rstd = [sb(f"rstd{i}", [128, 512], F32) for i in range(2)]
            kst = [sb(f"kst{i}", [128, 512], BF16) for i in range(3)]
            NG = len(vgroups)
            vst = [sb(f"vst{i}", [128, max(1, NG), 4, 8, 65], BF16) for i in range(2)] if NG else None
            psA = [pm(f"psA{i}", [128, 512], F32) for i in range(5)]
            ps2 = [pm(f"ps2{i}", [128, 512], F32) for i in range(2)]
            pt = pm("pt0", [128, 8, 128], BF16)
            s_a = fw.sem(tag + "a"); s_v = fw.sem(tag + "v"); s_p = fw.sem(tag + "p"); s_g = fw.sem(tag + "g")
            gwp = fw.group('pool')
            for c in range(8):
                fw.dma('pool', w_sb[:, c, :], w_ap[c * 128:(c + 1) * 128, :], inc=gwp)
            gws = fw.group('sp')
            fw.dma('sp', g_bc[:], norm_ap.partition_broadcast(128), inc=gws)
            for gi, (gap, gscale) in enumerate(gain_aps):
                for hh in range(2):
                    fw.dma('sp', gq[hh * 64:(hh + 1) * 64, gi:gi + 1], gap.rearrange("(d o) -> d o", o=1), inc=gws)
            W_W = [gwp.token(), gws.token()]
            t = fw.op('pool', lambda e: e.memset(st[:], 0.0), inc=s_g)
            if NG:
                for i in range(2):
                    t = fw.op('pool', lambda e: e.memset(vst[i][:], 1.0), inc=s_g)
            W_INIT = t
            t_gq = None
            for gi, (gap, gscale) in enumerate(gain_aps):
                if gscale != 1.0:
                    t_gq = fw.op('dve', lambda e: e.tensor_scalar(out=gq[:, gi:gi + 1], in0=gq[:, gi:gi + 1], scalar1=gscale, scalar2=None,
                                                                    op0=ALU.mult), waits=[W_W], inc=s_v)
            xs_free = [[], []]; xn_free = [[], []]; pt_free = [[]]; xnT_free = [[], []]
            psA_free = [[] for _ in range(5)]; ps2_free = [[], []]; sqk_free = [[], []]; lnv_free = [[], []]; rstd_free = [[], []]
            kst_free = [[], [], []]; vst_free = [[], []]
            cnt = dict(a=0, k=0, j=0, ks=0)

            def tile_front(tb, ti):
                tg = tb * 4 + ti
                b = tg % 2; bb = tb % 2
                tx = fw.dma('sp', xs[b][:], src[tg * 128:(tg + 1) * 128, :], waits=xs_free[b])
                t3 = rms_stats(xs[b][:], junk[:], st, 3 * tg, D, [tx, W_INIT], s_a)
                tn = fw.op('dve', lambda e: e.scalar_tensor_tensor(out=xn[b][:], in0=xs[b][:], scalar=st[:, 3 * tg + 2:3 * tg + 3],
                                                                    in1=g_bc[:], op0=ALU.mult, op1=ALU.mult),
                           waits=[t3, W_W] + xn_free[b], inc=s_v)
                xs_free[b] = [tn]
                tt = None
                for c in range(8):
                    tt = fw.op('pe', lambda e: e.transpose(pt[:, c, :], xn[b][:, c * 128:(c + 1) * 128], ident_bf[:]),
                               waits=[tn] + W_CST + pt_free[0], inc=(s_p if c == 7 else None))
                xn_free[b] = [tt]
                te = fw.op('dve', lambda e: e.tensor_copy(xnT[bb][:, :, ti * 128:(ti + 1) * 128], pt[:]),
                           waits=[tt] + xnT_free[bb], inc=s_v)
                pt_free[0] = [te]
                return te

            for tb in range(NBLK):
                bb = tb % 2
                t_xnT = None
                for ti in range(4):
                    t_xnT = tile_front(tb, ti)
                vb = tb % 2
                jobs = [("n",) + j for j in njobs]
                for ti in range(4):
                    for gi, vcol in enumerate(vgroups):
                        jobs.append(("v", ti, gi, vcol))

                def stage1(job):
                    a = cnt['a'] % 5; cnt['a'] += 1
                    if job[0] == "n":
                        _, col0, gcol, dst = job
                        tm = None
                        for c in range(8):
                            tm = fw.op('pe', lambda e: e.matmul(psA[a][:], w_sb[:, c, col0:col0 + 128], xnT[bb][:, c, :],
                                                                start=(c == 0), stop=(c == 7)),
                                       waits=[t_xnT, W_W] + psA_free[a], inc=(s_p if c == 7 else None))
                        k2 = cnt['k'] % 2; cnt['k'] += 1
                        tsq = fw.op('act', lambda e: e.activation(out=sqk[k2][:], in_=psA[a][:], func=AF.Square),
                                    waits=[tm] + sqk_free[k2], inc=s_a)
                        return ("norm", gcol, dst, a, k2, tm, tsq)
                    _, ti, gi, vcol = job
                    tm = None
                    for c in range(8):
                        tm = fw.op('pe', lambda e: e.matmul(psA[a][:], xnT[bb][:, c, ti * 128:(ti + 1) * 128], w_sb[:, c, vcol:vcol + 512],
                                                            start=(c == 0), stop=(c == 7)),
                                   waits=[t_xnT, W_W] + psA_free[a], inc=(s_p if c == 7 else None))
                    tev = fw.op('act', lambda e: e.copy(vst[vb][:, gi, ti, :, 0:64], psA[a][:].rearrange("p (h d) -> p h d", d=64)),
                                waits=[tm, W_INIT] + vst_free[vb], inc=s_a)
                    psA_free[a] = [tev]
                    return None

                def stage2(rec):
                    _, gcol, dst, a, k2, tm, tsq = rec
                    j = cnt['j'] % 2; cnt['j'] += 1
                    t2 = fw.op('pe', lambda e: e.matmul(ps2[j][:], blk_bf[:], sqk[k2][:], start=True, stop=True),
                               waits=[tsq] + W_CST + ps2_free[j], inc=s_p)
                    sqk_free[k2] = [t2]
                    tl = fw.op('act', lambda e: e.activation(out=lnv[j][:], in_=ps2[j][:], func=AF.Ln, scale=1.0 / 64, bias=EPS),
                               waits=[t2] + lnv_free[j], inc=s_a)
                    ps2_free[j] = [tl]
                    tr = fw.op('act', lambda e: e.activation(out=rstd[j][:], in_=lnv[j][:], func=AF.Exp, scale=-0.5),
                               waits=[tl] + rstd_free[j], inc=s_a)
                    lnv_free[j] = [tr]
                    ks = cnt['ks'] % 3; cnt['ks'] += 1
                    tk = fw.op('dve', lambda e: e.scalar_tensor_tensor(out=kst[ks][:], in0=psA[a][:], scalar=gq[:, gcol:gcol + 1],
                                                                        in1=rstd[j][:], op0=ALU.mult, op1=ALU.mult),
                               waits=[tr, t_gq, tm] + kst_free[ks], inc=s_v)
                    psA_free[a] = [tk]
                    rstd_free[j] = [tk]
                    td = fw.dma('pool', dst[:, tb * 512:(tb + 1) * 512], kst[ks][:], waits=[tk])
                    kst_free[ks] = [td]

                prev = None
                for job in jobs:
                    rec = stage1(job)
                    if prev is not None:
                        stage2(prev)
                    prev = rec
                if prev is not None:
                    stage2(prev)
                if NG:
                    tv = (s_a, s_a.n)
                    td = None
                    for gi in range(NG):
                        td = fw.dma('pool', VB[gi, tb * 512:(tb + 1) * 512, :].rearrange("(t p) c -> p t c", p=128),
                                    vst[vb][:, gi, :, :, :].rearrange("p t h c -> p t (h c)"), waits=[tv])
                    vst_free[vb] = [td] + fw.dma_all()
                xnT_free[bb] = [(s_p, s_p.n)]
            done = fw.dma_all()
            for e in ('pe', 'act', 'dve', 'pool', 'sp'):
                fw._waits(e, [done, (s_p, s_p.n), (s_a, s_a.n), (s_v, s_v.n), (s_g, s_g.n)])

    def phase5():
        njobs = [(ch * 128, 0, KB[ch * 128:(ch + 1) * 128, :]) for ch in range(12)]
        proj_phase("p5", H1, NQ, I["kv_norm"], I["kv_w"], 3072, [(I["kv_k_gain"], 1.0)], njobs, [1536, 2048, 2560])

    def phase5b():
        for bi, (qs, pv) in enumerate(cfg.BS):
            njobs = [(ch * 128, 0, QB[ch * 128:(ch + 1) * 128, bi * SPAN:(bi + 1) * SPAN]) for ch in range(12)]
            proj_phase(f"p5b{bi}", H1[qs * SPAN:(qs + 1) * SPAN, :], SPAN, I["b_norm"][0] if False else I["b_norm"], I["b_w_q"], 1536,
                       [(I["b_q_gain"], SCALE)], njobs, [])

    def phase6():
        with ExitStack() as ps:
            sb = lambda name, shape, dt: ps.enter_context(nc.sbuf_tensor("p6_" + name, shape, dt))
            pm = lambda name, shape, dt: ps.enter_context(nc.psum_tensor("p6P_" + name, shape, dt))
            expB = sb("expB", [128, 3, 8, 2, 128], BF16)
            kbias = sb("kbias", [128, len(cfg.QS)], F32)
            ps_s = [pm(f"s{i}", [128, 512], F32) for i in range(4)]
            ps_o = [pm(f"o{i}", [65, 512], F32) for i in range(2)]
            ps_bc = pm("bc", [128, 512], F32)
            s_a = fw.sem("p6a"); s_v = fw.sem("p6v"); s_p = fw.sem("p6p"); s_g = fw.sem("p6g")

            with ExitStack() as pb:
                sbb = lambda name, shape, dt: pb.enter_context(nc.sbuf_tensor("p6b_" + name, shape, dt))
                biasT = sbb("biasT", [128, 3, 8, 2, 128], F32)
                rb_sb = sbb("rb", [32, 24], F32)
                oh_sb = sbb("oh", [32, 3, 384], F32)
                ng_sb = sbb("ng", [1, 3, 384], F32)
                anti = sbb("anti", [128, 128], F32)
                fvs = sbb("fvs", [24, 3, 384], F32)
                hank = [sbb(f"hank{i}", [128, 128], F32) for i in range(2)]
                g0 = fw.group('sp')
                fw.dma('sp', rb_sb[:], I["rel_bias"][:, :], inc=g0)
                fw.dma('sp', oh_sb[:], I["c_oh"].rearrange("g k m -> k g m"), inc=g0)
                fw.dma('sp', ng_sb[:], I["c_ng"].rearrange("g o m -> o g m"), inc=g0)
                fw.dma('sp', anti[:], I["c_anti"][:, :], inc=g0)
                if cfg.masks:
                    fw.dma('sp', kbias[:], I["c_kbias"].partition_broadcast(128), inc=g0)
                tk0 = g0.token()
                tprev = []
                tds = []
                for g in range(3):
                    fw.op('pe', lambda e: e.matmul(ps_bc[0:24, 0:384], rb_sb[:], oh_sb[:, g, :], start=True, stop=False),
                          waits=[tk0] + W_CST + tprev)
                    tm = fw.op('pe', lambda e: e.matmul(ps_bc[0:24, 0:384], ones_f[0:1, 0:24], ng_sb[0:1, g, :], start=False, stop=True), inc=s_p)
                    tc = fw.op('dve', lambda e: e.tensor_copy(fvs[:, g, :], ps_bc[0:24, 0:384]), waits=[tm], inc=s_v)
                    tprev = [tc]
                    tds.append(fw.dma('sp', FV[g * 8:(g + 1) * 8, :], fvs[g * 8:(g + 1) * 8, g, :], waits=[tc]))
                hk_free = [[], []]
                ci = 0
                for g in range(3):
                    for h in range(8):
                        for pc in range(2):
                            hb = ci % 2; ci += 1
                            head = g * 8 + h
                            off = head * 384 + (128 if pc == 0 else 0)
                            srcap = bass.AP(FV.tensor, off, [[1, 128], [1, 128]])
                            tl = fw.dma('sp', hank[hb][:], srcap, waits=tds + hk_free[hb])
                            tm = fw.op('pe', lambda e: e.matmul(ps_bc[:, 0:128], anti[:], hank[hb][:], start=True, stop=True),
                                       waits=[tl] + tprev, inc=s_p)
                            hk_free[hb] = [tm]
                            tc = fw.op('dve', lambda e: e.tensor_copy(biasT[:, g, h, pc, :], ps_bc[:, 0:128]), waits=[tm], inc=s_v)
                            tprev = [tc]
                t_eb = fw.op('act', lambda e: e.activation(out=expB[:].rearrange("p g h c a -> p (g h c a)"),
                                                             in_=biasT[:].rearrange("p g h c a -> p (g h c a)"), func=AF.Exp), waits=tprev, inc=s_a)
                W_BIAS = [t_eb]
                for e in ('pe', 'act', 'dve', 'pool', 'sp'):
                    fw._waits(e, W_BIAS + [(s_p, s_p.n)] + fw.dma_all())

            k_sb = sb("k", [128, 4, 2 * SPAN], BF16)
            q_sb = sb("q", [128, 4, SPAN], BF16)
            v_sb = sb("v", [128, 2, 16, 520], BF16)
            oacc = sb("oacc", [65, 8, SPAN], F32)
            p_sb = [sb(f"p{i}", [128, 512], BF16) for i in range(4)]
            rc = [sb(f"rc{i}", [65, 512], F32) for i in range(2)]
            ost = [sb(f"ost{i}", [64, 512], BF16) for i in range(2)]
            ld_free = []
            oacc_free = []
            st_s = dict(a=0, o=0)
            psS_free = [[], [], [], []]
            pS_free = [[], [], [], []]
            pso_free = [[], []]
            bc_free = list(W_BIAS)
            rc_free = [[], []]; ost_free = [[], []]
            for bi, (qs, pv) in enumerate(cfg.BS):
                last_acc_tok = None
                for g, d in enumerate(B_DIL):
                    gl = fw.group('sp')
                    for c in range(4):
                        row0 = (4 * g + c) * 128
                        if pv is not None:
                            fw.dma('sp', k_sb[:, c, 0:SPAN], KB[row0:row0 + 128, pv * SPAN:(pv + 1) * SPAN], waits=ld_free, inc=gl)
                        fw.dma('sp', k_sb[:, c, SPAN:2 * SPAN], KB[row0:row0 + 128, qs * SPAN:(qs + 1) * SPAN], waits=ld_free, inc=gl)
                        fw.dma('sp', q_sb[:, c, :], QB[row0:row0 + 128, bi * SPAN:(bi + 1) * SPAN], waits=ld_free, inc=gl)

                    def vsrc(slot):
                        rows = VB[g, slot * SPAN:(slot + 1) * SPAN, :]
                        if d == 1:
                            return rows.rearrange("(b a) c -> a b c", a=128)
                        if d == 4:
                            return rows.rearrange("(m a r) c -> a m r c", m=4, a=128, r=4)
                        return rows.rearrange("(a r) c -> a r c", r=16)

                    def vdst(pc):
                        if d == 4:
                            return v_sb[:, pc, :, :].rearrange("a (m r) c -> a m r c", r=4)
                        return v_sb[:, pc, :, :]
                    if pv is not None:
                        fw.dma('sp', vdst(0), vsrc(pv), waits=ld_free, inc=gl)
                    fw.dma('sp', vdst(1), vsrc(qs), waits=ld_free, inc=gl)
                    W_LD = [gl.token()]

                    def blk_start(beta):
                        if d == 1:
                            return beta * 128
                        if d == 4:
                            return (beta // 4) * 512 + (beta % 4)
                        return beta

                    def prev_info(beta):
                        st0 = blk_start(beta) - 128 * d
                        if st0 >= 0:
                            return (1, beta - (1 if d == 1 else 4))
                        if pv is None:
                            return None
                        return (0, 15 if d == 1 else (12 + beta % 4 if d == 4 else beta))

                    units = [(h, qd) for h in range(8) for qd in range(4)]
                    urec = {}

                    def stageA(ui):
                        h, qd = units[ui]
                        c = h // 2; hh = h % 2
                        prs = [prev_info(4 * qd + i) for i in range(4)]
                        npv = sum(1 for p_ in prs if p_ is None)
                        c0 = 128 * npv
                        a0 = st_s['a'] % 4; a1 = (st_s['a'] + 1) % 4; st_s['a'] += 2
                        tp = None
                        for i in range(4):
                            if prs[i] is None:
                                continue
                            s0 = blk_start(4 * qd + i)
                            kcols = slice(SPAN + s0 - 128 * d, SPAN + s0 - 128 * d + 127 * d + 1, d)
                            qcols = slice(s0, s0 + 127 * d + 1, d)
                            tp = fw.op('pe', lambda e: e.matmul(ps_s[a0][:, i * 128:(i + 1) * 128], k_sb[hh * 64:(hh + 1) * 64, c, kcols],
                                                                q_sb[hh * 64:(hh + 1) * 64, c, qcols], start=True, stop=True),
                                       waits=W_LD + psS_free[a0], inc=s_p)
                        tcur = None
                        for i in range(4):
                            s0 = blk_start(4 * qd + i)
                            kcols = slice(SPAN + s0, SPAN + s0 + 127 * d + 1, d)
                            qcols = slice(s0, s0 + 127 * d + 1, d)
                            tcur = fw.op('pe', lambda e: e.matmul(ps_s[a1][:, i * 128:(i + 1) * 128], k_sb[hh * 64:(hh + 1) * 64, c, kcols],
                                                                  q_sb[hh * 64:(hh + 1) * 64, c, qcols], start=True, stop=True),
                                         waits=W_LD + psS_free[a1], inc=s_p)
                        tE0 = None
                        if tp is not None:
                            nb_ = 4 - npv
                            te0 = None
                            i0 = npv
                            while i0 < 4:
                                i1 = i0
                                while i1 < 4 and (prs[i1][0] == 0) == (prs[i0][0] == 0):
                                    i1 += 1
                                use_kb = cfg.masks and prs[i0][0] == 0
                                bias_arg = kbias[:, pv:pv + 1] if use_kb else 0.0
                                te0 = fw.op('act', lambda e: e.activation(out=p_sb[a0][:, i0 * 128:i1 * 128], in_=ps_s[a0][:, i0 * 128:i1 * 128],
                                                                          func=AF.Exp, bias=bias_arg, scale=1.0),
                                            waits=[tp, tk0] + pS_free[a0], inc=s_a)
                                i0 = i1
                            psS_free[a0] = [te0]
                            tE0 = fw.op('pool', lambda e: e.tensor_tensor(out=p_sb[a0][:, c0:512].rearrange("p (i a) -> p i a", a=128),
                                                                           in0=p_sb[a0][:, c0:512].rearrange("p (i a) -> p i a", a=128),
                                                                           in1=expB[:, g, h, 0, :].unsqueeze(1).to_broadcast([128, nb_, 128]), op=ALU.mult),
                                        waits=[te0] + W_BIAS, inc=s_g)
                        te1 = fw.op('act', lambda e: e.activation(out=p_sb[a1][:], in_=ps_s[a1][:], func=AF.Exp),
                                    waits=[tcur] + pS_free[a1], inc=s_a)
                        psS_free[a1] = [te1]
                        tE1 = fw.op('dve', lambda e: e.tensor_tensor(out=p_sb[a1][:].rearrange("p (i a) -> p i a", a=128),
                                                                      in0=p_sb[a1][:].rearrange("p (i a) -> p i a", a=128),
                                                                      in1=expB[:, g, h, 1, :].unsqueeze(1).to_broadcast([128, 4, 128]), op=ALU.mult),
                                    waits=[te1] + W_BIAS, inc=s_v)
                        urec[ui] = (prs, a0, a1, tE0, tE1)

                    def stageB(ui):
                        nonlocal last_acc_tok
                        h, qd = units[ui]
                        prs, a0, a1, tE0, tE1 = urec.pop(ui)
                        ob = st_s['o'] % 2; st_s['o'] += 1
                        tm = None
                        for i in range(4):
                            beta = 4 * qd + i
                            if prs[i] is not None:
                                vbuf, vblk = prs[i]
                                fw.op('pe', lambda e: e.matmul(ps_o[ob][:, i * 128:(i + 1) * 128], v_sb[:, vbuf, vblk, h * 65:(h + 1) * 65],
                                                               p_sb[a0][:, i * 128:(i + 1) * 128], start=True, stop=False),
                                      waits=[tE0, tE1] + pso_free[ob])
                            tm = fw.op('pe', lambda e: e.matmul(ps_o[ob][:, i * 128:(i + 1) * 128], v_sb[:, 1, beta, h * 65:(h + 1) * 65],
                                                                p_sb[a1][:, i * 128:(i + 1) * 128], start=(prs[i] is None), stop=True),
                                       waits=[tE0, tE1] + pso_free[ob], inc=(s_p if i == 3 else None))
                        pS_free[a0] = [tm]; pS_free[a1] = [tm]
                        if d == 1:
                            dst = oacc[0:65, h, qd * 512:(qd + 1) * 512].rearrange("p (i a) -> p i a", a=128)
                        elif d == 4:
                            dst = oacc[0:65, h, qd * 512:(qd + 1) * 512].rearrange("p (a r) -> p r a", r=4)
                        else:
                            dst = oacc[0:65, h, :].rearrange("p (a r) -> p r a", r=16)[:, 4 * qd:4 * qd + 4, :]
                        srcp = ps_o[ob][:].rearrange("p (i a) -> p i a", a=128)
                        if g == 0:
                            ta = fw.op('dve', lambda e: e.tensor_copy(dst, srcp), waits=[tm] + oacc_free, inc=s_v)
                        else:
                            ta = fw.op('dve', lambda e: e.tensor_tensor(out=dst, in0=dst, in1=srcp, op=ALU.add), waits=[tm, last_acc_tok], inc=s_v)
                        last_acc_tok = ta
                        pso_free[ob] = [ta]

                    for ui in range(len(units) + 1):
                        if ui < len(units):
                            stageA(ui)
                        if ui >= 1:
                            stageB(ui - 1)
                    ld_free = [(s_p, s_p.n)]
                t_last = None
                ei = 0
                for h in range(8):
                    for cb in range(4):
                        eb = ei % 2; ei += 1
                        cols = slice(cb * 512, (cb + 1) * 512)
                        t1 = fw.op('dve', lambda e: e.reciprocal(rc[eb][64:65, :], oacc[64:65, h, cols]), waits=[last_acc_tok] + rc_free[eb], inc=s_v)
                        t2 = fw.op('pe', lambda e: e.matmul(ps_bc[0:64, :], ones_f[64:65, 0:64], rc[eb][64:65, :], start=True, stop=True),
                                   waits=[t1] + W_CST + bc_free, inc=s_p)
                        rc_free[eb] = [t2]
                        t3 = fw.op('dve', lambda e: e.tensor_tensor(out=ost[eb][:], in0=oacc[0:64, h, cols], in1=ps_bc[0:64, :], op=ALU.mult),
                                   waits=[t2] + ost_free[eb], inc=s_v)
                        bc_free = [t3]
                        t4 = fw.dma('pool', AT1[h * 64:(h + 1) * 64, bi * SPAN + cb * 512: bi * SPAN + (cb + 1) * 512], ost[eb][:], waits=[t3])
                        ost_free[eb] = [t4]
                        t_last = t3
                oacc_free = [t_last]
            done = fw.dma_all()
            for e in ('pe', 'act', 'dve', 'pool', 'sp'):
                fw._waits(e, [done, (s_p, s_p.n), (s_a, s_a.n), (s_v, s_v.n), (s_g, s_g.n)])


    def moe_sparse(layer, N, resid_rows_fn, AT, w_out_name, nch, out_rows_fn, tag, wc_tok):
        NTT = N // 128
        G = 4
        SUP = 128 * G
        STOT = 2 * N + NE * SUP
        NTL = STOT // 128
        NSUP = STOT // SUP
        DUM = 2 * N
        OOBV = DUM
        XN = scratch(tag + "XN", [2 * N + 1, D], BF16)
        HM = scratch(tag + "HM", [N, D], F32)
        OUT2 = scratch(tag + "OUT2", [2 * N + 1, D], F32)
        TOKIDX = scratch(tag + "TOKIDX", [STOT, 1], mybir.dt.int32)
        I32 = mybir.dt.int32
        with ExitStack() as ps:
            sb = lambda name, shape, dt: ps.enter_context(nc.sbuf_tensor(f"{tag}_" + name, shape, dt))
            OH = sb("OH", [128, NTT, 2, 16], F32)
            OHb = sb("OHb", [128, NTT, 2, 16], BF16)
            W12 = sb("W12", [128, NTT, 2], F32)
            RANK = sb("RANK", [128, NTT, 2], F32)
            CNT = sb("CNT", [128, 16], F32)
            SLOTI = sb("SLOTI", [128, NTT, 2], I32)
            WIDX = sb("WIDX", [128, NSUP], I32)
            TOKID = sb("TOKID", [128, 2, NTT], I32)
            stri = sb("stri", [128, 128], BF16)
            ones_b = sb("ones_b", [128, 128], BF16)
            s_a = fw.sem(tag + "a"); s_v = fw.sem(tag + "v"); s_p = fw.sem(tag + "p"); s_g = fw.sem(tag + "g")
            gk = fw.group('pool')
            fw.dma('pool', stri[:], I["c_stri"][:, :], inc=gk)
            t_ob = fw.op('pool', lambda e: e.memset(ones_b[:], 1.0), inc=s_g)
            t_c0 = fw.op('pool', lambda e: e.memset(CNT[:], 0.0), inc=s_g)
            fw.op('pool', lambda e: e.iota(TOKID[:, 0, :], pattern=[[128, NTT]], base=0, channel_multiplier=1), inc=s_g)
            t_tid = fw.op('pool', lambda e: e.iota(TOKID[:, 1, :], pattern=[[128, NTT]], base=N, channel_multiplier=1), inc=s_g)
            W_K = [gk.token(), t_ob, t_c0]
            with ExitStack() as pa:
                sba = lambda name, shape, dt: pa.enter_context(nc.sbuf_tensor(f"{tag}a_" + name, shape, dt))
                pma = lambda name, shape, dt: pa.enter_context(nc.psum_tensor(f"{tag}aP_" + name, shape, dt))
                w_out = sba("w_out", [128, nch, D], BF16)
                at_sb = [sba(f"at{i}", [128, nch, 512], BF16) for i in range(2)]
                g_bc = sba("g_bc", [128, D], F32)
                hb_ = [sba(f"h{i}", [128, 4, D], F32) for i in range(2)]
                xn = [sba(f"xn{i}", [128, D], F32) for i in range(2)]
                xnb = [sba(f"xnb{i}", [128, D], BF16) for i in range(2)]
                junk = sba("junk", [128, D], BF16)
                xnTf = [sba(f"xnTf{i}", [128, 8, 128], F32) for i in range(2)]
                wr = sba("wr", [128, 8, 20], F32)
                rb = sba("rb", [128, 20], F32)
                st = sba("st", [128, 3 * NTT], F32)
                rt = [sba(f"rt{i}", [128, 640], F32) for i in range(2)]
                ps_op = [pma(f"op{i}", [128, 512], F32) for i in range(2)]
                ptfa = pma("ptfa", [128, 4, 128], F32)
                ptfb = pma("ptfb", [128, 4, 128], F32)
                ps_r = [pma(f"r{i}", [128, 512], F32) for i in range(2)]
                ps_k = pma("k", [128, 512], F32)

                s_w = fw.group('sp'); s_wp = fw.group('pool')
                for c in range(nch):
                    fw.dma('pool', w_out[:, c, :], I[w_out_name][c * 128:(c + 1) * 128, :], inc=s_wp)
                fw.dma('sp', g_bc[:], I["ffn_norm"][layer, :].partition_broadcast(128), inc=s_w)
                fw.dma('sp', wr[:, :, 0:4], I["moe_w_group"][layer].rearrange("(c p) g -> p c g", p=128), inc=s_w)
                fw.dma('sp', wr[:, :, 4:20], I["moe_w_expert"][layer].rearrange("(c p) g -> p c g", p=128), inc=s_w)
                fw.dma('sp', rb[:, 0:4], I["moe_b_group"][layer, :].partition_broadcast(128), inc=s_w)
                fw.dma('sp', rb[:, 4:20], I["moe_b_expert"][layer, :].partition_broadcast(128), inc=s_w)
                W_W = [s_w.token(), s_wp.token()]
                W_INIT = fw.op('pool', lambda e: e.memset(st[:], 0.0), inc=s_g)
                at_free = [[], []]; op_free = [[], []]; xn_free = [[], []]; ptf_free = []; xnTf_free = [[], []]
                psr_free = [[], []]; rt_free = [[], []]; h_free = [[], []]; xnb_free = [[], []]; psk_free = []
                last_cnt = t_c0
                pending_rank = []
                pipe = []
                stage_f2 = []
                rblocks = {}

                def advance_pipe(flush):
                    while stage_f2 and (flush or len(stage_f2) > 1 or True):
                        it = stage_f2.pop(0)
                        tr_ = it['F3'](it['te2'])
                        if it['ti'] == 3:
                            pending_rank.append((it['blk'], tr_))
                        break
                    while pipe and (flush or len(pipe) > 1):
                        it = pipe.pop(0)
                        it['te2'] = it['F2']()
                        stage_f2.append(it)
                        if not flush:
                            break
                    if flush:
                        while stage_f2:
                            it = stage_f2.pop(0)
                            tr_ = it['F3'](it['te2'])
                            if it['ti'] == 3:
                                pending_rank.append((it['blk'], tr_))

                def run_router_blocks(flush):
                    nonlocal rank_fn
                    while pending_rank and pending_rank[0][0] in rblocks:
                        bk, tr_ = pending_rank.pop(0)
                        fn = rblocks.pop(bk)(tr_)
                        if rank_fn is not None:
                            rank_fn()
                        rank_fn = fn
                    if flush and rank_fn is not None:
                        rank_fn()
                        rank_fn = None
                rank_fn = None
                for blk in range(N // 512):
                    ab = blk % 2
                    t_at = fw.dma('sp', at_sb[ab][:], AT[:, blk * 512:(blk + 1) * 512].rearrange("(c p) n -> p c n", p=128),
                                  waits=at_free[ab])
                    t_rs = fw.dma('sp', hb_[ab][:], resid_rows_fn(blk).rearrange("(t p) d -> p t d", p=128), waits=h_free[ab])
                    t_last_op = None
                    t_hdone = []
                    for ti in range(4):
                        t = blk * 4 + ti
                        xb = t % 2
                        hT = hb_[ab][:, ti, :]
                        tadd = None
                        for half in range(2):
                            tm = None
                            for c in range(nch):
                                tm = fw.op('pe', lambda e: e.matmul(ps_op[half][:], at_sb[ab][:, c, ti * 128:(ti + 1) * 128],
                                                                    w_out[:, c, half * 512:(half + 1) * 512],
                                                                    start=(c == 0), stop=(c == nch - 1)),
                                           waits=[t_at, W_W] + op_free[half], inc=(s_p if c == nch - 1 else None))
                            t_last_op = tm
                            tadd = fw.op('dve', lambda e: e.tensor_tensor(out=hT[:, half * 512:(half + 1) * 512],
                                                                           in0=hT[:, half * 512:(half + 1) * 512],
                                                                           in1=ps_op[half][:], op=ALU.add),
                                         waits=[tm, t_rs], inc=s_v)
                            op_free[half] = [tadd]
                        t3 = rms_stats(hT, junk[:], st, 3 * t, D, [tadd, W_INIT], s_a)
                        tn = fw.op('dve', lambda e: e.scalar_tensor_tensor(out=xn[xb][:], in0=hT, scalar=st[:, 3 * t + 2:3 * t + 3],
                                                                            in1=g_bc[:], op0=ALU.mult, op1=ALU.mult),
                                   waits=[t3, W_W] + xn_free[xb], inc=s_v)
                        t_hdone.append(tn)
                        tcb = fw.op('pool', lambda e: e.tensor_copy(xnb[xb][:], xn[xb][:]), waits=[tn] + xnb_free[xb], inc=s_g)
                        txs = fw.dma('sp', XN[t * 128:(t + 1) * 128, :], xnb[xb][:], waits=[tcb])
                        txs2 = fw.dma('sp', XN[N + t * 128:N + (t + 1) * 128, :], xnb[xb][:], waits=[tcb])
                        xnb_free[xb] = [txs, txs2]
                        def F2(t=t, xb=xb, tn=tn, tcb=tcb):
                            nonlocal ptf_free
                            tt = None
                            for c in range(8):
                                pdst = (ptfa if c < 4 else ptfb)[:, c % 4, :]
                                tt = fw.op('pe', lambda e: e.matmul(pdst, xn[xb][:, c * 128:(c + 1) * 128], ident_f[:], start=True, stop=True),
                                           waits=[tn] + W_CST + ptf_free, inc=(s_p if c == 7 else None))
                            xn_free[xb] = [tt, tcb]
                            fw.op('dve', lambda e: e.tensor_copy(xnTf[xb][:, 0:4, :], ptfa[:]), waits=[tt] + xnTf_free[xb], inc=s_v)
                            te2 = fw.op('dve', lambda e: e.tensor_copy(xnTf[xb][:, 4:8, :], ptfb[:]), inc=s_v)
                            ptf_free = [te2]
                            return te2

                        def F3(te2, t=t, xb=xb, blk=blk, ti=ti):
                            tr = None
                            for c in range(8):
                                tr = fw.op('pe', lambda e: e.matmul(ps_r[blk % 2][:, ti * 20:(ti + 1) * 20], xnTf[xb][:, c, :], wr[:, c, :], start=(c == 0), stop=(c == 7)),
                                           waits=[te2, W_W] + psr_free[blk % 2], inc=(s_p if c == 7 else None))
                            xnTf_free[xb] = [tr]
                            return tr
                        pipe.append(dict(F2=F2, F3=F3, blk=blk, ti=ti))
                        advance_pipe(False)
                    def router_block(tr_last, blk=blk, ab=ab):
                        nonlocal last_cnt, psk_free
                        rb_i = blk % 2
                        R = rt[rb_i]
                        t0 = blk * 4
                        off = [0]

                        def alloc(n):
                            v = R[:, off[0]:off[0] + n]
                            off[0] += n
                            return v
                        LG = alloc(80).rearrange("p (t c) -> p t c", c=20)
                        gmax = alloc(4); gsh = alloc(16).rearrange("p (t g) -> p t g", g=4); gsum = alloc(4); gval = alloc(4)
                        goh = alloc(16).rearrange("p (t g) -> p t g", g=4); pen = alloc(16).rearrange("p (t g) -> p t g", g=4)
                        elm = alloc(64).rearrange("p (t e) -> p t e", e=16); m1 = alloc(4)
                        elm2 = alloc(64).rearrange("p (t e) -> p t e", e=16); m2 = alloc(4); dm = alloc(4); ex = alloc(4)
                        den = alloc(4); e1 = alloc(4); e2 = alloc(4)
                        cntp = alloc(128).rearrange("p (t c) -> p t c", c=32)
                        rk = alloc(128).rearrange("p (t c) -> p t c", c=32)
                        V = lambda fn, w: fw.op('dve', fn, waits=w, inc=s_v)
                        A = lambda fn, w: fw.op('act', fn, waits=w, inc=s_a)
                        PR = ps_r[rb_i]
                        oh1 = OH[:, t0:t0 + 4, 0, :]; oh2 = OH[:, t0:t0 + 4, 1, :]
                        q = V(lambda e: e.tensor_tensor(out=LG, in0=PR[:, 0:80].rearrange("p (t c) -> p t c", c=20),
                                                        in1=rb[:].unsqueeze(1).to_broadcast([128, 4, 20]), op=ALU.add), [tr_last, W_W] + rt_free[rb_i])
                        psr_free[rb_i] = [q]
                        q = V(lambda e: e.reduce_max(out=gmax, in_=LG[:, :, 0:4], axis=AX.X), [q])
                        q = V(lambda e: e.tensor_tensor(out=gsh, in0=LG[:, :, 0:4], in1=gmax.unsqueeze(2).to_broadcast([128, 4, 4]), op=ALU.subtract), [q])
                        qa = A(lambda e: e.activation(out=gsh, in_=gsh, func=AF.Exp), [q])
                        q = V(lambda e: e.reduce_sum(out=gsum, in_=gsh, axis=AX.X), [qa])
                        q = V(lambda e: e.reciprocal(gval, gsum), [q])
                        q = V(lambda e: e.tensor_tensor(out=goh, in0=LG[:, :, 0:4], in1=gmax.unsqueeze(2).to_broadcast([128, 4, 4]), op=ALU.is_equal), [q])
                        q = V(lambda e: e.tensor_scalar(out=pen, in0=goh, scalar1=-1.0, scalar2=-NEGV, op0=ALU.add, op1=ALU.mult), [q])
                        q = V(lambda e: e.tensor_tensor(out=elm.rearrange("p t (g k) -> p t g k", k=4),
                                                        in0=LG[:, :, 4:20].rearrange("p t (g k) -> p t g k", k=4),
                                                        in1=pen.unsqueeze(3).to_broadcast([128, 4, 4, 4]), op=ALU.add), [q])
                        q = V(lambda e: e.reduce_max(out=m1, in_=elm, axis=AX.X), [q])
                        q = V(lambda e: e.tensor_tensor(out=oh1, in0=elm, in1=m1.unsqueeze(2).to_broadcast([128, 4, 16]), op=ALU.is_equal), [q, W_K])
                        q = V(lambda e: e.scalar_tensor_tensor(out=elm2, in0=oh1, scalar=NEGV, in1=elm, op0=ALU.mult, op1=ALU.add), [q])
                        q = V(lambda e: e.reduce_max(out=m2, in_=elm2, axis=AX.X), [q])
                        q = V(lambda e: e.tensor_tensor(out=oh2, in0=elm2, in1=m2.unsqueeze(2).to_broadcast([128, 4, 16]), op=ALU.is_equal), [q])
                        q = V(lambda e: e.tensor_tensor(out=dm, in0=m2, in1=m1, op=ALU.subtract), [q])
                        qa = A(lambda e: e.activation(out=ex, in_=dm, func=AF.Exp), [q])
                        q = V(lambda e: e.tensor_scalar(out=den, in0=ex, scalar1=1.0, scalar2=None, op0=ALU.add), [qa])
                        q = V(lambda e: e.reciprocal(e1, den), [q])
                        q = V(lambda e: e.tensor_tensor(out=e2, in0=ex, in1=e1, op=ALU.mult), [q])
                        q = V(lambda e: e.tensor_tensor(out=W12[:, t0:t0 + 4, 0], in0=e1, in1=gval, op=ALU.mult), [q])
                        q = V(lambda e: e.tensor_tensor(out=W12[:, t0:t0 + 4, 1], in0=e2, in1=gval, op=ALU.mult), [q])
                        ohf = OH[:, t0:t0 + 4, :, :].rearrange("p t k e -> p t (k e)")
                        ohb = OHb[:, t0:t0 + 4, :, :].rearrange("p t k e -> p t (k e)")
                        q_oh = V(lambda e: e.tensor_copy(ohb, ohf), [q])

                        def rank_part():
                            nonlocal last_cnt, psk_free
                            tk = None
                            for ti in range(4):
                                fw.op('pe', lambda e: e.matmul(ps_k[:, ti * 64:ti * 64 + 32], stri[:], OHb[:, t0 + ti, :, :].rearrange("p k e -> p (k e)"),
                                                               start=True, stop=True), waits=[q_oh] + W_K + psk_free)
                                tk = fw.op('pe', lambda e: e.matmul(ps_k[:, ti * 64 + 32:ti * 64 + 64], ones_b[:], OHb[:, t0 + ti, :, :].rearrange("p k e -> p (k e)"),
                                                                    start=True, stop=True), inc=s_p)
                            PK = ps_k[:, 0:256].rearrange("p (t c) -> p t c", c=64)
                            cntf = CNT[:]
                            qq = V(lambda e: e.tensor_copy(cntp[:, 0, 0:16], cntf), [tk, last_cnt])
                            for ti in range(4):
                                qq = V(lambda e: e.tensor_tensor(out=cntp[:, ti, 16:32], in0=cntp[:, ti, 0:16], in1=PK[:, ti, 32:48], op=ALU.add), [qq])
                                dst_ = cntp[:, ti + 1, 0:16] if ti < 3 else cntf
                                qq = V(lambda e: e.tensor_tensor(out=dst_, in0=cntp[:, ti, 16:32], in1=PK[:, ti, 48:64], op=ALU.add), [qq])
                            qc = qq
                            qq = V(lambda e: e.tensor_tensor(out=rk, in0=PK[:, :, 0:32], in1=cntp, op=ALU.add), [qc])
                            qq = V(lambda e: e.tensor_tensor(out=rk, in0=rk, in1=ohf, op=ALU.mult), [qq])
                            qq = V(lambda e: e.reduce_sum(out=RANK[:, t0:t0 + 4, :], in_=rk.rearrange("p t (k e) -> p t k e", e=16), axis=AX.X), [qq])
                            last_cnt = qq
                            psk_free = [qq]
                            rt_free[rb_i] = [qq]
                        return rank_part
                    rblocks[blk] = router_block
                    run_router_blocks(False)
                    at_free[ab] = [t_last_op]
                    ths = fw.dma('sp', HM[blk * 512:(blk + 1) * 512, :].rearrange("(t p) d -> p t d", p=128), hb_[ab][:], waits=t_hdone)
                    h_free[ab] = [ths]
                advance_pipe(True)
                run_router_blocks(True)
                assert not pipe and not stage_f2 and not pending_rank and not rblocks
                for e_ in ('pe', 'act', 'dve', 'pool', 'sp'):
                    fw._waits(e_, [last_cnt, (s_a, s_a.n), (s_p, s_p.n), (s_v, s_v.n), (s_g, s_g.n)] + fw.dma_all())
            with ExitStack() as pbx:
                sbb = lambda name, shape, dt: pbx.enter_context(nc.sbuf_tensor(f"{tag}B_" + name, shape, dt))
                MT = (2 * N) // SUP
                THR = sbb("THR", [128, MT], F32)
                THRJ = sbb("THRJ", [128, NSUP], F32)
                IOP = sbb("IOP", [128, 1], F32)
                LT = sbb("LT", [128, 16, 16], F32)
                big = sbb("big", [128, 16 * max(MT, NSUP)], F32)
                NTI = sbb("NTI", [128, 16], F32)
                PAD = sbb("PAD", [128, 16], F32)
                BASE = sbb("BASE", [128, 16], F32)
                END = sbb("END", [128, 16], F32)
                tmp2 = sbb("tmp2", [128, 16, 16], F32)
                SLT = sbb("SLT", [128, NTT, 2, 16], F32)
                SLF = sbb("SLF", [128, NTT, 2], F32)
                EBF = sbb("EBF", [128, NSUP], F32)
                FILL = sbb("FILL", [128, STOT // 128], I32)
                g1 = fw.group('sp')
                fw.dma('sp', LT[:].rearrange("p a b -> p (a b)"), I["c_lt"].partition_broadcast(128), inc=g1)
                fw.op('pool', lambda e: e.iota(THR[:], pattern=[[SUP, MT]], base=0, channel_multiplier=0, allow_small_or_imprecise_dtypes=True), inc=s_g)
                fw.op('pool', lambda e: e.iota(THRJ[:], pattern=[[SUP, NSUP]], base=0, channel_multiplier=0, allow_small_or_imprecise_dtypes=True), inc=s_g)
                fw.op('pool', lambda e: e.iota(IOP[:], pattern=[[0, 1]], base=0, channel_multiplier=1, allow_small_or_imprecise_dtypes=True), inc=s_g)
                t_fill = fw.op('pool', lambda e: e.memset(FILL[:], OOBV), inc=s_g)
                t_tf = fw.dma('pool', TOKIDX.rearrange("(p a) o -> p (a o)", p=128), FILL[:], waits=[t_fill])
                V = lambda fn, w: fw.op('dve', fn, waits=w, inc=s_v)
                q = V(lambda e: e.tensor_tensor(out=big[:, 0:16 * MT].rearrange("p (a m) -> p a m", m=MT),
                                                in0=CNT[:].unsqueeze(2).to_broadcast([128, 16, MT]),
                                                in1=THR[:].unsqueeze(1).to_broadcast([128, 16, MT]), op=ALU.is_gt), [(s_g, s_g.n), g1.token()])
                q = V(lambda e: e.reduce_sum(out=NTI[:], in_=big[:, 0:16 * MT].rearrange("p (a m) -> p a m", m=MT), axis=AX.X), [q])
                q = V(lambda e: e.tensor_scalar(out=PAD[:], in0=NTI[:], scalar1=float(SUP), scalar2=None, op0=ALU.mult), [q])
                q = V(lambda e: e.tensor_tensor(out=tmp2[:], in0=PAD[:].unsqueeze(1).to_broadcast([128, 16, 16]), in1=LT[:], op=ALU.mult), [q])
                q = V(lambda e: e.reduce_sum(out=BASE[:], in_=tmp2[:], axis=AX.X), [q])
                q = V(lambda e: e.tensor_tensor(out=END[:], in0=BASE[:], in1=PAD[:], op=ALU.add), [q])
                q = V(lambda e: e.tensor_tensor(out=SLT[:], in0=OH[:], in1=BASE[:].unsqueeze(1).unsqueeze(1).to_broadcast([128, NTT, 2, 16]), op=ALU.mult), [q])
                q = V(lambda e: e.reduce_sum(out=SLF[:], in_=SLT[:], axis=AX.X), [q])
                q = V(lambda e: e.tensor_tensor(out=SLF[:], in0=SLF[:], in1=RANK[:], op=ALU.add), [q])
                q = V(lambda e: e.tensor_copy(SLOTI[:], SLF[:]), [q])
                t_slot = q
                q = V(lambda e: e.tensor_tensor(out=big[:, 0:NSUP * 16].rearrange("p (j e) -> p j e", e=16),
                                                in0=END[:].unsqueeze(1).to_broadcast([128, NSUP, 16]),
                                                in1=THRJ[:].unsqueeze(2).to_broadcast([128, NSUP, 16]), op=ALU.is_le), [q])
                q = V(lambda e: e.reduce_sum(out=EBF[:], in_=big[:, 0:NSUP * 16].rearrange("p (j e) -> p j e", e=16), axis=AX.X), [q])
                q = V(lambda e: e.tensor_scalar(out=EBF[:], in0=EBF[:], scalar1=15.0, scalar2=128.0, op0=ALU.min, op1=ALU.mult), [q])
                q = V(lambda e: e.tensor_scalar(out=EBF[:], in0=EBF[:], scalar1=IOP[:, 0:1], scalar2=None, op0=ALU.add), [q])
                q = V(lambda e: e.tensor_copy(WIDX[:], EBF[:]), [q])
                t_widx = q
                tsc = []
                SSTOP = os.environ.get('SSTOP', '')
                for t in range(NTT if SSTOP != 'B' else 0):
                    for k in range(2):
                        fw._waits('pool', [t_slot, t_tf, t_tid])
                        sring = fw._ring_next('pool')
                        ins = nc.gpsimd.indirect_dma_start(out=TOKIDX[:, :], out_offset=bass.IndirectOffsetOnAxis(ap=SLOTI[:, t, k:k + 1], axis=0),
                                                           in_=TOKID[:, k, t:t + 1], in_offset=None)
                        sring.n += 16
                        ins.then_inc(sring.h, 16)
                for e_ in ('pe', 'act', 'dve', 'pool', 'sp'):
                    fw._waits(e_, [t_widx, (s_v, s_v.n), (s_g, s_g.n)] + fw.dma_all())
            if SSTOP in ('B', 'C'):
                return
            with ExitStack() as pd:
                sbd = lambda name, shape, dt: pd.enter_context(nc.sbuf_tensor(f"{tag}D_" + name, shape, dt))
                pmd = lambda name, shape, dt: pd.enter_context(nc.psum_tensor(f"{tag}DP_" + name, shape, dt))
                wall = [sbd(f"wall{i}", [128, 12288], BF16) for i in range(3)]
                NXG = 5
                xg = [sbd(f"xg{i}", [128, D], BF16) for i in range(NXG)]
                NID = 10
                idt = [sbd(f"idt{i}", [128, 1], I32) for i in range(NID)]
                xgT = [sbd(f"xgT{i}", [128, 8, 128], BF16) for i in range(2)]
                sg = [sbd(f"sg{i}", [128, 512], F32) for i in range(2)]
                hid = [sbd(f"hid{i}", [128, 512], BF16) for i in range(2)]
                hidT = [sbd(f"hidT{i}", [128, 4, 128], BF16) for i in range(2)]
                NOD = 4
                od = [sbd(f"od{i}", [128, D], F32) for i in range(NOD)]
                ptx = pmd("ptx", [128, 8, 128], BF16)
                pth = pmd("pth", [128, 8, 128], BF16)
                ps_g = [pmd(f"g{i}", [128, 512], F32) for i in range(1)]
                ps_u = [pmd(f"u{i}", [128, 512], F32) for i in range(1)]
                ps_d = [pmd(f"d{i}", [128, 512], F32) for i in range(2)]
                t_z = None
                for i in range(NXG):
                    t_z = fw.op('pool', lambda e: e.memset(xg[i][:], 0.0), inc=s_g)
                t_zr = fw.dma('pool', XN[DUM:DUM + 1, :], xg[0][0:1, :], waits=[t_z])
                t_z = [t_z, t_zr]
                wall_free = [[], [], []]; xg_free = [[] for _ in range(NXG)]; idt_free = [[] for _ in range(NID)]; xgT_free = [[], []]
                sg_free = [[], []]; hid_free = [[], []]; hidT_free = [[], []]; od_free = [[] for _ in range(NOD)]
                ptx_free = []; pth_free = []; g_free = []; u_free = []; d_free = [[], []]
                rec = {}
                sc_pend = {}
                wtok = {}
                last_sc = [[], []]

                def ind_dma(out, out_off, in_, in_off, bound, waits):
                    fw._waits('pool', waits)
                    sring = fw._ring_next('pool')
                    ins = nc.gpsimd.indirect_dma_start(out=out, out_offset=out_off, in_=in_, in_offset=in_off)
                    sring.n += 16
                    ins.then_inc(sring.h, 16)
                    return (sring, sring.n)

                idtok = {}

                def load_idt(j):
                    ib = j % NID
                    idtok[j] = fw.dma('sp', idt[ib][:], TOKIDX[j * 128:(j + 1) * 128, :], waits=idt_free[ib])

                def loads(j):
                    ib = j % NID; xb = j % NXG
                    J = j // G; wb = J % 3
                    t_i = idtok.pop(j)
                    if j % G == 0:
                        wtok[J] = ind_dma(wall[wb][:], None, WALL[layer][:, :], bass.IndirectOffsetOnAxis(ap=WIDX[:, J:J + 1], axis=0), NE * 128 - 1,
                                          [wc_tok] + wall_free[wb])
                    t_x = ind_dma(xg[xb][:], None, XN[:, :], bass.IndirectOffsetOnAxis(ap=idt[ib][:, :], axis=0), N - 1, [t_i, t_z] + xg_free[xb])
                    rec[j] = dict(t_x=t_x, t_w=wtok[J], t_i=t_i)

                def stage1(j):
                    xb = j % 2; xgb = j % NXG; wb = (j // G) % 3; r = rec[j]
                    tt = None
                    for c in range(8):
                        tt = fw.op('pe', lambda e: e.transpose(ptx[:, c, :], xg[xgb][:, c * 128:(c + 1) * 128], ident_bf[:]),
                                   waits=[r['t_x']] + W_CST + ptx_free, inc=(s_p if c == 7 else None))
                    xg_free[xgb] = [tt]
                    te = fw.op('dve', lambda e: e.tensor_copy(xgT[xb][:], ptx[:]), waits=[tt] + xgT_free[xb], inc=s_v)
                    ptx_free[:] = [te]
                    tg = tu = None
                    for c in range(8):
                        tg = fw.op('pe', lambda e: e.matmul(ps_g[0][:], xgT[xb][:, c, :], wall[wb][:, c * 512:(c + 1) * 512], start=(c == 0), stop=(c == 7)),
                                   waits=[te, r['t_w']] + g_free, inc=(s_p if c == 7 else None))
                        tu = fw.op('pe', lambda e: e.matmul(ps_u[0][:], xgT[xb][:, c, :], wall[wb][:, 4096 + c * 512:4096 + (c + 1) * 512],
                                                            start=(c == 0), stop=(c == 7)),
                                   waits=u_free, inc=(s_p if c == 7 else None))
                    xgT_free[xb] = [tu]
                    ts = fw.op('act', lambda e: e.activation(out=sg[xb][:], in_=ps_g[0][:], func=AF.Silu), waits=[tg] + sg_free[xb], inc=s_a)
                    g_free[:] = [ts]
                    th = fw.op('dve', lambda e: e.tensor_tensor(out=hid[xb][:], in0=sg[xb][:], in1=ps_u[0][:], op=ALU.mult),
                               waits=[ts, tu] + hid_free[xb], inc=s_v)
                    u_free[:] = [th]
                    sg_free[xb] = [th]
                    r['th'] = th

                def stage2(j):
                    xb = j % 2; wb = (j // G) % 3; ib = j % NID; ob = j % NOD; r = rec.pop(j)
                    tt = None
                    for fc in range(4):
                        tt = fw.op('pe', lambda e: e.transpose(pth[:, fc, :], hid[xb][:, fc * 128:(fc + 1) * 128], ident_bf[:]),
                                   waits=[r['th']] + pth_free, inc=(s_p if fc == 3 else None))
                    hid_free[xb] = [tt]
                    te = fw.op('dve', lambda e: e.tensor_copy(hidT[xb][:], pth[:, 0:4, :]), waits=[tt] + hidT_free[xb], inc=s_v)
                    pth_free[:] = [te]
                    tds = []
                    tm = None
                    for dh in range(2):
                        for fc in range(4):
                            tm = fw.op('pe', lambda e: e.matmul(ps_d[dh][:], hidT[xb][:, fc, :],
                                                                wall[wb][:, 8192 + fc * 1024 + dh * 512:8192 + fc * 1024 + (dh + 1) * 512],
                                                                start=(fc == 0), stop=(fc == 3)),
                                       waits=[te] + d_free[dh], inc=(s_p if fc == 3 else None))
                        tv = fw.op('dve', lambda e: e.tensor_copy(od[ob][:, dh * 512:(dh + 1) * 512], ps_d[dh][:]), waits=[tm] + od_free[ob], inc=s_v)
                        d_free[dh] = [tv]
                        tds.append(tv)
                    hidT_free[xb] = [tm]
                    if j % G == G - 1:
                        wall_free[wb] = [tm]
                    sc_pend[j] = (0, ib, ob, tds[-1])

                def scatter(j):
                    k, ib, ob, tdone = sc_pend.pop(j)
                    t_o = ind_dma(OUT2[:, :], bass.IndirectOffsetOnAxis(ap=idt[ib][:, :], axis=0), od[ob][:], None, N - 1, [tdone] + last_sc[k])
                    last_sc[k] = [t_o]
                    od_free[ob] = [t_o]
                    idt_free[ib] = [t_o]

                LA_I = 6; LA_X = 3
                for j in range(min(LA_I, NTL)):
                    load_idt(j)
                for j in range(min(LA_X, NTL)):
                    loads(j)
                for j in range(NTL + 3):
                    if j + LA_I < NTL:
                        load_idt(j + LA_I)
                    if j + LA_X < NTL:
                        loads(j + LA_X)
                    if j < NTL:
                        stage1(j)
                    if 1 <= j <= NTL:
                        stage2(j - 1)
                    if 2 <= j - 0 and (j - 2) in sc_pend:
                        scatter(j - 2)
                assert not sc_pend and not rec
                for e_ in ('pe', 'act', 'dve', 'pool', 'sp'):
                    fw._waits(e_, [(s_a, s_a.n), (s_p, s_p.n), (s_v, s_v.n), (s_g, s_g.n)] + fw.dma_all())
            with ExitStack() as pe_:
                sbe = lambda name, shape, dt: pe_.enter_context(nc.sbuf_tensor(f"{tag}E_" + name, shape, dt))
                hE = [sbe(f"h{i}", [128, D], F32) for i in range(2)]
                o1 = [sbe(f"o1{i}", [128, D], F32) for i in range(2)]
                o2 = [sbe(f"o2{i}", [128, D], F32) for i in range(2)]
                e_free = [[], []]
                for t in range(NTT):
                    b = t % 2
                    ge_ = fw.group('sp')
                    fw.dma('sp', hE[b][:], HM[t * 128:(t + 1) * 128, :], waits=e_free[b], inc=ge_)
                    fw.dma('sp', o1[b][:], OUT2[t * 128:(t + 1) * 128, :], waits=e_free[b], inc=ge_)
                    fw.dma('sp', o2[b][:], OUT2[N + t * 128:N + (t + 1) * 128, :], waits=e_free[b], inc=ge_)
                    q = fw.op('dve', lambda e: e.scalar_tensor_tensor(out=hE[b][:], in0=o1[b][:], scalar=W12[:, t, 0:1], in1=hE[b][:],
                                                                       op0=ALU.mult, op1=ALU.add), waits=[ge_.token()], inc=s_v)
                    q = fw.op('dve', lambda e: e.scalar_tensor_tensor(out=hE[b][:], in0=o2[b][:], scalar=W12[:, t, 1:2], in1=hE[b][:],
                                                                       op0=ALU.mult, op1=ALU.add), waits=[q], inc=s_v)
                    td = fw.dma('pool', out_rows_fn(t), hE[b][:], waits=[q])
                    e_free[b] = [td]
                for e_ in ('pe', 'act', 'dve', 'pool', 'sp'):
                    fw._waits(e_, [(s_v, s_v.n)] + fw.dma_all())

    WALL = [scratch(f"WALL{l}", [NE * 128, 12288], BF16) for l in range(2)]
    wc_state = dict(tok=None)

    wc_jobs = []

    def weight_convert_prepare():
        for l in range(2):
            for e_ in range(NE):
                wc_jobs.append((WALL[l][e_ * 128:(e_ + 1) * 128, 0:4096].rearrange("p (c f) -> p c f", c=8),
                                I["moe_w_gate"][l, e_].rearrange("(c p) f -> p c f", p=128)))
                wc_jobs.append((WALL[l][e_ * 128:(e_ + 1) * 128, 4096:8192].rearrange("p (c f) -> p c f", c=8),
                                I["moe_w_up"][l, e_].rearrange("(c p) f -> p c f", p=128)))
                wc_jobs.append((WALL[l][e_ * 128:(e_ + 1) * 128, 8192:12288].rearrange("p (c f) -> p c f", c=4),
                                I["moe_w_down"][l, e_].rearrange("(c p) f -> p c f", p=128)))

    def wc_issue(n):
        for _ in range(n):
            if not wc_jobs:
                break
            o_, i_ = wc_jobs.pop(0)
            fw.dma('pool', o_, i_)
        if not wc_jobs:
            wc_state['tok'] = [(s_, s_.n) for s_ in fw.rings['pool']]

    def weight_convert():
        weight_convert_prepare()
        wc_issue(10 ** 6)

    def moe0s():
        def rr(blk):
            slot = (blk * 512) // SPAN
            sp = cfg.QS[slot]
            off = (blk * 512) % SPAN
            return I["x"][sp * SPAN + off: sp * SPAN + off + 512, :]
        wc_issue(10 ** 6)
        moe_sparse(0, NQ, rr, AT0, "a_w_out", 8, lambda t: H1[t * 128:(t + 1) * 128, :], "s0", wc_state['tok'])

    def moe1s():
        def rr(blk):
            slot = (blk * 512) // SPAN
            qs = cfg.BS[slot][0]
            off = (blk * 512) % SPAN
            return H1[qs * SPAN + off: qs * SPAN + off + 512, :]
        moe_sparse(1, NB, rr, AT1, "b_w_out", 4, lambda t: Y[t * 128:(t + 1) * 128, :], "s1", wc_state['tok'])

    def moe0():
        return moe_phase(0, len(cfg.QS), lambda pz: I["x"][cfg.QS[pz] * SPAN:(cfg.QS[pz] + 1) * SPAN, :], AT0, "a_w_out", 8,
                         lambda pz: H1[pz * SPAN:(pz + 1) * SPAN, :], "m0")

    def moe1():
        return moe_phase(1, len(cfg.BS), lambda pz: H1[cfg.BS[pz][0] * SPAN:(cfg.BS[pz][0] + 1) * SPAN, :], AT1, "b_w_out", 4,
                         lambda pz: Y[pz * SPAN:(pz + 1) * SPAN, :], "m1")

    stages = {"wc": weight_convert, "wci": (lambda: None), "m0s": moe0s, "m1s": moe1s, "p1": phase1, "p2": phase2, "m0": moe0, "m1": moe1, "p5": phase5, "p5b": phase5b, "p6": phase6}
    last = None
    for sname in cfg.stages:
        last = stages[sname]()
    es.close()
    return nc


_SQUEEZE = ("a_norm", "a_w_in", "a_b_f", "a_q_gain", "a_k_gain", "a_w_out", "b_norm", "b_w_q", "b_q_gain", "b_w_out")
_NC_CACHE = {}


def _get_nc(key, cfg):
    if key not in _NC_CACHE:
        _NC_CACHE[key] = build(cfg)
    return _NC_CACHE[key]


def run_cfg(cfg, inputs, n_batch):
    nc = build(cfg)
    used = nc._used_inputs
    shared = {}
    for k, v in inputs.items():
        if k == "x":
            continue
        a = np.ascontiguousarray(np.asarray(v, dtype=np.float32))
        if k in _SQUEEZE:
            a = a.reshape(a.shape[1:])
        if k in used:
            shared[k] = a
    for k, v in make_consts().items():
        if k in used:
            shared[k] = v
    x = np.asarray(inputs["x"], dtype=np.float32)
    in_maps = []
    for b in range(n_batch):
        m = dict(shared)
        m["x"] = np.ascontiguousarray(x[b])
        in_maps.append(m)
    res = run_bass_kernel_spmd(nc, in_maps, core_ids=list(range(n_batch)))
    return res


def kernel(**inputs):
    x = np.asarray(inputs["x"], dtype=np.float32)
    B, S, _ = x.shape
    KS = S // SPAN
    half = KS // 2
    QS = tuple(range(half - 1, KS))
    BS = tuple((i, i - 1) for i in range(1, half + 1))
    cfg = Cfg(KS=KS, QS=QS, BS=BS, stages=("wci", "p1", "p2", "m0s", "p5", "p5b", "p6", "m1s"), masks=True, n_cores=2 * B)
    nc = build(cfg)
    used = nc._used_inputs
    shared = {}
    for k, v in inputs.items():
        if k == "x":
            continue
        a_ = np.ascontiguousarray(np.asarray(v, dtype=np.float32))
        if k in _SQUEEZE:
            a_ = a_.reshape(a_.shape[1:])
        if k in used:
            shared[k] = a_
    for k, v in make_consts().items():
        if k in used:
            shared[k] = v
    in_maps = []
    for b in range(B):
        for r in range(2):
            m = dict(shared)
            big = np.zeros(S, np.float32)
            kb = np.zeros(len(QS), np.float32)
            if r == 1:
                xs = x[b]
            else:
                xs = np.concatenate([x[b, 0:SPAN]] * half + [x[b, 0:half * SPAN]], axis=0)
                big[0:half * SPAN] = -NEGV
                kb[0] = NEGV
            m["x"] = np.ascontiguousarray(xs)
            m["c_big"] = big
            m["c_kbias"] = kb
            in_maps.append(m)
    res = run_bass_kernel_spmd(nc, in_maps, core_ids=list(range(2 * B)))
    out = np.empty((B, S, D), np.float32)
    for b in range(B):
        out[b, 0:half * SPAN] = np.asarray(res.results[2 * b]["y"])
        out[b, half * SPAN:] = np.asarray(res.results[2 * b + 1]["y"])
    return out
```

```python
import math
import os
from contextlib import ExitStack

import numpy as np
import ml_dtypes

import concourse.bass as bass
import concourse.mybir as mybir
from concourse.bass_utils import run_bass_kernel_spmd

F32 = mybir.dt.float32
BF16 = mybir.dt.bfloat16
AF = mybir.ActivationFunctionType
ALU = mybir.AluOpType
AX = mybir.AxisListType

D = 1024
SPAN = 2048
NH_A = 16
HD = 64
NE = 16
DE = 512
EPS = 1e-6
SCALE = HD ** -0.5
NEGV = -30000.0
B_DIL = (1, 4, 16)


class Cfg:
    def __init__(self, KS=4, QS=(0, 1, 2, 3), BS=((0, None), (1, 0), (2, 1), (3, 2)),
                 stages=("p1", "p2", "m0", "p5", "p5b", "p6", "m1"), expose=(), inject=(), n_cores=4, masks=False):
        self.KS = KS
        self.QS = tuple(QS)
        self.BS = tuple(BS)
        self.SK = KS * SPAN
        self.NQ = len(QS) * SPAN
        self.NB = len(BS) * SPAN
        self.stages = tuple(stages)
        self.expose = set(expose)
        self.inject = set(inject)
        self.n_cores = n_cores
        self.masks = masks


class Sem:
    def __init__(self, nc, es, name):
        self.h = es.enter_context(nc.semaphore(name))
        self.n = 0
        self.name = name
        self.kind = None

    def mark(self, kind):
        assert self.kind in (None, kind), (self.name, self.kind, kind)
        self.kind = kind


class FW:
    def __init__(self, nc, es):
        self.nc = nc
        self.es = es
        self.eng = dict(pe=nc.tensor, act=nc.scalar, dve=nc.vector, pool=nc.gpsimd, sp=nc.sync)
        self.seen = {k: {} for k in self.eng}
        self.nsem = 0
        self.rings = {}
        self.ring_i = {}

    def sem(self, name):
        self.nsem += 1
        return Sem(self.nc, self.es, f"{name}_{self.nsem}")

    def _waits(self, e, waits):
        for w in waits:
            if w is None:
                continue
            if isinstance(w, list):
                self._waits(e, w)
                continue
            s, v = w
            if v is None or v <= 0:
                continue
            if self.seen[e].get(s.name, 0) >= v:
                continue
            self.seen[e][s.name] = v
            self.eng[e].wait_ge(s.h, v)

    def op(self, e, fn, waits=(), inc=None):
        self._waits(e, waits)
        ins = fn(self.eng[e])
        if inc is not None:
            inc.mark('eng')
            inc.n += 1
            ins.then_inc(inc.h, 1)
            return (inc, inc.n)
        return None

    def _ring_next(self, e):
        if e not in self.rings:
            self.rings[e] = [self.sem(f"dq{e}{i}") for i in range(8)]
            self.ring_i[e] = 0
        s = self.rings[e][self.ring_i[e] % len(self.rings[e])]
        self.ring_i[e] += 1
        self._waits(e, [(s, s.n)])
        return s

    def group(self, e):
        return DmaGroup(self, e)

    def dma(self, e, out, in_, waits=(), inc=None):
        if isinstance(inc, DmaGroup):
            assert inc.e == e
            s = inc.sem
        else:
            s = self._ring_next(e)
        self._waits(e, waits)
        ins = self.eng[e].dma_start(out=out, in_=in_)
        s.n += 16
        ins.then_inc(s.h, 16)
        return (s, s.n)

    def dma_all(self):
        return [(s, s.n) for r in self.rings.values() for s in r]


class DmaGroup:
    def __init__(self, fw, e):
        self.e = e
        self.sem = fw._ring_next(e)

    def token(self):
        return (self.sem, self.sem.n)


def _t5_bucket_np(dist):
    dist = np.asarray(dist, np.int64)
    max_exact = 16
    d_f = np.maximum(dist, max_exact).astype(np.float32)
    large = max_exact + (np.log(d_f / np.float32(max_exact)) / np.float32(math.log(2048 / max_exact))
                         * np.float32(32 - max_exact)).astype(np.int32)
    large = np.minimum(large, 31)
    return np.where(dist < max_exact, dist, large)


def make_consts():
    c = {}
    c["c_ident"] = np.eye(128, dtype=np.float32)
    k = np.arange(128)[:, None]
    q = np.arange(128)[None, :]
    c["c_blk"] = ((k // 64) == (q // 64)).astype(np.float32)
    c["c_triu"] = np.where(k <= q, -1.0, 0.0).astype(np.float32)
    qq = np.arange(512)[None, None, :]
    jj = np.arange(4)[None, :, None]
    kk = np.arange(128)[:, None, None]
    c["c_negm"] = np.where(qq < 128 * jj + kk, NEGV, 0.0).astype(np.float32).reshape(128, 2048)
    c["c_anti"] = np.eye(128, dtype=np.float32)[::-1].copy()
    oh = np.zeros((3, 32, 384), np.float32)
    ng = np.zeros((3, 1, 384), np.float32)
    for g, d in enumerate(B_DIL):
        for m in range(384):
            n = m - 127
            if 0 <= n <= 128:
                oh[g, int(_t5_bucket_np(n * d)), m] = 1.0
            else:
                ng[g, 0, m] = NEGV
    c["c_oh"] = oh
    c["c_stri"] = (k < q).astype(np.float32)
    ee = np.arange(16)
    c["c_lt"] = (ee[None, :] < ee[:, None]).astype(np.float32).reshape(256)
    c["c_ng"] = ng
    return c


def build(cfg):
    nc = bass.Bass("TRN2", target_bir_lowering=False)
    es = ExitStack()
    fw = FW(nc, es)
    SK, NQ, NB = cfg.SK, cfg.NQ, cfg.NB

    def ext_in(name, shape, dtype=F32):
        return nc.dram_tensor(name, list(shape), dtype, kind="ExternalInput").ap()

    def scratch(name, shape, dtype):
        if name in cfg.inject:
            kind = "ExternalInput"
        elif name in cfg.expose:
            kind = "ExternalOutput"
        else:
            kind = "Internal"
        return nc.dram_tensor(name, list(shape), dtype, kind=kind).ap()

    SHAPES = dict([("x", [SK, D]), ("a_norm", [D]), ("a_w_in", [D, 3088]), ("a_b_f", [16]), ("a_q_gain", [64]), ("a_k_gain", [64]),
                   ("a_w_out", [D, D]), ("kv_norm", [D]), ("kv_w", [D, 3072]), ("kv_k_gain", [64]),
                   ("rel_bias", [32, 24]), ("b_norm", [D]), ("b_w_q", [D, 1536]), ("b_q_gain", [64]),
                   ("b_w_out", [512, D]), ("ffn_norm", [2, D]), ("moe_w_group", [2, D, 4]), ("moe_b_group", [2, 4]),
                   ("moe_w_expert", [2, D, 16]), ("moe_b_expert", [2, 16]), ("moe_w_gate", [2, NE, D, DE]),
                   ("moe_w_up", [2, NE, D, DE]), ("moe_w_down", [2, NE, DE, D]),
                   ("c_ident", [128, 128]), ("c_blk", [128, 128]), ("c_triu", [128, 128]), ("c_negm", [128, 2048]),
                   ("c_anti", [128, 128]), ("c_oh", [3, 32, 384]), ("c_ng", [3, 1, 384]), ("c_stri", [128, 128]), ("c_lt", [256]), ("c_big", [SK]), ("c_kbias", [len(cfg.QS)])])

    class LazyIn(dict):
        def __missing__(self, k):
            v = ext_in(k, SHAPES[k])
            self[k] = v
            return v
    I = LazyIn()
    nc._used_inputs = I

    QT = scratch("QT", [NH_A, 70, NQ], BF16)
    KT = scratch("KT", [NH_A, 70, SK], BF16)
    VP = scratch("VP", [NH_A, 128, SK // 128, 65], BF16)
    AT0 = scratch("AT0", [D, NQ], BF16)
    H1 = scratch("H1", [NQ, D], F32)
    KB = scratch("KB", [1536, NQ], BF16)
    VB = scratch("VB", [3, NQ, 8 * 65], BF16)
    QB = scratch("QB", [1536, NB], BF16)
    FV = scratch("FV", [24, 384], F32)
    AT1 = scratch("AT1", [512, NB], BF16)
    Y = nc.dram_tensor("y", [NB, D], F32, kind="ExternalOutput").ap()

    s_fin = fw.sem("fin")
    fin_waits = []

    cst = es.enter_context(nc.sbuf_tensor("cst_ident_bf", [128, 128], BF16))
    ident_bf = cst
    ident_f = es.enter_context(nc.sbuf_tensor("cst_ident_f", [128, 128], F32))
    blk_bf = es.enter_context(nc.sbuf_tensor("cst_blk", [128, 128], BF16))
    triu_f = es.enter_context(nc.sbuf_tensor("cst_triu", [128, 128], F32))
    negm_bf = es.enter_context(nc.sbuf_tensor("cst_negm", [128, 2048], BF16))
    ones_f = es.enter_context(nc.sbuf_tensor("cst_ones", [128, 128], F32))
    s_c = fw.group('sp')
    fw.dma('sp', ident_f[:], I["c_ident"][:, :], inc=s_c)
    fw.dma('sp', triu_f[:], I["c_triu"][:, :], inc=s_c)
    s_cp = fw.group('pool')
    fw.dma('pool', ident_bf[:], I["c_ident"][:, :], inc=s_cp)
    fw.dma('pool', blk_bf[:], I["c_blk"][:, :], inc=s_cp)
    fw.dma('pool', negm_bf[:], I["c_negm"][:, :], inc=s_cp)
    s_c1 = fw.sem("cst1")
    w_ones = fw.op('pool', lambda e: e.memset(ones_f[:], 1.0), inc=s_c1)
    W_CST = [s_c.token(), s_cp.token(), w_ones]

    def rms_stats(xs_ap, junk_ap, st_tile, col, n, wait_in, s_act):
        t1 = fw.op('act', lambda e: e.activation(out=junk_ap, in_=xs_ap, func=AF.Square,
                                                  accum_out=st_tile[:, col:col + 1]), waits=wait_in, inc=s_act)
        t2 = fw.op('act', lambda e: e.activation(out=st_tile[:, col + 1:col + 2], in_=st_tile[:, col:col + 1],
                                                  func=AF.Ln, scale=1.0 / n, bias=EPS), waits=[t1], inc=s_act)
        t3 = fw.op('act', lambda e: e.activation(out=st_tile[:, col + 2:col + 3], in_=st_tile[:, col + 1:col + 2],
                                                  func=AF.Exp, scale=-0.5), waits=[t2], inc=s_act)
        return t3

    def phase1():
        NT = SK // 128
        NBLK = SK // 512
        def qcol_of_block(tb):
            span = (tb * 512) // SPAN
            if span in cfg.QS:
                return cfg.QS.index(span) * SPAN + (tb * 512) % SPAN
            return None
        with ExitStack() as ps:
            sb = lambda name, shape, dt: ps.enter_context(nc.sbuf_tensor("p1_" + name, shape, dt))
            pm = lambda name, shape, dt: ps.enter_context(nc.psum_tensor("p1P_" + name, shape, dt))
            w_in = sb("w_in", [128, 8, 3088], BF16)
            g_bc = sb("g_bc", [128, D], F32)
            bf_bc = sb("bf_bc", [128, 16], F32)
            gq = sb("gq", [128, 2], F32)
            xs = [sb(f"xs{i}", [128, D], F32) for i in range(2)]
            junk = sb("junk", [128, D], BF16)
            xn = [sb(f"xn{i}", [128, D], BF16) for i in range(2)]
            xnT = [sb(f"xnT{i}", [128, 8, 512], BF16) for i in range(2)]
            st = sb("st", [128, 3 * NT], F32)
            sqk = [sb(f"sqk{i}", [128, 512], BF16) for i in range(2)]
            lnv = [sb(f"lnv{i}", [128, 512], F32) for i in range(2)]
            rstd = [sb(f"rstd{i}", [128, 512], F32) for i in range(2)]
            kst = [sb(f"kst{i}", [128, 512], BF16) for i in range(3)]
            vst = [sb(f"vst{i}", [128, 16, 4, 65], BF16) for i in range(2)]
            fl = [sb(f"fl{i}", [128, 16], F32) for i in range(2)]
            spv = [sb(f"spv{i}", [128, 16], F32) for i in range(2)]
            cum = sb("cum", [16, SK], F32)
            psA = [pm(f"psA{i}", [128, 512], F32) for i in range(3)]
            ps2 = [pm(f"ps2{i}", [128, 512], F32) for i in range(2)]
            pt = [pm("pt0", [128, 8, 128], BF16)] * 2
            psf = pm("psf", [128, 512], F32)
            pscum = pm("pscum", [128, 512], F32)

            s_w = fw.group('sp'); s_x = None; s_a = fw.sem("p1a"); s_v = fw.sem("p1v")
            s_p = fw.sem("p1p"); s_g = fw.sem("p1g"); s_st = None

            s_wp = fw.group('pool')
            for c in range(8):
                fw.dma('pool', w_in[:, c, :], I["a_w_in"][c * 128:(c + 1) * 128, :], inc=s_wp)
            fw.dma('sp', g_bc[:], I["a_norm"].partition_broadcast(128), inc=s_w)
            fw.dma('sp', bf_bc[:], I["a_b_f"].partition_broadcast(128), inc=s_w)
            for hh in range(2):
                fw.dma('sp', gq[hh * 64:(hh + 1) * 64, 0:1], I["a_q_gain"].rearrange("(d o) -> d o", o=1), inc=s_w)
                fw.dma('sp', gq[hh * 64:(hh + 1) * 64, 1:2], I["a_k_gain"].rearrange("(d o) -> d o", o=1), inc=s_w)
            W_W = [s_w.token(), s_wp.token()]
            t = fw.op('pool', lambda e: e.memset(st[:], 0.0), inc=s_g)
            for i in range(2):
                t = fw.op('pool', lambda e: e.memset(vst[i][:], 1.0), inc=s_g)
            W_INIT = t
            t_gq = fw.op('dve', lambda e: e.tensor_scalar(out=gq[:, 0:1], in0=gq[:, 0:1], scalar1=SCALE, scalar2=None,
                                                            op0=ALU.mult), waits=[W_W], inc=s_v)

            xs_free = [[], []]
            xn_free = [[], []]
            pt_free_l = [[]]
            xnT_free = [[], []]
            psA_free = [[], [], []]
            ps2_free = [[], []]
            sqk_free = [[], []]
            lnv_free = [[], []]
            rstd_free = [[], []]
            kst_free = [[], [], []]
            vst_free = [[], []]
            fl_free = [[], []]
            spv_free = [[], []]
            psf_free = []
            pscum_free = []
            cnt = dict(a=0, k=0, j=0)
            last_cum = [None]

            def emit_tile_front(tb, ti):
                tg = tb * 4 + ti
                b = tg % 2
                bb = tb % 2
                tx = fw.dma('sp', xs[b][:], I["x"][tg * 128:(tg + 1) * 128, :], waits=xs_free[b], inc=s_x)
                t3 = rms_stats(xs[b][:], junk[:], st, 3 * tg, D, [tx, W_INIT], s_a)
                tn = fw.op('dve', lambda e: e.scalar_tensor_tensor(out=xn[b][:], in0=xs[b][:], scalar=st[:, 3 * tg + 2:3 * tg + 3],
                                                                    in1=g_bc[:], op0=ALU.mult, op1=ALU.mult),
                           waits=[t3, W_W] + xn_free[b], inc=s_v)
                xs_free[b] = [tn]
                tt = None
                for c in range(8):
                    tt = fw.op('pe', lambda e: e.transpose(pt[b][:, c, :], xn[b][:, c * 128:(c + 1) * 128], ident_bf[:]),
                               waits=[tn] + W_CST + pt_free_l[0], inc=(s_p if c == 7 else None))
                xn_free[b] = [tt]
                te = fw.op('dve', lambda e: e.tensor_copy(xnT[bb][:, :, ti * 128:(ti + 1) * 128], pt[b][:]),
                           waits=[tt] + xnT_free[bb], inc=s_v)
                pt_free_l[0] = [te]
                return te

            def emit_block(tb):
                bb = tb % 2
                tes = [emit_tile_front(tb, ti) for ti in range(4)]
                t_xnT = tes[-1]
                qcol = qcol_of_block(tb)
                jobs = []
                for kc in range(8):
                    jobs.append(("k", kc))
                if qcol is not None:
                    for qc in range(8):
                        jobs.append(("q", qc))
                for ti in range(4):
                    for half in range(2):
                        jobs.append(("v", ti, half))
                for ti in range(4):
                    jobs.append(("f", ti))
                vb = tb % 2
                pend = []
                last_pe = [None]

                def stage1(job):
                    kind = job[0]
                    if kind in ("k", "q"):
                        ch = job[1]
                        a = cnt['a'] % 3; cnt['a'] += 1
                        col0 = (1024 if kind == "k" else 0) + ch * 128
                        tm = None
                        for c in range(8):
                            tm = fw.op('pe', lambda e: e.matmul(psA[a][:], w_in[:, c, col0:col0 + 128], xnT[bb][:, c, :],
                                                                start=(c == 0), stop=(c == 7)),
                                       waits=[t_xnT, W_W] + psA_free[a], inc=(s_p if c == 7 else None))
                        last_pe[0] = tm
                        k2 = cnt['k'] % 2; cnt['k'] += 1
                        tsq = fw.op('act', lambda e: e.activation(out=sqk[k2][:], in_=psA[a][:], func=AF.Square),
                                    waits=[tm] + sqk_free[k2], inc=s_a)
                        return ("norm", kind, ch, a, k2, tm, tsq)
                    if kind == "v":
                        ti, half = job[1], job[2]
                        a = cnt['a'] % 3; cnt['a'] += 1
                        tm = None
                        for c in range(8):
                            tm = fw.op('pe', lambda e: e.matmul(psA[a][:], xnT[bb][:, c, ti * 128:(ti + 1) * 128],
                                                                w_in[:, c, 2048 + half * 512:2048 + (half + 1) * 512],
                                                                start=(c == 0), stop=(c == 7)),
                                       waits=[t_xnT, W_W] + psA_free[a], inc=(s_p if c == 7 else None))
                        last_pe[0] = tm
                        tev = fw.op('act', lambda e: e.copy(vst[vb][:, half * 8:(half + 1) * 8, ti, 0:64],
                                                            psA[a][:].rearrange("p (h d) -> p h d", d=64)),
                                    waits=[tm, W_INIT] + vst_free[vb], inc=s_a)
                        psA_free[a] = [tev]
                        return ("vdone", tev)
                    if kind == "f":
                        ti = job[1]
                        tg = tb * 4 + ti
                        f2 = tg % 2
                        tm = None
                        for c in range(8):
                            tm = fw.op('pe', lambda e: e.matmul(psf[:, 0:16], xnT[bb][:, c, ti * 128:(ti + 1) * 128],
                                                                w_in[:, c, 3072:3088], start=(c == 0), stop=(c == 7)),
                                       waits=[t_xnT, W_W] + psf_free, inc=(s_p if c == 7 else None))
                        last_pe[0] = tm
                        t1 = fw.op('dve', lambda e: e.tensor_tensor(out=fl[f2][:], in0=psf[:, 0:16], in1=bf_bc[:], op=ALU.add),
                                   waits=[tm] + fl_free[f2], inc=s_v)
                        psf_free[:] = [t1]
                        t2 = fw.op('act', lambda e: e.activation(out=fl[f2][:], in_=fl[f2][:], func=AF.Exp, scale=-1.0),
                                   waits=[t1], inc=s_a)
                        t3 = fw.op('act', lambda e: e.activation(out=spv[f2][:], in_=fl[f2][:], func=AF.Ln, bias=1.0, scale=1.0),
                                   waits=[t2] + spv_free[f2], inc=s_a)
                        fl_free[f2] = [t3]
                        return ("cum", ti, tg, f2, t3)
                    raise ValueError

                def stage2(rec):
                    if rec[0] == "norm":
                        _, kind, ch, a, k2, tm, tsq = rec
                        j = cnt['j'] % 2; cnt['j'] += 1
                        t2 = fw.op('pe', lambda e: e.matmul(ps2[j][:], blk_bf[:], sqk[k2][:], start=True, stop=True),
                                   waits=[tsq] + W_CST + ps2_free[j], inc=s_p)
                        sqk_free[k2] = [t2]
                        tl = fw.op('act', lambda e: e.activation(out=lnv[j][:], in_=ps2[j][:], func=AF.Ln, scale=1.0 / 64, bias=EPS),
                                   waits=[t2] + lnv_free[j], inc=s_a)
                        ps2_free[j] = [tl]
                        tr = fw.op('act', lambda e: e.activation(out=rstd[j][:], in_=lnv[j][:], func=AF.Exp, scale=-0.5),
                                   waits=[tl] + rstd_free[j], inc=s_a)
                        lnv_free[j] = [tr]
                        ks = cnt.setdefault('ks', 0) % 3; cnt['ks'] = cnt.get('ks', 0) + 1
                        gcol = 1 if kind == "k" else 0
                        tk = fw.op('dve', lambda e: e.scalar_tensor_tensor(out=kst[ks][:], in0=psA[a][:], scalar=gq[:, gcol:gcol + 1],
                                                                            in1=rstd[j][:], op0=ALU.mult, op1=ALU.mult),
                                   waits=[tr, t_gq, tm] + kst_free[ks], inc=s_v)
                        psA_free[a] = [tk]
                        rstd_free[j] = [tk]
                        tdl = []
                        for hh in range(2):
                            h = 2 * ch + hh
                            if kind == "k":
                                dst = KT[h, 0:64, tb * 512:(tb + 1) * 512]
                            else:
                                dst = QT[h, 0:64, qcol:qcol + 512]
                            tdl.append(fw.dma('pool', dst, kst[ks][hh * 64:(hh + 1) * 64, :], waits=[tk], inc=s_st))
                        kst_free[ks] = tdl
                    elif rec[0] == "cum":
                        _, ti, tg, f2, t3 = rec
                        tc = fw.op('pe', lambda e: e.matmul(pscum[0:16, 0:128], spv[f2][:], triu_f[:], start=True, stop=True),
                                   waits=[t3] + W_CST + pscum_free, inc=s_p)
                        spv_free[f2] = [tc]
                        if tg == 0:
                            td = fw.op('dve', lambda e: e.tensor_copy(cum[0:16, 0:128], pscum[0:16, 0:128]), waits=[tc], inc=s_v)
                        else:
                            td = fw.op('dve', lambda e: e.tensor_scalar(out=cum[0:16, tg * 128:(tg + 1) * 128], in0=pscum[0:16, 0:128],
                                                                         scalar1=cum[0:16, tg * 128 - 1:tg * 128], scalar2=None, op0=ALU.add),
                                       waits=[tc, last_cum[0]], inc=s_v)
                        last_cum[0] = td
                        pscum_free[:] = [td]

                prev = None
                for job in jobs:
                    rec = stage1(job)
                    if prev is not None:
                        stage2(prev)
                    prev = rec if rec[0] in ("norm", "cum") else None
                    if rec[0] == "vdone":
                        pass
                if prev is not None:
                    stage2(prev)
                tv = (s_a, s_a.n)
                td = fw.dma('pool', VP.rearrange("h p t c -> p h t c")[:, :, tb * 4:(tb + 1) * 4, :], vst[vb][:], waits=[tv], inc=s_st)
                vst_free[vb] = [td]
                xnT_free[bb] = [(s_p, s_p.n)]

            if "wci" in cfg.stages:
                weight_convert_prepare()
            for tb in range(NBLK):
                emit_block(tb)

            t_cum = last_cum[0]
            with ExitStack() as ps2c:
                sb2 = lambda name, shape, dt: ps2c.enter_context(nc.sbuf_tensor("p1c_" + name, shape, dt))
                cq = [sb2("cq0", [16, 6, 1024], BF16)]
                ck = [sb2("ck0", [16, 6, 1024], BF16)]
                r1 = sb2("r1", [16, 1024], F32)
                r2 = sb2("r2", [16, 1024], F32)
                csum = sb2("csum", [16, 1024], F32)
                bigb = [sb2("bigb0", [16, 1024], F32)]
                csum_free = []
                tinit = None
                for i in range(1):
                    fw.op('pool', lambda e: e.memset(cq[i][:], 1.0), inc=s_g)
                    tinit = fw.op('pool', lambda e: e.memset(ck[i][:], 1.0), inc=s_g)
                c_free = [[], []]
                for cc in range(SK // 1024):
                    b = 0
                    src = cum[0:16, cc * 1024:(cc + 1) * 1024]
                    w0 = [t_cum, tinit] + c_free[b]
                    if cfg.masks:
                        tbb = fw.dma('sp', bigb[b][:], I["c_big"][cc * 1024:(cc + 1) * 1024].partition_broadcast(16), waits=c_free[b])
                        tsum = fw.op('dve', lambda e: e.tensor_tensor(out=csum[:], in0=src, in1=bigb[b][:], op=ALU.add), waits=[t_cum, tbb] + csum_free, inc=s_v)
                        src = csum[:]
                        w0 = [tsum, tinit] + c_free[b]
                    t = fw.op('dve', lambda e: e.tensor_copy(cq[b][:, 0, :], src), waits=w0, inc=s_v)
                    t = fw.op('dve', lambda e: e.tensor_tensor(out=r1[:], in0=src, in1=cq[b][:, 0, :], op=ALU.subtract), waits=[t], inc=s_v)
                    t = fw.op('dve', lambda e: e.tensor_copy(cq[b][:, 1, :], r1[:]), waits=[t], inc=s_v)
                    t = fw.op('dve', lambda e: e.tensor_tensor(out=r2[:], in0=r1[:], in1=cq[b][:, 1, :], op=ALU.subtract), waits=[t], inc=s_v)
                    t = fw.op('dve', lambda e: e.tensor_copy(cq[b][:, 2, :], r2[:]), waits=[t], inc=s_v)
                    t = fw.op('dve', lambda e: e.tensor_scalar(out=ck[b][:, 3:6, :], in0=cq[b][:, 0:3, :], scalar1=-1.0, scalar2=None,
                                                                op0=ALU.mult), waits=[t], inc=s_v)
                    csum_free = [t]
                    tdl = [fw.dma('pool', KT[:, 64:70, cc * 1024:(cc + 1) * 1024], ck[b][:], waits=[t], inc=s_st)]
                    span = (cc * 1024) // SPAN
                    if span in cfg.QS:
                        qc0 = cfg.QS.index(span) * SPAN + (cc * 1024) % SPAN
                        tdl.append(fw.dma('pool', QT[:, 64:70, qc0:qc0 + 1024], cq[b][:], waits=[t], inc=s_st))
                    c_free[b] = tdl
                done = fw.dma_all()
                for e in ('pe', 'act', 'dve', 'pool', 'sp'):
                    fw._waits(e, [done, (s_p, s_p.n), (s_a, s_a.n), (s_v, s_v.n), (s_g, s_g.n)])
            return done

    def phase2():
        with ExitStack() as ps:
            sb = lambda name, shape, dt: ps.enter_context(nc.sbuf_tensor("p2_" + name, shape, dt))
            pm = lambda name, shape, dt: ps.enter_context(nc.psum_tensor("p2P_" + name, shape, dt))
            kT = [sb(f"kT{i}", [70, SK], BF16) for i in range(2)]
            qT = [sb(f"qT{i}", [70, NQ], BF16) for i in range(2)]
            vS = [sb(f"vS{i}", [128, SK // 128, 65], BF16) for i in range(2)]
            NPB = 4
            pT = [sb(f"pT{i}", [128, 512], BF16) for i in range(NPB)]
            osb = [sb(f"osb{i}", [65, 512], F32) for i in range(2)]
            rc = [sb(f"rc{i}", [65, 512], F32) for i in range(2)]
            ost = [sb(f"ost{i}", [64, 512], BF16) for i in range(2)]
            ps_s = [pm(f"s{i}", [128, 512], F32) for i in range(NPB)]
            ps_o = [pm(f"o{i}", [65, 512], F32) for i in range(2)]
            ps_bc = pm("bc", [64, 512], F32)

            s_ld = None; s_S = fw.sem("p2S"); s_E = fw.sem("p2E"); s_PV = fw.sem("p2PV")
            s_oc = fw.sem("p2oc"); s_v = fw.sem("p2v"); s_bc = fw.sem("p2bc"); s_st = None

            qblocks = []
            for j, dspan in enumerate(cfg.QS):
                for qb in range(4):
                    nt = dspan * 16 + 4 * qb + 4
                    qblocks.append((j * SPAN + qb * 512, nt))

            head_free = [[], []]
            n = 0
            nqb = 0
            ld_tok = {}
            osb_free = [[], []]
            rc_free = [[], []]
            ost_free = [[], []]
            psbc_free = []
            pso_free = [[], []]

            def load_head(h):
                b = h % 2
                w = head_free[b]
                gl = fw.group('sp')
                fw.dma('sp', kT[b][:], KT[h, :, :], waits=w, inc=gl)
                fw.dma('sp', qT[b][:], QT[h, :, :], waits=w, inc=gl)
                t = fw.dma('sp', vS[b][:], VP[h, :, :, :], waits=w, inc=gl)
                ld_tok[h] = t

            load_head(0)
            for h in range(NH_A):
                b = h % 2
                if h + 1 < NH_A:
                    load_head(h + 1)
                if "wci" in cfg.stages:
                    wc_issue(6)
                W_LD = [ld_tok[h]] + W_CST
                pairs = []
                for qi, (qc0, nt) in enumerate(qblocks):
                    for kt in range(nt):
                        jj = kt - (nt - 4) if kt >= nt - 4 else None
                        pairs.append((qi, qc0, kt, jj, kt == 0, kt == nt - 1))
                NP_ = len(pairs)
                deferred = {}

                def S_job(idx):
                    nonlocal n
                    qi, qc0, kt, jj, first, last = pairs[idx]
                    g = n + idx
                    sbuf = g % NPB
                    wfree = [(s_E, g - NPB + 1)] if g - NPB + 1 > 0 else []
                    if jj is not None:
                        c0 = 128 * jj if not first else 0
                        fw.op('pe', lambda e: e.matmul(ps_s[sbuf][:, c0:512], ident_bf[:], negm_bf[:, jj * 512 + c0:(jj + 1) * 512],
                                                       start=True, stop=False), waits=W_LD + wfree)
                        fw.op('pe', lambda e: e.matmul(ps_s[sbuf][:, c0:512], kT[b][:, kt * 128:(kt + 1) * 128], qT[b][:, qc0 + c0:qc0 + 512],
                                                       start=False, stop=True), inc=s_S)
                    else:
                        fw.op('pe', lambda e: e.matmul(ps_s[sbuf][:], kT[b][:, kt * 128:(kt + 1) * 128], qT[b][:, qc0:qc0 + 512],
                                                       start=True, stop=True), waits=W_LD + wfree, inc=s_S)

                def E_job(idx):
                    g = n + idx
                    sbuf = g % NPB
                    jj_ = pairs[idx][3]
                    c0 = 128 * jj_ if (jj_ is not None and not pairs[idx][4]) else 0
                    wfree = [(s_PV, g - NPB + 1)] if g - NPB + 1 > 0 else []
                    fw.op('act', lambda e: e.activation(out=pT[sbuf][:, c0:512], in_=ps_s[sbuf][:, c0:512], func=AF.Exp),
                          waits=[(s_S, g + 1)] + wfree, inc=s_E)

                def PV_job(idx):
                    qi, qc0, kt, jj, first, last = pairs[idx]
                    g = n + idx
                    sbuf = g % NPB
                    ob = (nqb + qi) % 2
                    w = [(s_E, g + 1)]
                    if first:
                        w = w + pso_free[ob]
                    c0 = 128 * jj if (jj is not None and not first) else 0
                    fw.op('pe', lambda e: e.matmul(ps_o[ob][:, c0:512], vS[b][:, kt, :], pT[sbuf][:, c0:512], start=first, stop=last),
                          waits=w, inc=s_PV)

                def epi_act(qi, idx_last):
                    ob = (nqb + qi) % 2
                    g = n + idx_last
                    t = fw.op('dve', lambda e: e.tensor_copy(osb[ob][:], ps_o[ob][:]), waits=[(s_PV, g + 1)] + osb_free[ob], inc=s_v)
                    pso_free[ob] = [t]
                    t2 = fw.op('dve', lambda e: e.reciprocal(rc[ob][64:65, :], osb[ob][64:65, :]), waits=[t] + rc_free[ob], inc=s_v)
                    return t2

                def epi_pe(qi, t2):
                    ob = (nqb + qi) % 2
                    qc0 = qblocks[qi][0]
                    t3 = fw.op('pe', lambda e: e.matmul(ps_bc[:], ones_f[64:65, 0:64], rc[ob][64:65, :], start=True, stop=True),
                               waits=[t2] + W_CST + psbc_free, inc=s_bc)
                    rc_free[ob] = [t3]
                    t4 = fw.op('dve', lambda e: e.tensor_tensor(out=ost[ob][:], in0=osb[ob][0:64, :], in1=ps_bc[:], op=ALU.mult),
                               waits=[t3] + ost_free[ob], inc=s_v)
                    psbc_free[:] = [t4]
                    osb_free[ob] = [t4]
                    t5 = fw.dma('pool', AT0[h * 64:(h + 1) * 64, qc0:qc0 + 512], ost[ob][:], waits=[t4], inc=s_st)
                    ost_free[ob] = [t5]

                epi_state = {}
                for step in range(NP_ + 8):
                    if step < NP_:
                        S_job(step)
                    if step - 1 >= 0 and step - 1 < NP_:
                        E_job(step - 1)
                    if step - 2 >= 0 and step - 2 < NP_:
                        PV_job(step - 2)
                        qi, qc0, kt, jj, first, last = pairs[step - 2]
                        if last:
                            deferred.setdefault(step + 2, []).append(("act", qi, step - 2))
                    for item in deferred.pop(step, []):
                        if item[0] == "act":
                            t2 = epi_act(item[1], item[2])
                            deferred.setdefault(step + 3, []).append(("pe", item[1], t2))
                        else:
                            epi_pe(item[1], item[2])
                assert not deferred
                n += NP_
                nqb += len(qblocks)
                head_free[b] = [(s_PV, s_PV.n), (s_S, s_S.n)]
            done = fw.dma_all()
            for e in ('pe', 'act', 'dve', 'pool', 'sp'):
                fw._waits(e, [done, (s_PV, s_PV.n), (s_E, s_E.n), (s_v, s_v.n), (s_bc, s_bc.n), (s_oc, s_oc.n), (s_S, s_S.n)])
            return done


    def moe_phase(layer, n_pass, resid_fn, AT, w_out_name, nch, out_fn, tag):
        with ExitStack() as ps:
            sb = lambda name, shape, dt: ps.enter_context(nc.sbuf_tensor(f"{tag}_" + name, shape, dt))
            acc = sb("acc", [128, 16, D], F32)
            xnT = sb("xnT", [128, 8, SPAN], BF16)
            gates = sb("gates", [128, 16, NE], F32)
            s_ld = None; s_a = fw.sem(tag + "a"); s_v = fw.sem(tag + "v"); s_p = fw.sem(tag + "p")
            s_g = fw.sem(tag + "g"); s_st = None
            pass_free = []
            for pz in range(n_pass):
                resid = resid_fn(pz)
                outp = out_fn(pz)
                with ExitStack() as pa:
                    sba = lambda name, shape, dt: pa.enter_context(nc.sbuf_tensor(f"{tag}a{pz}_" + name, shape, dt))
                    pma = lambda name, shape, dt: pa.enter_context(nc.psum_tensor(f"{tag}aP{pz}_" + name, shape, dt))
                    w_out = sba("w_out", [128, nch, D], BF16)
                    at_sb = [sba(f"at{i}", [128, nch, 512], BF16) for i in range(2)]
                    g_bc = sba("g_bc", [128, D], F32)
                    xn = [sba(f"xn{i}", [128, D], F32) for i in range(2)]
                    junk = sba("junk", [128, D], BF16)
                    xnTf = [sba(f"xnTf{i}", [128, 8, 128], F32) for i in range(2)]
                    wr = sba("wr", [128, 8, 20], F32)
                    rb = sba("rb", [128, 20], F32)
                    st = sba("st", [128, 3 * 16], F32)
                    rt = [sba(f"rt{i}", [128, 96], F32) for i in range(2)]
                    ps_op = [pma(f"op{i}", [128, 512], F32) for i in range(2)]
                    ptfa = pma("ptfa", [128, 4, 128], F32)
                    ptfb = pma("ptfb", [128, 4, 128], F32)
                    ps_r = pma("r", [128, 512], F32)

                    s_w = fw.group('sp'); s_wp = fw.group('pool')
                    for c in range(nch):
                        fw.dma('pool', w_out[:, c, :], I[w_out_name][c * 128:(c + 1) * 128, :], waits=pass_free, inc=s_wp)
                    fw.dma('sp', g_bc[:], I["ffn_norm"][layer, :].partition_broadcast(128), waits=pass_free, inc=s_w)
                    fw.dma('sp', wr[:, :, 0:4], I["moe_w_group"][layer].rearrange("(c p) g -> p c g", p=128), inc=s_w)
                    fw.dma('sp', wr[:, :, 4:20], I["moe_w_expert"][layer].rearrange("(c p) g -> p c g", p=128), inc=s_w)
                    fw.dma('sp', rb[:, 0:4], I["moe_b_group"][layer, :].partition_broadcast(128), inc=s_w)
                    fw.dma('sp', rb[:, 4:20], I["moe_b_expert"][layer, :].partition_broadcast(128), inc=s_w)
                    W_W = [s_w.token(), s_wp.token()]
                    W_INIT = fw.op('pool', lambda e: e.memset(st[:], 0.0), waits=pass_free, inc=s_g)
                    at_free = [[], []]
                    op_free = [[], []]
                    xn_free = [[], []]
                    ptf_free = []
                    xnTf_free = [[], []]
                    psr_free = []
                    rt_free = [[], []]
                    last_gate = None
                    for blk in range(4):
                        ab = blk % 2
                        t_at = fw.dma('sp', at_sb[ab][:], AT[:, pz * SPAN + blk * 512: pz * SPAN + (blk + 1) * 512]
                                      .rearrange("(c p) n -> p c n", p=128), waits=at_free[ab] + pass_free, inc=s_ld)
                        t_rs = fw.dma('sp', acc[:, blk * 4:(blk + 1) * 4, :],
                                      resid[blk * 512:(blk + 1) * 512, :].rearrange("(t p) d -> p t d", p=128),
                                      waits=pass_free, inc=s_ld)
                        t_last_op = None
                        for ti in range(4):
                            t = blk * 4 + ti
                            xb = t % 2
                            tadd = None
                            for half in range(2):
                                tm = None
                                for c in range(nch):
                                    tm = fw.op('pe', lambda e: e.matmul(ps_op[half][:], at_sb[ab][:, c, ti * 128:(ti + 1) * 128],
                                                                        w_out[:, c, half * 512:(half + 1) * 512],
                                                                        start=(c == 0), stop=(c == nch - 1)),
                                               waits=[t_at, W_W] + op_free[half], inc=(s_p if c == nch - 1 else None))
                                t_last_op = tm
                                tadd = fw.op('dve', lambda e: e.tensor_tensor(out=acc[:, t, half * 512:(half + 1) * 512],
                                                                               in0=acc[:, t, half * 512:(half + 1) * 512],
                                                                               in1=ps_op[half][:], op=ALU.add),
                                             waits=[tm, t_rs], inc=s_v)
                                op_free[half] = [tadd]
                            KSTOP = int(os.environ.get('KSTOP', 9))
                            if KSTOP <= 1:
                                continue
                            t3 = rms_stats(acc[:, t, :], junk[:], st, 3 * t, D, [tadd, W_INIT], s_a)
                            tn = fw.op('dve', lambda e: e.scalar_tensor_tensor(out=xn[xb][:], in0=acc[:, t, :], scalar=st[:, 3 * t + 2:3 * t + 3],
                                                                                in1=g_bc[:], op0=ALU.mult, op1=ALU.mult),
                                       waits=[t3, W_W] + xn_free[xb], inc=s_v)
                            if KSTOP <= 2:
                                continue
                            tt = None
                            for c in range(8):
                                pdst = (ptfa if c < 4 else ptfb)[:, c % 4, :]
                                tt = fw.op('pe', lambda e: e.matmul(pdst, xn[xb][:, c * 128:(c + 1) * 128], ident_f[:], start=True, stop=True),
                                           waits=[tn] + W_CST + ptf_free, inc=(s_p if c == 7 else None))
                            xn_free[xb] = [tt]
                            fw.op('dve', lambda e: e.tensor_copy(xnTf[xb][:, 0:4, :], ptfa[:]), waits=[tt] + xnTf_free[xb], inc=s_v)
                            te2 = fw.op('dve', lambda e: e.tensor_copy(xnTf[xb][:, 4:8, :], ptfb[:]), inc=s_v)
                            te1 = fw.op('pool', lambda e: e.tensor_copy(xnT[:, :, t * 128:(t + 1) * 128], xnTf[xb][:]), waits=[te2] + pass_free, inc=s_g)
                            ptf_free = [te2]
                            if KSTOP <= 3:
                                continue
                            tr = None
                            for c in range(8):
                                tr = fw.op('pe', lambda e: e.matmul(ps_r[:, 0:20], xnTf[xb][:, c, :], wr[:, c, :], start=(c == 0), stop=(c == 7)),
                                           waits=[te2, W_W] + psr_free, inc=(s_p if c == 7 else None))
                            xnTf_free[xb] = [tr, te1]
                            if KSTOP <= 4:
                                continue
                            R = rt[xb]
                            lg = R[:, 0:20]; gmax = R[:, 20:21]; ngmax = R[:, 21:22]; ge = R[:, 22:26]; gsum = R[:, 26:27]
                            gval = R[:, 27:28]; goh = R[:, 28:32]; pen = R[:, 32:36]; elm = R[:, 36:52]; m1 = R[:, 52:53]
                            oh1 = R[:, 53:69]; elm2 = R[:, 69:85]; m2 = R[:, 85:86]; dm = R[:, 86:87]; ex = R[:, 87:88]
                            den = R[:, 88:89]; e1 = R[:, 89:90]; e2 = R[:, 90:91]; w1 = R[:, 91:92]; w2 = R[:, 92:93]
                            oh2 = xn[xb][:, 0:16]
                            oh2 = R[:, 93:96]
                            V = lambda fn, w: fw.op('dve', fn, waits=w, inc=s_v)
                            A = lambda fn, w: fw.op('act', fn, waits=w, inc=s_a)
                            q = V(lambda e: e.tensor_tensor(out=lg, in0=ps_r[:, 0:20], in1=rb[:], op=ALU.add), [tr, W_W] + rt_free[xb])
                            psr_free = [q]
                            q = V(lambda e: e.memset(gsum, 0.0), [q])
                            q = V(lambda e: e.reduce_max(out=gmax, in_=lg[:, 0:4], axis=AX.X), [q])
                            q = V(lambda e: e.tensor_scalar(out=ngmax, in0=gmax, scalar1=-1.0, scalar2=None, op0=ALU.mult), [q])
                            qa = A(lambda e: e.activation(out=ge, in_=lg[:, 0:4], func=AF.Exp, bias=ngmax, scale=1.0, accum_out=gsum), [q])
                            q = V(lambda e: e.reciprocal(gval, gsum), [qa])
                            q = V(lambda e: e.tensor_scalar(out=goh, in0=lg[:, 0:4], scalar1=gmax, scalar2=None, op0=ALU.is_equal), [q])
                            q = V(lambda e: e.tensor_scalar(out=pen, in0=goh, scalar1=-1.0, scalar2=-NEGV, op0=ALU.add, op1=ALU.mult), [q])
                            q = V(lambda e: e.tensor_tensor(out=elm.rearrange("p (g k) -> p g k", k=4), in0=lg[:, 4:20].rearrange("p (g k) -> p g k", k=4),
                                                            in1=pen.unsqueeze(2).to_broadcast([128, 4, 4]), op=ALU.add), [q])
                            q = V(lambda e: e.reduce_max(out=m1, in_=elm, axis=AX.X), [q])
                            q = V(lambda e: e.tensor_scalar(out=oh1, in0=elm, scalar1=m1, scalar2=None, op0=ALU.is_equal), [q])
                            q = V(lambda e: e.scalar_tensor_tensor(out=elm2, in0=oh1, scalar=NEGV, in1=elm, op0=ALU.mult, op1=ALU.add), [q])
                            q = V(lambda e: e.reduce_max(out=m2, in_=elm2, axis=AX.X), [q])
                            q = V(lambda e: e.tensor_tensor(out=dm, in0=m2, in1=m1, op=ALU.subtract), [q])
                            qa = A(lambda e: e.activation(out=ex, in_=dm, func=AF.Exp), [q])
                            q = V(lambda e: e.tensor_scalar(out=den, in0=ex, scalar1=1.0, scalar2=None, op0=ALU.add), [qa])
                            q = V(lambda e: e.reciprocal(e1, den), [q])
                            q = V(lambda e: e.tensor_tensor(out=e2, in0=ex, in1=e1, op=ALU.mult), [q])
                            q = V(lambda e: e.tensor_tensor(out=w1, in0=e1, in1=gval, op=ALU.mult), [q])
                            q = V(lambda e: e.tensor_tensor(out=w2, in0=e2, in1=gval, op=ALU.mult), [q])
                            q = V(lambda e: e.tensor_scalar(out=elm, in0=elm2, scalar1=m2, scalar2=w2, op0=ALU.is_equal, op1=ALU.mult), [q])
                            q = V(lambda e: e.scalar_tensor_tensor(out=gates[:, t, :], in0=oh1, scalar=w1, in1=elm, op0=ALU.mult, op1=ALU.add),
                                  [q] + pass_free)
                            rt_free[xb] = [q]
                            last_gate = q
                        at_free[ab] = [t_last_op]
                    W_A = [last_gate, (s_a, s_a.n), (s_p, s_p.n), (s_g, s_g.n)]
                    for e_ in ('pe', 'act', 'dve', 'pool', 'sp'):
                        fw._waits(e_, W_A + [(s_v, s_v.n), (s_g, s_g.n)] + fw.dma_all())
                with ExitStack() as pb:
                    sbb = lambda name, shape, dt: pb.enter_context(nc.sbuf_tensor(f"{tag}b{pz}_" + name, shape, dt))
                    pmb = lambda name, shape, dt: pb.enter_context(nc.psum_tensor(f"{tag}bP{pz}_" + name, shape, dt))
                    wg = [sbb(f"wg{i}", [128, 8, DE], BF16) for i in range(2)]
                    wu = [sbb(f"wu{i}", [128, 8, DE], BF16) for i in range(2)]
                    wd = [sbb(f"wd{i}", [128, 4, D], BF16) for i in range(2)]
                    hidT = [sbb(f"hid{i}", [128, 4, 512], BF16) for i in range(2)]
                    sg = [sbb(f"sg{i}", [128, 512], F32) for i in range(2)]
                    ps_g = [pmb(f"g{i}", [128, 512], F32) for i in range(2)]
                    ps_u = [pmb(f"u{i}", [128, 512], F32) for i in range(2)]
                    ps_d = [pmb(f"d{i}", [128, 512], F32) for i in range(3)]
                    w_free = [[], []]
                    w_tok = {}

                    def load_w(e_):
                        b = e_ % 2
                        s_wp = fw.group('pool')
                        for c in range(8):
                            fw.dma('pool', wg[b][:, c, :], I["moe_w_gate"][layer, e_, c * 128:(c + 1) * 128, :], waits=w_free[b], inc=s_wp)
                            fw.dma('pool', wu[b][:, c, :], I["moe_w_up"][layer, e_, c * 128:(c + 1) * 128, :], waits=w_free[b], inc=s_wp)
                        for c in range(4):
                            fw.dma('pool', wd[b][:, c, :], I["moe_w_down"][layer, e_, c * 128:(c + 1) * 128, :], waits=w_free[b], inc=s_wp)
                        w_tok[e_] = s_wp.token()

                    g_free = [[], []]; u_free = [[], []]; d_free = [[], [], []]
                    hid_free = [[], []]; sg_free = [[], []]
                    cntb = dict(gu=0, d=0, u=0)
                    units = [(e_, blk) for e_ in range(int(os.environ.get('KNEXP', NE))) for blk in range(4)]
                    hid_ready = {}

                    def stageA(ui):
                        e_, blk = units[ui]
                        b = e_ % 2
                        hb = ui % 2
                        toks = []
                        for fc in range(4):
                            gb = cntb['gu'] % 2; cntb['gu'] += 1
                            tg = None
                            for c in range(8):
                                tg = fw.op('pe', lambda e: e.matmul(ps_g[gb][:], wg[b][:, c, fc * 128:(fc + 1) * 128], xnT[:, c, blk * 512:(blk + 1) * 512],
                                                                    start=(c == 0), stop=(c == 7)),
                                           waits=[w_tok[e_]] + g_free[gb], inc=(s_p if c == 7 else None))
                            tu = None
                            for c in range(8):
                                tu = fw.op('pe', lambda e: e.matmul(ps_u[gb][:], wu[b][:, c, fc * 128:(fc + 1) * 128], xnT[:, c, blk * 512:(blk + 1) * 512],
                                                                    start=(c == 0), stop=(c == 7)),
                                           waits=u_free[gb], inc=(s_p if c == 7 else None))
                            ts = fw.op('act', lambda e: e.activation(out=sg[gb][:], in_=ps_g[gb][:], func=AF.Silu),
                                       waits=[tg] + sg_free[gb], inc=s_a)
                            g_free[gb] = [ts]
                            th = fw.op('dve', lambda e: e.tensor_tensor(out=hidT[hb][:, fc, :], in0=sg[gb][:], in1=ps_u[gb][:], op=ALU.mult),
                                       waits=[ts, tu] + (hid_free[hb] if fc == 0 else []), inc=s_v)
                            u_free[gb] = [th]
                            sg_free[gb] = [th]
                            toks.append(th)
                        hid_ready[ui] = toks[-1]

                    def stageB(ui):
                        e_, blk = units[ui]
                        b = e_ % 2
                        hb = ui % 2
                        tm = None
                        for tt in range(4):
                            t = blk * 4 + tt
                            for dh in range(2):
                                db = cntb['d'] % 3; cntb['d'] += 1
                                for fc in range(4):
                                    tm = fw.op('pe', lambda e: e.matmul(ps_d[db][:], hidT[hb][:, fc, tt * 128:(tt + 1) * 128],
                                                                        wd[b][:, fc, dh * 512:(dh + 1) * 512], start=(fc == 0), stop=(fc == 3)),
                                               waits=[hid_ready[ui]] + d_free[db], inc=(s_p if fc == 3 else None))
                                ta = fw.op('dve', lambda e: e.scalar_tensor_tensor(out=acc[:, t, dh * 512:(dh + 1) * 512], in0=ps_d[db][:],
                                                                                    scalar=gates[:, t, e_:e_ + 1], in1=acc[:, t, dh * 512:(dh + 1) * 512],
                                                                                    op0=ALU.mult, op1=ALU.add),
                                           waits=[tm], inc=s_v)
                                d_free[db] = [ta]
                        hid_free[hb] = [tm]
                        if blk == 3:
                            w_free[b] = [tm]
                            if e_ + 2 < len(units) // 4:
                                load_w(e_ + 2)

                    if len(units) > 0:
                        load_w(0)
                    if len(units) > 4:
                        load_w(1)
                    for ui in range(len(units) + 1):
                        if ui < len(units):
                            stageA(ui)
                        if ui - 1 >= 0:
                            stageB(ui - 1)
                    t_fin = (s_v, s_v.n)
                    td = fw.dma('sp', outp.rearrange("(t p) d -> p t d", p=128), acc[:], waits=[t_fin], inc=s_st)
                    pass_free = [td, (s_p, s_p.n)]
                    for e_ in ('pe', 'act', 'dve', 'pool', 'sp'):
                        fw._waits(e_, [td, (s_p, s_p.n), (s_a, s_a.n), (s_v, s_v.n)] + fw.dma_all())
            return pass_free[0]


    def proj_phase(tag, src, N, norm_ap, w_ap, WC, gain_aps, njobs, vgroups):
        NBLK = N // 512
        with ExitStack() as ps:
            sb = lambda name, shape, dt: ps.enter_context(nc.sbuf_tensor(f"{tag}_" + name, shape, dt))
            pm = lambda name, shape, dt: ps.enter_context(nc.psum_tensor(f"{tag}P_" + name, shape, dt))
            w_sb = sb("w", [128, 8, WC], BF16)
            g_bc = sb("g_bc", [128, D], F32)
            gq = sb("gq", [128, max(1, len(gain_aps))], F32)
            xs = [sb(f"xs{i}", [128, D], F32) for i in range(2)]
            junk = sb("junk", [128, D], BF16)
            xn = [sb(f"xn{i}", [128, D], BF16) for i in range(2)]
            xnT = [sb(f"xnT{i}", [128, 8, 512], BF16) for i in range(2)]
            st = sb("st", [128, 3 * (N // 128)], F32)
            sqk = [sb(f"sqk{i}", [128, 512], BF16) for i in range(2)]
            lnv = [sb(f"lnv{i}", [128, 512], F32) for i in range(2)]
            rstd = [sb(f"rstd{i}", [128, 512], F32) for i in range(2)]
            kst = [sb(f"kst{i}", [128, 512], BF16) for i in range(3)]
            NG = len(vgroups)
            vst = [sb(f"vst{i}", [128, max(1, NG), 4, 8, 65], BF16) for i in range(2)] if NG else None
            psA = [pm(f"psA{i}", [128, 512], F32) for i in range(5)]
            ps2 = [pm(f"ps2{i}", [128, 512], F32) for i in range(2)]
            pt = pm("pt0", [128, 8, 128], BF16)
            s_a = fw.sem(tag + "a"); s_v = fw.sem(tag + "v"); s_p = fw.sem(tag + "p"); s_g = fw.sem(tag + "g")
            gwp = fw.group('pool')
            for c in range(8):
                fw.dma('pool', w_sb[:, c, :], w_ap[c * 128:(c + 1) * 128, :], inc=gwp)
            gws = fw.group('sp')
            fw.dma('sp', g_bc[:], norm_ap.partition_broadcast(128), inc=gws)
            for gi, (gap, gscale) in enumerate(gain_aps):
                for hh in range(2):
                    fw.dma('sp', gq[hh * 64:(hh + 1) * 64, gi:gi + 1], gap.rearrange("(d o) -> d o", o=1), inc=gws)
            W_W = [gwp.token(), gws.token()]
            t = fw.op('pool', lambda e: e.memset(st[:], 0.0), inc=s_g)
            if NG:
                for i in range(2):
                    t = fw.op('pool', lambda e: e.memset(vst[i][:], 1.0), inc=s_g)
            W_INIT = t
            t_gq = None
            for gi, (gap, gscale) in enumerate(gain_aps):
                if gscale != 1.0:
                    t_gq = fw.op('dve', lambda e: e.tensor_scalar(out=gq[:, gi:gi + 1], in0=gq[:, gi:gi + 1], scalar1=gscale, scalar2=None,
                                                                    op0=ALU.mult), waits=[W_W], inc=s_v)
            xs_free = [[], []]; xn_free = [[], []]; pt_free = [[]]; xnT_free = [[], []]
            psA_free = [[] for _ in range(5)]; ps2_free = [[], []]; sqk_free = [[], []]; lnv_free = [[], []]; rstd_free = [[], []]
            kst_free = [[], [], []]; vst_free = [[], []]
            cnt = dict(a=0, k=0, j=0, ks=0)

            def tile_front(tb, ti):
                tg = tb * 4 + ti
                b = tg % 2; bb = tb % 2
                tx = fw.dma('sp', xs[b][:], src[tg * 128:(tg + 1) * 128, :], waits=xs_free[b])
                t3 = rms_stats(xs[b][:], junk[:], st, 3 * tg, D, [tx, W_INIT], s_a)
                tn = fw.op('dve', lambda e: e.scalar_tensor_tensor(out=xn[b][:], in0=xs[b][:], scalar=st[:, 3 * tg + 2:3 * tg + 3],
                                                                    in1=g_bc[:], op0=ALU.mult, op1=ALU.mult),
                           waits=[t3, W_W] + xn_free[b], inc=s_v)
                xs_free[b] = [tn]
                tt = None
                for c in range(8):
                    tt = fw.op('pe', lambda e: e.transpose(pt[:, c, :], xn[b][:, c * 128:(c + 1) * 128], ident_bf[:]),
                               waits=[tn] + W_CST + pt_free[0], inc=(s_p if c == 7 else None))
                xn_free[b] = [tt]
                te = fw.op('dve', lambda e: e.tensor_copy(xnT[bb][:, :, ti * 128:(ti + 1) * 128], pt[:]),
                           waits=[tt] + xnT_free[bb], inc=s_v)
                pt_free[0] = [te]
                return te

            for tb in range(NBLK):
                bb = tb % 2
                t_xnT = None
                for ti in range(4):
                    t_xnT = tile_front(tb, ti)
                vb = tb % 2
                jobs = [("n",) + j for j in njobs]
                for ti in range(4):
                    for gi, vcol in enumerate(vgroups):
                        jobs.append(("v", ti, gi, vcol))

                def stage1(job):
                    a = cnt['a'] % 5; cnt['a'] += 1
                    if job[0] == "n":
                        _, col0, gcol, dst = job
                        tm = None
                        for c in range(8):
                            tm = fw.op('pe', lambda e: e.matmul(psA[a][:], w_sb[:, c, col0:col0 + 128], xnT[bb][:, c, :],
                                                                start=(c == 0), stop=(c == 7)),
                                       waits=[t_xnT, W_W] + psA_free[a], inc=(s_p if c == 7 else None))
                        k2 = cnt['k'] % 2; cnt['k'] += 1
                        tsq = fw.op('act', lambda e: e.activation(out=sqk[k2][:], in_=psA[a][:], func=AF.Square),
                                    waits=[tm] + sqk_free[k2], inc=s_a)
                        return ("norm", gcol, dst, a, k2, tm, tsq)
                    _, ti, gi, vcol = job
                    tm = None
                    for c in range(8):
                        tm = fw.op('pe', lambda e: e.matmul(psA[a][:], xnT[bb][:, c, ti * 128:(ti + 1) * 128], w_sb[:, c, vcol:vcol + 512],
                                                            start=(c == 0), stop=(c == 7)),
                                   waits=[t_xnT, W_W] + psA_free[a], inc=(s_p if c == 7 else None))
                    tev = fw.op('act', lambda e: e.copy(vst[vb][:, gi, ti, :, 0:64], psA[a][:].rearrange("p (h d) -> p h d", d=64)),
                                waits=[tm, W_INIT] + vst_free[vb], inc=s_a)
                    psA_free[a] = [tev]
                    return None

                def stage2(rec):
                    _, gcol, dst, a, k2, tm, tsq = rec
                    j = cnt['j'] % 2; cnt['j'] += 1
                    t2 = fw.op('pe', lambda e: e.matmul(ps2[j][:], blk_bf[:], sqk[k2][:], start=True, stop=True),
                               waits=[tsq] + W_CST + ps2_free[j], inc=s_p)
                    sqk_free[k2] = [t2]
                    tl = fw.op('act', lambda e: e.activation(out=lnv[j][:], in_=ps2[j][:], func=AF.Ln, scale=1.0 / 64, bias=EPS),
                               waits=[t2] + lnv_free[j], inc=s_a)
                    ps2_free[j] = [tl]
                    tr = fw.op('act', lambda e: e.activation(out=rstd[j][:], in_=lnv[j][:], func=AF.Exp, scale=-0.5),
                               waits=[tl] + rstd_free[j], inc=s_a)
                    lnv_free[j] = [tr]
                    ks = cnt['ks'] % 3; cnt['ks'] += 1
                    tk = fw.op('dve', lambda e: e.scalar_tensor_tensor(out=kst[ks][:], in0=psA[a][:], scalar=gq[:, gcol:gcol + 1],
                                                                        in1=rstd[j][:], op0=ALU.mult, op1=ALU.mult),
                               waits=[tr, t_gq, tm] + kst_free[ks], inc=s_v)
                    psA_free[a] = [tk]
                    rstd_free[j] = [tk]
                    td = fw.dma('pool', dst[:, tb * 512:(tb + 1) * 512], kst[ks][:], waits=[tk])
                    kst_free[ks] = [td]

                prev = None
                for job in jobs:
                    rec = stage1(job)
                    if prev is not None:
                        stage2(prev)
                    prev = rec
                if prev is not None:
                    stage2(prev)
                if NG:
                    tv = (s_a, s_a.n)
                    td = None
                    for gi in range(NG):
                        td = fw.dma('pool', VB[gi, tb * 512:(tb + 1) * 512, :].rearrange("(t p) c -> p t c", p=128),
                                    vst[vb][:, gi, :, :, :].rearrange("p t h c -> p t (h c)"), waits=[tv])
                    vst_free[vb] = [td] + fw.dma_all()
                xnT_free[bb] = [(s_p, s_p.n)]
            done = fw.dma_all()
            for e in ('pe', 'act', 'dve', 'pool', 'sp'):
                fw._waits(e, [done, (s_p, s_p.n), (s_a, s_a.n), (s_v, s_v.n), (s_g, s_g.n)])

    def phase5():
        njobs = [(ch * 128, 0, KB[ch * 128:(ch + 1) * 128, :]) for ch in range(12)]
        proj_phase("p5", H1, NQ, I["kv_norm"], I["kv_w"], 3072, [(I["kv_k_gain"], 1.0)], njobs, [1536, 2048, 2560])

    def phase5b():
        for bi, (qs, pv) in enumerate(cfg.BS):
            njobs = [(ch * 128, 0, QB[ch * 128:(ch + 1) * 128, bi * SPAN:(bi + 1) * SPAN]) for ch in range(12)]
            proj_phase(f"p5b{bi}", H1[qs * SPAN:(qs + 1) * SPAN, :], SPAN, I["b_norm"][0] if False else I["b_norm"], I["b_w_q"], 1536,
                       [(I["b_q_gain"], SCALE)], njobs, [])

    def phase6():
        with ExitStack() as ps:
            sb = lambda name, shape, dt: ps.enter_context(nc.sbuf_tensor("p6_" + name, shape, dt))
            pm = lambda name, shape, dt: ps.enter_context(nc.psum_tensor("p6P_" + name, shape, dt))
            expB = sb("expB", [128, 3, 8, 2, 128], BF16)
            kbias = sb("kbias", [128, len(cfg.QS)], F32)
            ps_s = [pm(f"s{i}", [128, 512], F32) for i in range(4)]
            ps_o = [pm(f"o{i}", [65, 512], F32) for i in range(2)]
            ps_bc = pm("bc", [128, 512], F32)
            s_a = fw.sem("p6a"); s_v = fw.sem("p6v"); s_p = fw.sem("p6p"); s_g = fw.sem("p6g")

            with ExitStack() as pb:
                sbb = lambda name, shape, dt: pb.enter_context(nc.sbuf_tensor("p6b_" + name, shape, dt))
                biasT = sbb("biasT", [128, 3, 8, 2, 128], F32)
                rb_sb = sbb("rb", [32, 24], F32)
                oh_sb = sbb("oh", [32, 3, 384], F32)
                ng_sb = sbb("ng", [1, 3, 384], F32)
                anti = sbb("anti", [128, 128], F32)
                fvs = sbb("fvs", [24, 3, 384], F32)
                hank = [sbb(f"hank{i}", [128, 128], F32) for i in range(2)]
                g0 = fw.group('sp')
                fw.dma('sp', rb_sb[:], I["rel_bias"][:, :], inc=g0)
                fw.dma('sp', oh_sb[:], I["c_oh"].rearrange("g k m -> k g m"), inc=g0)
                fw.dma('sp', ng_sb[:], I["c_ng"].rearrange("g o m -> o g m"), inc=g0)
                fw.dma('sp', anti[:], I["c_anti"][:, :], inc=g0)
                if cfg.masks:
                    fw.dma('sp', kbias[:], I["c_kbias"].partition_broadcast(128), inc=g0)
                tk0 = g0.token()
                tprev = []
                tds = []
                for g in range(3):
                    fw.op('pe', lambda e: e.matmul(ps_bc[0:24, 0:384], rb_sb[:], oh_sb[:, g, :], start=True, stop=False),
                          waits=[tk0] + W_CST + tprev)
                    tm = fw.op('pe', lambda e: e.matmul(ps_bc[0:24, 0:384], ones_f[0:1, 0:24], ng_sb[0:1, g, :], start=False, stop=True), inc=s_p)
                    tc = fw.op('dve', lambda e: e.tensor_copy(fvs[:, g, :], ps_bc[0:24, 0:384]), waits=[tm], inc=s_v)
                    tprev = [tc]
                    tds.append(fw.dma('sp', FV[g * 8:(g + 1) * 8, :], fvs[g * 8:(g + 1) * 8, g, :], waits=[tc]))
                hk_free = [[], []]
                ci = 0
                for g in range(3):
                    for h in range(8):
                        for pc in range(2):
                            hb = ci % 2; ci += 1
                            head = g * 8 + h
                            off = head * 384 + (128 if pc == 0 else 0)
                            srcap = bass.AP(FV.tensor, off, [[1, 128], [1, 128]])
                            tl = fw.dma('sp', hank[hb][:], srcap, waits=tds + hk_free[hb])
                            tm = fw.op('pe', lambda e: e.matmul(ps_bc[:, 0:128], anti[:], hank[hb][:], start=True, stop=True),
                                       waits=[tl] + tprev, inc=s_p)
                            hk_free[hb] = [tm]
                            tc = fw.op('dve', lambda e: e.tensor_copy(biasT[:, g, h, pc, :], ps_bc[:, 0:128]), waits=[tm], inc=s_v)
                            tprev = [tc]
                t_eb = fw.op('act', lambda e: e.activation(out=expB[:].rearrange("p g h c a -> p (g h c a)"),
                                                             in_=biasT[:].rearrange("p g h c a -> p (g h c a)"), func=AF.Exp), waits=tprev, inc=s_a)
                W_BIAS = [t_eb]
                for e in ('pe', 'act', 'dve', 'pool', 'sp'):
                    fw._waits(e, W_BIAS + [(s_p, s_p.n)] + fw.dma_all())

            k_sb = sb("k", [128, 4, 2 * SPAN], BF16)
            q_sb = sb("q", [128, 4, SPAN], BF16)
            v_sb = sb("v", [128, 2, 16, 520], BF16)
            oacc = sb("oacc", [65, 8, SPAN], F32)
            p_sb = [sb(f"p{i}", [128, 512], BF16) for i in range(4)]
            rc = [sb(f"rc{i}", [65, 512], F32) for i in range(2)]
            ost = [sb(f"ost{i}", [64, 512], BF16) for i in range(2)]
            ld_free = []
            oacc_free = []
            st_s = dict(a=0, o=0)
            psS_free = [[], [], [], []]
            pS_free = [[], [], [], []]
            pso_free = [[], []]
            bc_free = list(W_BIAS)
            rc_free = [[], []]; ost_free = [[], []]
            for bi, (qs, pv) in enumerate(cfg.BS):
                last_acc_tok = None
                for g, d in enumerate(B_DIL):
                    gl = fw.group('sp')
                    for c in range(4):
                        row0 = (4 * g + c) * 128
                        if pv is not None:
                            fw.dma('sp', k_sb[:, c, 0:SPAN], KB[row0:row0 + 128, pv * SPAN:(pv + 1) * SPAN], waits=ld_free, inc=gl)
                        fw.dma('sp', k_sb[:, c, SPAN:2 * SPAN], KB[row0:row0 + 128, qs * SPAN:(qs + 1) * SPAN], waits=ld_free, inc=gl)
                        fw.dma('sp', q_sb[:, c, :], QB[row0:row0 + 128, bi * SPAN:(bi + 1) * SPAN], waits=ld_free, inc=gl)

                    def vsrc(slot):
                        rows = VB[g, slot * SPAN:(slot + 1) * SPAN, :]
                        if d == 1:
                            return rows.rearrange("(b a) c -> a b c", a=128)
                        if d == 4:
                            return rows.rearrange("(m a r) c -> a m r c", m=4, a=128, r=4)
                        return rows.rearrange("(a r) c -> a r c", r=16)

                    def vdst(pc):
                        if d == 4:
                            return v_sb[:, pc, :, :].rearrange("a (m r) c -> a m r c", r=4)
                        return v_sb[:, pc, :, :]
                    if pv is not None:
                        fw.dma('sp', vdst(0), vsrc(pv), waits=ld_free, inc=gl)
                    fw.dma('sp', vdst(1), vsrc(qs), waits=ld_free, inc=gl)
                    W_LD = [gl.token()]

                    def blk_start(beta):
                        if d == 1:
                            return beta * 128
                        if d == 4:
                            return (beta // 4) * 512 + (beta % 4)
                        return beta

                    def prev_info(beta):
                        st0 = blk_start(beta) - 128 * d
                        if st0 >= 0:
                            return (1, beta - (1 if d == 1 else 4))
                        if pv is None:
                            return None
                        return (0, 15 if d == 1 else (12 + beta % 4 if d == 4 else beta))

                    units = [(h, qd) for h in range(8) for qd in range(4)]
                    urec = {}

                    def stageA(ui):
                        h, qd = units[ui]
                        c = h // 2; hh = h % 2
                        prs = [prev_info(4 * qd + i) for i in range(4)]
                        npv = sum(1 for p_ in prs if p_ is None)
                        c0 = 128 * npv
                        a0 = st_s['a'] % 4; a1 = (st_s['a'] + 1) % 4; st_s['a'] += 2
                        tp = None
                        for i in range(4):
                            if prs[i] is None:
                                continue
                            s0 = blk_start(4 * qd + i)
                            kcols = slice(SPAN + s0 - 128 * d, SPAN + s0 - 128 * d + 127 * d + 1, d)
                            qcols = slice(s0, s0 + 127 * d + 1, d)
                            tp = fw.op('pe', lambda e: e.matmul(ps_s[a0][:, i * 128:(i + 1) * 128], k_sb[hh * 64:(hh + 1) * 64, c, kcols],
                                                                q_sb[hh * 64:(hh + 1) * 64, c, qcols], start=True, stop=True),
                                       waits=W_LD + psS_free[a0], inc=s_p)
                        tcur = None
                        for i in range(4):
                            s0 = blk_start(4 * qd + i)
                            kcols = slice(SPAN + s0, SPAN + s0 + 127 * d + 1, d)
                            qcols = slice(s0, s0 + 127 * d + 1, d)
                            tcur = fw.op('pe', lambda e: e.matmul(ps_s[a1][:, i * 128:(i + 1) * 128], k_sb[hh * 64:(hh + 1) * 64, c, kcols],
                                                                  q_sb[hh * 64:(hh + 1) * 64, c, qcols], start=True, stop=True),
                                         waits=W_LD + psS_free[a1], inc=s_p)
                        tE0 = None
                        if tp is not None:
                            nb_ = 4 - npv
                            te0 = None
                            i0 = npv
                            while i0 < 4:
                                i1 = i0
                                while i1 < 4 and (prs[i1][0] == 0) == (prs[i0][0] == 0):
                                    i1 += 1
                                use_kb = cfg.masks and prs[i0][0] == 0
                                bias_arg = kbias[:, pv:pv + 1] if use_kb else 0.0
                                te0 = fw.op('act', lambda e: e.activation(out=p_sb[a0][:, i0 * 128:i1 * 128], in_=ps_s[a0][:, i0 * 128:i1 * 128],
                                                                          func=AF.Exp, bias=bias_arg, scale=1.0),
                                            waits=[tp, tk0] + pS_free[a0], inc=s_a)
                                i0 = i1
                            psS_free[a0] = [te0]
                            tE0 = fw.op('pool', lambda e: e.tensor_tensor(out=p_sb[a0][:, c0:512].rearrange("p (i a) -> p i a", a=128),
                                                                           in0=p_sb[a0][:, c0:512].rearrange("p (i a) -> p i a", a=128),
                                                                           in1=expB[:, g, h, 0, :].unsqueeze(1).to_broadcast([128, nb_, 128]), op=ALU.mult),
                                        waits=[te0] + W_BIAS, inc=s_g)
                        te1 = fw.op('act', lambda e: e.activation(out=p_sb[a1][:], in_=ps_s[a1][:], func=AF.Exp),
                                    waits=[tcur] + pS_free[a1], inc=s_a)
                        psS_free[a1] = [te1]
                        tE1 = fw.op('dve', lambda e: e.tensor_tensor(out=p_sb[a1][:].rearrange("p (i a) -> p i a", a=128),
                                                                      in0=p_sb[a1][:].rearrange("p (i a) -> p i a", a=128),
                                                                      in1=expB[:, g, h, 1, :].unsqueeze(1).to_broadcast([128, 4, 128]), op=ALU.mult),
                                    waits=[te1] + W_BIAS, inc=s_v)
                        urec[ui] = (prs, a0, a1, tE0, tE1)

                    def stageB(ui):
                        nonlocal last_acc_tok
                        h, qd = units[ui]
                        prs, a0, a1, tE0, tE1 = urec.pop(ui)
                        ob = st_s['o'] % 2; st_s['o'] += 1
                        tm = None
                        for i in range(4):
                            beta = 4 * qd + i
                            if prs[i] is not None:
                                vbuf, vblk = prs[i]
                                fw.op('pe', lambda e: e.matmul(ps_o[ob][:, i * 128:(i + 1) * 128], v_sb[:, vbuf, vblk, h * 65:(h + 1) * 65],
                                                               p_sb[a0][:, i * 128:(i + 1) * 128], start=True, stop=False),
                                      waits=[tE0, tE1] + pso_free[ob])
                            tm = fw.op('pe', lambda e: e.matmul(ps_o[ob][:, i * 128:(i + 1) * 128], v_sb[:, 1, beta, h * 65:(h + 1) * 65],
                                                                p_sb[a1][:, i * 128:(i + 1) * 128], start=(prs[i] is None), stop=True),
                                       waits=[tE0, tE1] + pso_free[ob], inc=(s_p if i == 3 else None))
                        pS_free[a0] = [tm]; pS_free[a1] = [tm]
                        if d == 1:
                            dst = oacc[0:65, h, qd * 512:(qd + 1) * 512].rearrange("p (i a) -> p i a", a=128)
                        elif d == 4:
                            dst = oacc[0:65, h, qd * 512:(qd + 1) * 512].rearrange("p (a r) -> p r a", r=4)
                        else:
                            dst = oacc[0:65, h, :].rearrange("p (a r) -> p r a", r=16)[:, 4 * qd:4 * qd + 4, :]
                        srcp = ps_o[ob][:].rearrange("p (i a) -> p i a", a=128)
                        if g == 0:
                            ta = fw.op('dve', lambda e: e.tensor_copy(dst, srcp), waits=[tm] + oacc_free, inc=s_v)
                        else:
                            ta = fw.op('dve', lambda e: e.tensor_tensor(out=dst, in0=dst, in1=srcp, op=ALU.add), waits=[tm, last_acc_tok], inc=s_v)
                        last_acc_tok = ta
                        pso_free[ob] = [ta]

                    for ui in range(len(units) + 1):
                        if ui < len(units):
                            stageA(ui)
                        if ui >= 1:
                            stageB(ui - 1)
                    ld_free = [(s_p, s_p.n)]
                t_last = None
                ei = 0
                for h in range(8):
                    for cb in range(4):
                        eb = ei % 2; ei += 1
                        cols = slice(cb * 512, (cb + 1) * 512)
                        t1 = fw.op('dve', lambda e: e.reciprocal(rc[eb][64:65, :], oacc[64:65, h, cols]), waits=[last_acc_tok] + rc_free[eb], inc=s_v)
                        t2 = fw.op('pe', lambda e: e.matmul(ps_bc[0:64, :], ones_f[64:65, 0:64], rc[eb][64:65, :], start=True, stop=True),
                                   waits=[t1] + W_CST + bc_free, inc=s_p)
                        rc_free[eb] = [t2]
                        t3 = fw.op('dve', lambda e: e.tensor_tensor(out=ost[eb][:], in0=oacc[0:64, h, cols], in1=ps_bc[0:64, :], op=ALU.mult),
                                   waits=[t2] + ost_free[eb], inc=s_v)
                        bc_free = [t3]
                        t4 = fw.dma('pool', AT1[h * 64:(h + 1) * 64, bi * SPAN + cb * 512: bi * SPAN + (cb + 1) * 512], ost[eb][:], waits=[t3])
                        ost_free[eb] = [t4]
                        t_last = t3
                oacc_free = [t_last]
            done = fw.dma_all()
            for e in ('pe', 'act', 'dve', 'pool', 'sp'):
                fw._waits(e, [done, (s_p, s_p.n), (s_a, s_a.n), (s_v, s_v.n), (s_g, s_g.n)])


    def moe_sparse(layer, N, resid_rows_fn, AT, w_out_name, nch, out_rows_fn, tag, wc_tok):
        NTT = N // 128
        G = 2
        SUP = 128 * G
        STOT = 2 * N + NE * SUP
        NTL = STOT // 128
        NSUP = STOT // SUP
        DUM = 2 * N
        OOBV = DUM
        XN = scratch(tag + "XN", [2 * N + 128, D], BF16)
        HM = scratch(tag + "HM", [N, D], F32)
        OUT2 = scratch(tag + "OUT2", [2 * N + 128, D], F32)
        TOKIDX = scratch(tag + "TOKIDX", [STOT, 1], mybir.dt.int32)
        I32 = mybir.dt.int32
        with ExitStack() as ps:
            sb = lambda name, shape, dt: ps.enter_context(nc.sbuf_tensor(f"{tag}_" + name, shape, dt))
            OH = sb("OH", [128, NTT, 2, 16], F32)
            OHb = sb("OHb", [128, NTT, 2, 16], BF16)
            W12 = sb("W12", [128, NTT, 2], F32)
            RANK = sb("RANK", [128, NTT, 2], F32)
            CNT = sb("CNT", [128, 16], F32)
            SLOTI = sb("SLOTI", [128, NTT, 2], I32)
            WIDX = sb("WIDX", [128, NSUP], I32)
            TOKID = sb("TOKID", [128, 2, NTT], I32)
            stri = sb("stri", [128, 128], BF16)
            ones_b = sb("ones_b", [128, 128], BF16)
            s_a = fw.sem(tag + "a"); s_v = fw.sem(tag + "v"); s_p = fw.sem(tag + "p"); s_g = fw.sem(tag + "g")
            gk = fw.group('pool')
            fw.dma('pool', stri[:], I["c_stri"][:, :], inc=gk)
            t_ob = fw.op('pool', lambda e: e.memset(ones_b[:], 1.0), inc=s_g)
            t_c0 = fw.op('pool', lambda e: e.memset(CNT[:], 0.0), inc=s_g)
            fw.op('pool', lambda e: e.iota(TOKID[:, 0, :], pattern=[[128, NTT]], base=0, channel_multiplier=1), inc=s_g)
            t_tid = fw.op('pool', lambda e: e.iota(TOKID[:, 1, :], pattern=[[128, NTT]], base=N, channel_multiplier=1), inc=s_g)
            W_K = [gk.token(), t_ob, t_c0]
            with ExitStack() as pa:
                sba = lambda name, shape, dt: pa.enter_context(nc.sbuf_tensor(f"{tag}a_" + name, shape, dt))
                pma = lambda name, shape, dt: pa.enter_context(nc.psum_tensor(f"{tag}aP_" + name, shape, dt))
                w_out = sba("w_out", [128, nch, D], BF16)
                at_sb = [sba(f"at{i}", [128, nch, 512], BF16) for i in range(2)]
                g_bc = sba("g_bc", [128, D], F32)
                hb_ = [sba(f"h{i}", [128, 4, D], F32) for i in range(2)]
                xn = [sba(f"xn{i}", [128, D], F32) for i in range(2)]
                xnb = [sba(f"xnb{i}", [128, D], BF16) for i in range(2)]
                junk = sba("junk", [128, D], BF16)
                xnTf = [sba(f"xnTf{i}", [128, 8, 128], F32) for i in range(2)]
                wr = sba("wr", [128, 8, 20], F32)
                rb = sba("rb", [128, 20], F32)
                st = sba("st", [128, 3 * NTT], F32)
                rt = [sba(f"rt{i}", [128, 640], F32) for i in range(2)]
                ps_op = [pma(f"op{i}", [128, 512], F32) for i in range(2)]
                ptfa = pma("ptfa", [128, 4, 128], F32)
                ptfb = pma("ptfb", [128, 4, 128], F32)
                ps_r = [pma(f"r{i}", [128, 512], F32) for i in range(2)]
                ps_k = pma("k", [128, 512], F32)

                s_w = fw.group('sp'); s_wp = fw.group('pool')
                for c in range(nch):
                    fw.dma('pool', w_out[:, c, :], I[w_out_name][c * 128:(c + 1) * 128, :], inc=s_wp)
                fw.dma('sp', g_bc[:], I["ffn_norm"][layer, :].partition_broadcast(128), inc=s_w)
                fw.dma('sp', wr[:, :, 0:4], I["moe_w_group"][layer].rearrange("(c p) g -> p c g", p=128), inc=s_w)
                fw.dma('sp', wr[:, :, 4:20], I["moe_w_expert"][layer].rearrange("(c p) g -> p c g", p=128), inc=s_w)
                fw.dma('sp', rb[:, 0:4], I["moe_b_group"][layer, :].partition_broadcast(128), inc=s_w)
                fw.dma('sp', rb[:, 4:20], I["moe_b_expert"][layer, :].partition_broadcast(128), inc=s_w)
                W_W = [s_w.token(), s_wp.token()]
                W_INIT = fw.op('pool', lambda e: e.memset(st[:], 0.0), inc=s_g)
                at_free = [[], []]; op_free = [[], []]; xn_free = [[], []]; ptf_free = []; xnTf_free = [[], []]
                psr_free = [[], []]; rt_free = [[], []]; h_free = [[], []]; xnb_free = [[], []]; psk_free = []
                last_cnt = t_c0
                pending_rank = []
                pipe = []
                stage_f2 = []
                rblocks = {}

                def advance_pipe(flush):
                    while stage_f2 and (flush or len(stage_f2) > 1 or True):
                        it = stage_f2.pop(0)
                        tr_ = it['F3'](it['te2'])
                        if it['ti'] == 3:
                            pending_rank.append((it['blk'], tr_))
                        break
                    while pipe and (flush or len(pipe) > 1):
                        it = pipe.pop(0)
                        it['te2'] = it['F2']()
                        stage_f2.append(it)
                        if not flush:
                            break
                    if flush:
                        while stage_f2:
                            it = stage_f2.pop(0)
                            tr_ = it['F3'](it['te2'])
                            if it['ti'] == 3:
                                pending_rank.append((it['blk'], tr_))

                def run_router_blocks(flush):
                    nonlocal rank_fn
                    while pending_rank and pending_rank[0][0] in rblocks:
                        bk, tr_ = pending_rank.pop(0)
                        fn = rblocks.pop(bk)(tr_)
                        if rank_fn is not None:
                            rank_fn()
                        rank_fn = fn
                    if flush and rank_fn is not None:
                        rank_fn()
                        rank_fn = None
                rank_fn = None
                for blk in range(N // 512):
                    ab = blk % 2
                    t_at = fw.dma('sp', at_sb[ab][:], AT[:, blk * 512:(blk + 1) * 512].rearrange("(c p) n -> p c n", p=128),
                                  waits=at_free[ab])
                    t_rs = fw.dma('sp', hb_[ab][:], resid_rows_fn(blk).rearrange("(t p) d -> p t d", p=128), waits=h_free[ab])
                    t_last_op = None
                    t_hdone = []
                    for ti in range(4):
                        t = blk * 4 + ti
                        xb = t % 2
                        hT = hb_[ab][:, ti, :]
                        tadd = None
                        for half in range(2):
                            tm = None
                            for c in range(nch):
                                tm = fw.op('pe', lambda e: e.matmul(ps_op[half][:], at_sb[ab][:, c, ti * 128:(ti + 1) * 128],
                                                                    w_out[:, c, half * 512:(half + 1) * 512],
                                                                    start=(c == 0), stop=(c == nch - 1)),
                                           waits=[t_at, W_W] + op_free[half], inc=(s_p if c == nch - 1 else None))
                            t_last_op = tm
                            tadd = fw.op('dve', lambda e: e.tensor_tensor(out=hT[:, half * 512:(half + 1) * 512],
                                                                           in0=hT[:, half * 512:(half + 1) * 512],
                                                                           in1=ps_op[half][:], op=ALU.add),
                                         waits=[tm, t_rs], inc=s_v)
                            op_free[half] = [tadd]
                        t3 = rms_stats(hT, junk[:], st, 3 * t, D, [tadd, W_INIT], s_a)
                        tn = fw.op('dve', lambda e: e.scalar_tensor_tensor(out=xn[xb][:], in0=hT, scalar=st[:, 3 * t + 2:3 * t + 3],
                                                                            in1=g_bc[:], op0=ALU.mult, op1=ALU.mult),
                                   waits=[t3, W_W] + xn_free[xb], inc=s_v)
                        t_hdone.append(tn)
                        tcb = fw.op('pool', lambda e: e.tensor_copy(xnb[xb][:], xn[xb][:]), waits=[tn] + xnb_free[xb], inc=s_g)
                        txs = fw.dma('sp', XN[t * 128:(t + 1) * 128, :], xnb[xb][:], waits=[tcb])
                        txs2 = fw.dma('sp', XN[N + t * 128:N + (t + 1) * 128, :], xnb[xb][:], waits=[tcb])
                        xnb_free[xb] = [txs, txs2]
                        def F2(t=t, xb=xb, tn=tn, tcb=tcb):
                            nonlocal ptf_free
                            tt = None
                            for c in range(8):
                                pdst = (ptfa if c < 4 else ptfb)[:, c % 4, :]
                                tt = fw.op('pe', lambda e: e.matmul(pdst, xn[xb][:, c * 128:(c + 1) * 128], ident_f[:], start=True, stop=True),
                                           waits=[tn] + W_CST + ptf_free, inc=(s_p if c == 7 else None))
                            xn_free[xb] = [tt, tcb]
                            fw.op('dve', lambda e: e.tensor_copy(xnTf[xb][:, 0:4, :], ptfa[:]), waits=[tt] + xnTf_free[xb], inc=s_v)
                            te2 = fw.op('dve', lambda e: e.tensor_copy(xnTf[xb][:, 4:8, :], ptfb[:]), inc=s_v)
                            ptf_free = [te2]
                            return te2

                        def F3(te2, t=t, xb=xb, blk=blk, ti=ti):
                            tr = None
                            for c in range(8):
                                tr = fw.op('pe', lambda e: e.matmul(ps_r[blk % 2][:, ti * 20:(ti + 1) * 20], xnTf[xb][:, c, :], wr[:, c, :], start=(c == 0), stop=(c == 7)),
                                           waits=[te2, W_W] + psr_free[blk % 2], inc=(s_p if c == 7 else None))
                            xnTf_free[xb] = [tr]
                            return tr
                        pipe.append(dict(F2=F2, F3=F3, blk=blk, ti=ti))
                        advance_pipe(False)
                    def router_block(tr_last, blk=blk, ab=ab):
                        nonlocal last_cnt, psk_free
                        rb_i = blk % 2
                        R = rt[rb_i]
                        t0 = blk * 4
                        off = [0]

                        def alloc(n):
                            v = R[:, off[0]:off[0] + n]
                            off[0] += n
                            return v
                        LG = alloc(80).rearrange("p (t c) -> p t c", c=20)
                        gmax = alloc(4); gsh = alloc(16).rearrange("p (t g) -> p t g", g=4); gsum = alloc(4); gval = alloc(4)
                        goh = alloc(16).rearrange("p (t g) -> p t g", g=4); pen = alloc(16).rearrange("p (t g) -> p t g", g=4)
                        elm = alloc(64).rearrange("p (t e) -> p t e", e=16); m1 = alloc(4)
                        elm2 = alloc(64).rearrange("p (t e) -> p t e", e=16); m2 = alloc(4); dm = alloc(4); ex = alloc(4)
                        den = alloc(4); e1 = alloc(4); e2 = alloc(4)
                        cntp = alloc(128).rearrange("p (t c) -> p t c", c=32)
                        rk = alloc(128).rearrange("p (t c) -> p t c", c=32)
                        V = lambda fn, w: fw.op('dve', fn, waits=w, inc=s_v)
                        A = lambda fn, w: fw.op('act', fn, waits=w, inc=s_a)
                        PR = ps_r[rb_i]
                        oh1 = OH[:, t0:t0 + 4, 0, :]; oh2 = OH[:, t0:t0 + 4, 1, :]
                        q = V(lambda e: e.tensor_tensor(out=LG, in0=PR[:, 0:80].rearrange("p (t c) -> p t c", c=20),
                                                        in1=rb[:].unsqueeze(1).to_broadcast([128, 4, 20]), op=ALU.add), [tr_last, W_W] + rt_free[rb_i])
                        psr_free[rb_i] = [q]
                        q = V(lambda e: e.reduce_max(out=gmax, in_=LG[:, :, 0:4], axis=AX.X), [q])
                        q = V(lambda e: e.tensor_tensor(out=gsh, in0=LG[:, :, 0:4], in1=gmax.unsqueeze(2).to_broadcast([128, 4, 4]), op=ALU.subtract), [q])
                        qa = A(lambda e: e.activation(out=gsh, in_=gsh, func=AF.Exp), [q])
                        q = V(lambda e: e.reduce_sum(out=gsum, in_=gsh, axis=AX.X), [qa])
                        q = V(lambda e: e.reciprocal(gval, gsum), [q])
                        q = V(lambda e: e.tensor_tensor(out=goh, in0=LG[:, :, 0:4], in1=gmax.unsqueeze(2).to_broadcast([128, 4, 4]), op=ALU.is_equal), [q])
                        q = V(lambda e: e.tensor_scalar(out=pen, in0=goh, scalar1=-1.0, scalar2=-NEGV, op0=ALU.add, op1=ALU.mult), [q])
                        q = V(lambda e: e.tensor_tensor(out=elm.rearrange("p t (g k) -> p t g k", k=4),
                                                        in0=LG[:, :, 4:20].rearrange("p t (g k) -> p t g k", k=4),
                                                        in1=pen.unsqueeze(3).to_broadcast([128, 4, 4, 4]), op=ALU.add), [q])
                        q = V(lambda e: e.reduce_max(out=m1, in_=elm, axis=AX.X), [q])
                        q = V(lambda e: e.tensor_tensor(out=oh1, in0=elm, in1=m1.unsqueeze(2).to_broadcast([128, 4, 16]), op=ALU.is_equal), [q, W_K])
                        q = V(lambda e: e.scalar_tensor_tensor(out=elm2, in0=oh1, scalar=NEGV, in1=elm, op0=ALU.mult, op1=ALU.add), [q])
                        q = V(lambda e: e.reduce_max(out=m2, in_=elm2, axis=AX.X), [q])
                        q = V(lambda e: e.tensor_tensor(out=oh2, in0=elm2, in1=m2.unsqueeze(2).to_broadcast([128, 4, 16]), op=ALU.is_equal), [q])
                        q = V(lambda e: e.tensor_tensor(out=dm, in0=m2, in1=m1, op=ALU.subtract), [q])
                        qa = A(lambda e: e.activation(out=ex, in_=dm, func=AF.Exp), [q])
                        q = V(lambda e: e.tensor_scalar(out=den, in0=ex, scalar1=1.0, scalar2=None, op0=ALU.add), [qa])
                        q = V(lambda e: e.reciprocal(e1, den), [q])
                        q = V(lambda e: e.tensor_tensor(out=e2, in0=ex, in1=e1, op=ALU.mult), [q])
                        q = V(lambda e: e.tensor_tensor(out=W12[:, t0:t0 + 4, 0], in0=e1, in1=gval, op=ALU.mult), [q])
                        q = V(lambda e: e.tensor_tensor(out=W12[:, t0:t0 + 4, 1], in0=e2, in1=gval, op=ALU.mult), [q])
                        ohf = OH[:, t0:t0 + 4, :, :].rearrange("p t k e -> p t (k e)")
                        ohb = OHb[:, t0:t0 + 4, :, :].rearrange("p t k e -> p t (k e)")
                        q_oh = V(lambda e: e.tensor_copy(ohb, ohf), [q])

                        def rank_part():
                            nonlocal last_cnt, psk_free
                            tk = None
                            for ti in range(4):
                                fw.op('pe', lambda e: e.matmul(ps_k[:, ti * 64:ti * 64 + 32], stri[:], OHb[:, t0 + ti, :, :].rearrange("p k e -> p (k e)"),
                                                               start=True, stop=True), waits=[q_oh] + W_K + psk_free)
                                tk = fw.op('pe', lambda e: e.matmul(ps_k[:, ti * 64 + 32:ti * 64 + 64], ones_b[:], OHb[:, t0 + ti, :, :].rearrange("p k e -> p (k e)"),
                                                                    start=True, stop=True), inc=s_p)
                            PK = ps_k[:, 0:256].rearrange("p (t c) -> p t c", c=64)
                            cntf = CNT[:]
                            qq = V(lambda e: e.tensor_copy(cntp[:, 0, 0:16], cntf), [tk, last_cnt])
                            for ti in range(4):
                                qq = V(lambda e: e.tensor_tensor(out=cntp[:, ti, 16:32], in0=cntp[:, ti, 0:16], in1=PK[:, ti, 32:48], op=ALU.add), [qq])
                                dst_ = cntp[:, ti + 1, 0:16] if ti < 3 else cntf
                                qq = V(lambda e: e.tensor_tensor(out=dst_, in0=cntp[:, ti, 16:32], in1=PK[:, ti, 48:64], op=ALU.add), [qq])
                            qc = qq
                            qq = V(lambda e: e.tensor_tensor(out=rk, in0=PK[:, :, 0:32], in1=cntp, op=ALU.add), [qc])
                            qq = V(lambda e: e.tensor_tensor(out=rk, in0=rk, in1=ohf, op=ALU.mult), [qq])
                            qq = V(lambda e: e.reduce_sum(out=RANK[:, t0:t0 + 4, :], in_=rk.rearrange("p t (k e) -> p t k e", e=16), axis=AX.X), [qq])
                            last_cnt = qq
                            psk_free = [qq]
                            rt_free[rb_i] = [qq]
                        return rank_part
                    rblocks[blk] = router_block
                    run_router_blocks(False)
                    at_free[ab] = [t_last_op]
                    ths = fw.dma('sp', HM[blk * 512:(blk + 1) * 512, :].rearrange("(t p) d -> p t d", p=128), hb_[ab][:], waits=t_hdone)
                    h_free[ab] = [ths]
                advance_pipe(True)
                run_router_blocks(True)
                assert not pipe and not stage_f2 and not pending_rank and not rblocks
                for e_ in ('pe', 'act', 'dve', 'pool', 'sp'):
                    fw._waits(e_, [last_cnt, (s_a, s_a.n), (s_p, s_p.n), (s_v, s_v.n), (s_g, s_g.n)] + fw.dma_all())
            with ExitStack() as pbx:
                sbb = lambda name, shape, dt: pbx.enter_context(nc.sbuf_tensor(f"{tag}B_" + name, shape, dt))
                MT = (2 * N) // SUP
                THR = sbb("THR", [128, MT], F32)
                THRJ = sbb("THRJ", [128, NSUP], F32)
                IOP = sbb("IOP", [128, 1], F32)
                LT = sbb("LT", [128, 16, 16], F32)
                big = sbb("big", [128, 16 * max(MT, NSUP)], F32)
                NTI = sbb("NTI", [128, 16], F32)
                PAD = sbb("PAD", [128, 16], F32)
                BASE = sbb("BASE", [128, 16], F32)
                END = sbb("END", [128, 16], F32)
                tmp2 = sbb("tmp2", [128, 16, 16], F32)
                SLT = sbb("SLT", [128, NTT, 2, 16], F32)
                SLF = sbb("SLF", [128, NTT, 2], F32)
                EBF = sbb("EBF", [128, NSUP], F32)
                FILL = sbb("FILL", [128, STOT // 128], I32)
                g1 = fw.group('sp')
                fw.dma('sp', LT[:].rearrange("p a b -> p (a b)"), I["c_lt"].partition_broadcast(128), inc=g1)
                fw.op('pool', lambda e: e.iota(THR[:], pattern=[[SUP, MT]], base=0, channel_multiplier=0, allow_small_or_imprecise_dtypes=True), inc=s_g)
                fw.op('pool', lambda e: e.iota(THRJ[:], pattern=[[SUP, NSUP]], base=0, channel_multiplier=0, allow_small_or_imprecise_dtypes=True), inc=s_g)
                fw.op('pool', lambda e: e.iota(IOP[:], pattern=[[0, 1]], base=0, channel_multiplier=1, allow_small_or_imprecise_dtypes=True), inc=s_g)
                t_fill = fw.op('pool', lambda e: e.iota(FILL[:], pattern=[[1, STOT // 128]], base=OOBV, channel_multiplier=0), inc=s_g)
                t_tf = fw.dma('pool', TOKIDX.rearrange("(p a) o -> p (a o)", p=128), FILL[:], waits=[t_fill])
                V = lambda fn, w: fw.op('dve', fn, waits=w, inc=s_v)
                q = V(lambda e: e.tensor_tensor(out=big[:, 0:16 * MT].rearrange("p (a m) -> p a m", m=MT),
                                                in0=CNT[:].unsqueeze(2).to_broadcast([128, 16, MT]),
                                                in1=THR[:].unsqueeze(1).to_broadcast([128, 16, MT]), op=ALU.is_gt), [(s_g, s_g.n), g1.token()])
                q = V(lambda e: e.reduce_sum(out=NTI[:], in_=big[:, 0:16 * MT].rearrange("p (a m) -> p a m", m=MT), axis=AX.X), [q])
                q = V(lambda e: e.tensor_scalar(out=PAD[:], in0=NTI[:], scalar1=float(SUP), scalar2=None, op0=ALU.mult), [q])
                q = V(lambda e: e.tensor_tensor(out=tmp2[:], in0=PAD[:].unsqueeze(1).to_broadcast([128, 16, 16]), in1=LT[:], op=ALU.mult), [q])
                q = V(lambda e: e.reduce_sum(out=BASE[:], in_=tmp2[:], axis=AX.X), [q])
                q = V(lambda e: e.tensor_tensor(out=END[:], in0=BASE[:], in1=PAD[:], op=ALU.add), [q])
                q = V(lambda e: e.tensor_tensor(out=SLT[:], in0=OH[:], in1=BASE[:].unsqueeze(1).unsqueeze(1).to_broadcast([128, NTT, 2, 16]), op=ALU.mult), [q])
                q = V(lambda e: e.reduce_sum(out=SLF[:], in_=SLT[:], axis=AX.X), [q])
                q = V(lambda e: e.tensor_tensor(out=SLF[:], in0=SLF[:], in1=RANK[:], op=ALU.add), [q])
                q = V(lambda e: e.tensor_copy(SLOTI[:], SLF[:]), [q])
                t_slot = q
                q = V(lambda e: e.tensor_tensor(out=big[:, 0:NSUP * 16].rearrange("p (j e) -> p j e", e=16),
                                                in0=END[:].unsqueeze(1).to_broadcast([128, NSUP, 16]),
                                                in1=THRJ[:].unsqueeze(2).to_broadcast([128, NSUP, 16]), op=ALU.is_le), [q])
                q = V(lambda e: e.reduce_sum(out=EBF[:], in_=big[:, 0:NSUP * 16].rearrange("p (j e) -> p j e", e=16), axis=AX.X), [q])
                q = V(lambda e: e.tensor_scalar(out=EBF[:], in0=EBF[:], scalar1=15.0, scalar2=128.0, op0=ALU.min, op1=ALU.mult), [q])
                q = V(lambda e: e.tensor_scalar(out=EBF[:], in0=EBF[:], scalar1=IOP[:, 0:1], scalar2=None, op0=ALU.add), [q])
                q = V(lambda e: e.tensor_copy(WIDX[:], EBF[:]), [q])
                t_widx = q
                tsc = []
                SSTOP = os.environ.get('SSTOP', '')
                for t in range(NTT if SSTOP != 'B' else 0):
                    for k in range(2):
                        fw._waits('pool', [t_slot, t_tf, t_tid])
                        sring = fw._ring_next('pool')
                        ins = nc.gpsimd.indirect_dma_start(out=TOKIDX[:, :], out_offset=bass.IndirectOffsetOnAxis(ap=SLOTI[:, t, k:k + 1], axis=0),
                                                           in_=TOKID[:, k, t:t + 1], in_offset=None)
                        sring.n += 16
                        ins.then_inc(sring.h, 16)
                for e_ in ('pe', 'act', 'dve', 'pool', 'sp'):
                    fw._waits(e_, [t_widx, (s_v, s_v.n), (s_g, s_g.n)] + fw.dma_all())
            if SSTOP in ('B', 'C'):
                return
            with ExitStack() as pd:
                sbd = lambda name, shape, dt: pd.enter_context(nc.sbuf_tensor(f"{tag}D_" + name, shape, dt))
                pmd = lambda name, shape, dt: pd.enter_context(nc.psum_tensor(f"{tag}DP_" + name, shape, dt))
                wall = [sbd(f"wall{i}", [128, 12288], BF16) for i in range(3)]
                NXG = 5
                xg = [sbd(f"xg{i}", [128, D], BF16) for i in range(NXG)]
                NID = 10
                idt = [sbd(f"idt{i}", [128, 1], I32) for i in range(NID)]
                xgT = [sbd(f"xgT{i}", [128, 8, 128], BF16) for i in range(2)]
                sg = [sbd(f"sg{i}", [128, 512], F32) for i in range(2)]
                hid = [sbd(f"hid{i}", [128, 512], BF16) for i in range(2)]
                hidT = [sbd(f"hidT{i}", [128, 4, 128], BF16) for i in range(2)]
                NOD = 4
                od = [sbd(f"od{i}", [128, D], F32) for i in range(NOD)]
                ptx = pmd("ptx", [128, 8, 128], BF16)
                pth = pmd("pth", [128, 8, 128], BF16)
                ps_g = [pmd(f"g{i}", [128, 512], F32) for i in range(1)]
                ps_u = [pmd(f"u{i}", [128, 512], F32) for i in range(1)]
                ps_d = [pmd(f"d{i}", [128, 512], F32) for i in range(2)]
                t_z = None
                for i in range(NXG):
                    t_z = fw.op('pool', lambda e: e.memset(xg[i][:], 0.0), inc=s_g)
                t_zr = fw.dma('pool', XN[DUM:DUM + 128, :], xg[0][:, :], waits=[t_z])
                t_z = [t_z, t_zr]
                wall_free = [[], [], []]; xg_free = [[] for _ in range(NXG)]; idt_free = [[] for _ in range(NID)]; xgT_free = [[], []]
                sg_free = [[], []]; hid_free = [[], []]; hidT_free = [[], []]; od_free = [[] for _ in range(NOD)]
                ptx_free = []; pth_free = []; g_free = []; u_free = []; d_free = [[], []]
                rec = {}
                sc_pend = {}
                wtok = {}
                last_sc = [[], []]

                def ind_dma(out, out_off, in_, in_off, bound, waits):
                    fw._waits('pool', waits)
                    sring = fw._ring_next('pool')
                    ins = nc.gpsimd.indirect_dma_start(out=out, out_offset=out_off, in_=in_, in_offset=in_off)
                    sring.n += 16
                    ins.then_inc(sring.h, 16)
                    return (sring, sring.n)

                idtok = {}

                def load_idt(j):
                    ib = j % NID
                    idtok[j] = fw.dma('sp', idt[ib][:], TOKIDX[j * 128:(j + 1) * 128, :], waits=idt_free[ib])

                def loads(j):
                    ib = j % NID; xb = j % NXG
                    J = j // G; wb = J % 3
                    t_i = idtok.pop(j)
                    if j % G == 0:
                        wtok[J] = ind_dma(wall[wb][:], None, WALL[layer][:, :], bass.IndirectOffsetOnAxis(ap=WIDX[:, J:J + 1], axis=0), NE * 128 - 1,
                                          [wc_tok] + wall_free[wb])
                    t_x = ind_dma(xg[xb][:], None, XN[:, :], bass.IndirectOffsetOnAxis(ap=idt[ib][:, :], axis=0), N - 1, [t_i, t_z] + xg_free[xb])
                    rec[j] = dict(t_x=t_x, t_w=wtok[J], t_i=t_i)

                def stage1(j):
                    xb = j % 2; xgb = j % NXG; wb = (j // G) % 3; r = rec[j]
                    tt = None
                    for c in range(8):
                        tt = fw.op('pe', lambda e: e.transpose(ptx[:, c, :], xg[xgb][:, c * 128:(c + 1) * 128], ident_bf[:]),
                                   waits=[r['t_x']] + W_CST + ptx_free, inc=(s_p if c == 7 else None))
                    xg_free[xgb] = [tt]
                    te = fw.op('dve', lambda e: e.tensor_copy(xgT[xb][:], ptx[:]), waits=[tt] + xgT_free[xb], inc=s_v)
                    ptx_free[:] = [te]
                    tg = tu = None
                    for c in range(8):
                        tg = fw.op('pe', lambda e: e.matmul(ps_g[0][:], xgT[xb][:, c, :], wall[wb][:, c * 512:(c + 1) * 512], start=(c == 0), stop=(c == 7)),
                                   waits=[te, r['t_w']] + g_free, inc=(s_p if c == 7 else None))
                        tu = fw.op('pe', lambda e: e.matmul(ps_u[0][:], xgT[xb][:, c, :], wall[wb][:, 4096 + c * 512:4096 + (c + 1) * 512],
                                                            start=(c == 0), stop=(c == 7)),
                                   waits=u_free, inc=(s_p if c == 7 else None))
                    xgT_free[xb] = [tu]
                    ts = fw.op('act', lambda e: e.activation(out=sg[xb][:], in_=ps_g[0][:], func=AF.Silu), waits=[tg] + sg_free[xb], inc=s_a)
                    g_free[:] = [ts]
                    th = fw.op('dve', lambda e: e.tensor_tensor(out=hid[xb][:], in0=sg[xb][:], in1=ps_u[0][:], op=ALU.mult),
                               waits=[ts, tu] + hid_free[xb], inc=s_v)
                    u_free[:] = [th]
                    sg_free[xb] = [th]
                    r['th'] = th

                def stage2(j):
                    xb = j % 2; wb = (j // G) % 3; ib = j % NID; ob = j % NOD; r = rec.pop(j)
                    tt = None
                    for fc in range(4):
                        tt = fw.op('pe', lambda e: e.transpose(pth[:, fc, :], hid[xb][:, fc * 128:(fc + 1) * 128], ident_bf[:]),
                                   waits=[r['th']] + pth_free, inc=(s_p if fc == 3 else None))
                    hid_free[xb] = [tt]
                    te = fw.op('dve', lambda e: e.tensor_copy(hidT[xb][:], pth[:, 0:4, :]), waits=[tt] + hidT_free[xb], inc=s_v)
                    pth_free[:] = [te]
                    tds = []
                    tm = None
                    for dh in range(2):
                        for fc in range(4):
                            tm = fw.op('pe', lambda e: e.matmul(ps_d[dh][:], hidT[xb][:, fc, :],
                                                                wall[wb][:, 8192 + fc * 1024 + dh * 512:8192 + fc * 1024 + (dh + 1) * 512],
                                                                start=(fc == 0), stop=(fc == 3)),
                                       waits=[te] + d_free[dh], inc=(s_p if fc == 3 else None))
                        tv = fw.op('dve', lambda e: e.tensor_copy(od[ob][:, dh * 512:(dh + 1) * 512], ps_d[dh][:]), waits=[tm] + od_free[ob], inc=s_v)
                        d_free[dh] = [tv]
                        tds.append(tv)
                    hidT_free[xb] = [tm]
                    if j % G == G - 1:
                        wall_free[wb] = [tm]
                    sc_pend[j] = (0, ib, ob, tds[-1])

                def scatter(j):
                    k, ib, ob, tdone = sc_pend.pop(j)
                    t_o = ind_dma(OUT2[:, :], bass.IndirectOffsetOnAxis(ap=idt[ib][:, :], axis=0), od[ob][:], None, N - 1, [tdone] + last_sc[k])
                    last_sc[k] = [t_o]
                    od_free[ob] = [t_o]
                    idt_free[ib] = [t_o]

                LA_I = 6; LA_X = 3
                for j in range(min(LA_I, NTL)):
                    load_idt(j)
                for j in range(min(LA_X, NTL)):
                    loads(j)
                for j in range(NTL + 3):
                    if j + LA_I < NTL:
                        load_idt(j + LA_I)
                    if j + LA_X < NTL:
                        loads(j + LA_X)
                    if j < NTL:
                        stage1(j)
                    if 1 <= j <= NTL:
                        stage2(j - 1)
                    if 2 <= j - 0 and (j - 2) in sc_pend:
                        scatter(j - 2)
                assert not sc_pend and not rec
                for e_ in ('pe', 'act', 'dve', 'pool', 'sp'):
                    fw._waits(e_, [(s_a, s_a.n), (s_p, s_p.n), (s_v, s_v.n), (s_g, s_g.n)] + fw.dma_all())
            with ExitStack() as pe_:
                sbe = lambda name, shape, dt: pe_.enter_context(nc.sbuf_tensor(f"{tag}E_" + name, shape, dt))
                hE = [sbe(f"h{i}", [128, D], F32) for i in range(2)]
                o1 = [sbe(f"o1{i}", [128, D], F32) for i in range(2)]
                o2 = [sbe(f"o2{i}", [128, D], F32) for i in range(2)]
                e_free = [[], []]
                for t in range(NTT):
                    b = t % 2
                    ge_ = fw.group('sp')
                    fw.dma('sp', hE[b][:], HM[t * 128:(t + 1) * 128, :], waits=e_free[b], inc=ge_)
                    fw.dma('sp', o1[b][:], OUT2[t * 128:(t + 1) * 128, :], waits=e_free[b], inc=ge_)
                    fw.dma('sp', o2[b][:], OUT2[N + t * 128:N + (t + 1) * 128, :], waits=e_free[b], inc=ge_)
                    q = fw.op('dve', lambda e: e.scalar_tensor_tensor(out=hE[b][:], in0=o1[b][:], scalar=W12[:, t, 0:1], in1=hE[b][:],
                                                                       op0=ALU.mult, op1=ALU.add), waits=[ge_.token()], inc=s_v)
                    q = fw.op('dve', lambda e: e.scalar_tensor_tensor(out=hE[b][:], in0=o2[b][:], scalar=W12[:, t, 1:2], in1=hE[b][:],
                                                                       op0=ALU.mult, op1=ALU.add), waits=[q], inc=s_v)
                    td = fw.dma('pool', out_rows_fn(t), hE[b][:], waits=[q])
                    e_free[b] = [td]
                for e_ in ('pe', 'act', 'dve', 'pool', 'sp'):
                    fw._waits(e_, [(s_v, s_v.n)] + fw.dma_all())

    WALL = [scratch(f"WALL{l}", [NE * 128, 12288], BF16) for l in range(2)]
    wc_state = dict(tok=None)

    wc_jobs = []

    def weight_convert_prepare():
        for l in range(2):
            for e_ in range(NE):
                wc_jobs.append((WALL[l][e_ * 128:(e_ + 1) * 128, 0:4096].rearrange("p (c f) -> p c f", c=8),
                                I["moe_w_gate"][l, e_].rearrange("(c p) f -> p c f", p=128)))
                wc_jobs.append((WALL[l][e_ * 128:(e_ + 1) * 128, 4096:8192].rearrange("p (c f) -> p c f", c=8),
                                I["moe_w_up"][l, e_].rearrange("(c p) f -> p c f", p=128)))
                wc_jobs.append((WALL[l][e_ * 128:(e_ + 1) * 128, 8192:12288].rearrange("p (c f) -> p c f", c=4),
                                I["moe_w_down"][l, e_].rearrange("(c p) f -> p c f", p=128)))

    def wc_issue(n):
        for _ in range(n):
            if not wc_jobs:
                break
            o_, i_ = wc_jobs.pop(0)
            fw.dma('pool', o_, i_)
        if not wc_jobs:
            wc_state['tok'] = [(s_, s_.n) for s_ in fw.rings['pool']]

    def weight_convert():
        weight_convert_prepare()
        wc_issue(10 ** 6)

    def moe0s():
        def rr(blk):
            slot = (blk * 512) // SPAN
            sp = cfg.QS[slot]
            off = (blk * 512) % SPAN
            return I["x"][sp * SPAN + off: sp * SPAN + off + 512, :]
        wc_issue(10 ** 6)
        moe_sparse(0, NQ, rr, AT0, "a_w_out", 8, lambda t: H1[t * 128:(t + 1) * 128, :], "s0", wc_state['tok'])

    def moe1s():
        def rr(blk):
            slot = (blk * 512) // SPAN
            qs = cfg.BS[slot][0]
            off = (blk * 512) % SPAN
            return H1[qs * SPAN + off: qs * SPAN + off + 512, :]
        moe_sparse(1, NB, rr, AT1, "b_w_out", 4, lambda t: Y[t * 128:(t + 1) * 128, :], "s1", wc_state['tok'])

    def moe0():
        return moe_phase(0, len(cfg.QS), lambda pz: I["x"][cfg.QS[pz] * SPAN:(cfg.QS[pz] + 1) * SPAN, :], AT0, "a_w_out", 8,
                         lambda pz: H1[pz * SPAN:(pz + 1) * SPAN, :], "m0")

    def moe1():
        return moe_phase(1, len(cfg.BS), lambda pz: H1[cfg.BS[pz][0] * SPAN:(cfg.BS[pz][0] + 1) * SPAN, :], AT1, "b_w_out", 4,
                         lambda pz: Y[pz * SPAN:(pz + 1) * SPAN, :], "m1")

    stages = {"wc": weight_convert, "wci": (lambda: None), "m0s": moe0s, "m1s": moe1s, "p1": phase1, "p2": phase2, "m0": moe0, "m1": moe1, "p5": phase5, "p5b": phase5b, "p6": phase6}
    last = None
    for sname in cfg.stages:
        last = stages[sname]()
    es.close()
    return nc


_SQUEEZE = ("a_norm", "a_w_in", "a_b_f", "a_q_gain", "a_k_gain", "a_w_out", "b_norm", "b_w_q", "b_q_gain", "b_w_out")
_NC_CACHE = {}


def _get_nc(key, cfg):
    if key not in _NC_CACHE:
        _NC_CACHE[key] = build(cfg)
    return _NC_CACHE[key]


def run_cfg(cfg, inputs, n_batch):
    nc = build(cfg)
    used = nc._used_inputs
    shared = {}
    for k, v in inputs.items():
        if k == "x":
            continue
        a = np.ascontiguousarray(np.asarray(v, dtype=np.float32))
        if k in _SQUEEZE:
            a = a.reshape(a.shape[1:])
        if k in used:
            shared[k] = a
    for k, v in make_consts().items():
        if k in used:
            shared[k] = v
    x = np.asarray(inputs["x"], dtype=np.float32)
    in_maps = []
    for b in range(n_batch):
        m = dict(shared)
        m["x"] = np.ascontiguousarray(x[b])
        in_maps.append(m)
    res = run_bass_kernel_spmd(nc, in_maps, core_ids=list(range(n_batch)))
    return res


def kernel(**inputs):
    x = np.asarray(inputs["x"], dtype=np.float32)
    B, S, _ = x.shape
    KS = S // SPAN
    half = KS // 2
    QS = tuple(range(half - 1, KS))
    BS = tuple((i, i - 1) for i in range(1, half + 1))
    cfg = Cfg(KS=KS, QS=QS, BS=BS, stages=("wci", "p1", "p2", "m0s", "p5", "p5b", "p6", "m1s"), masks=True, n_cores=2 * B)
    nc = build(cfg)
    used = nc._used_inputs
    shared = {}
    for k, v in inputs.items():
        if k == "x":
            continue
        a_ = np.ascontiguousarray(np.asarray(v, dtype=np.float32))
        if k in _SQUEEZE:
            a_ = a_.reshape(a_.shape[1:])
        if k in used:
            shared[k] = a_
    for k, v in make_consts().items():
        if k in used:
            shared[k] = v
    in_maps = []
    for b in range(B):
        for r in range(2):
            m = dict(shared)
            big = np.zeros(S, np.float32)
            kb = np.zeros(len(QS), np.float32)
            if r == 1:
                xs = x[b]
            else:
                xs = np.concatenate([x[b, 0:SPAN]] * half + [x[b, 0:half * SPAN]], axis=0)
                big[0:half * SPAN] = -NEGV
                kb[0] = NEGV
            m["x"] = np.ascontiguousarray(xs)
            m["c_big"] = big
            m["c_kbias"] = kb
            in_maps.append(m)
    res = run_bass_kernel_spmd(nc, in_maps, core_ids=list(range(2 * B)))
    out = np.empty((B, S, D), np.float32)
    for b in range(B):
        out[b, 0:half * SPAN] = np.asarray(res.results[2 * b]["y"])
        out[b, half * SPAN:] = np.asarray(res.results[2 * b + 1]["y"])
    return out
```

```python
import math
import os
from contextlib import ExitStack

import numpy as np
import ml_dtypes

import concourse.bass as bass
import concourse.mybir as mybir
from concourse.bass_utils import run_bass_kernel_spmd

F32 = mybir.dt.float32
BF16 = mybir.dt.bfloat16
AF = mybir.ActivationFunctionType
ALU = mybir.AluOpType
AX = mybir.AxisListType

D = 1024
SPAN = 2048
NH_A = 16
HD = 64
NE = 16
DE = 512
EPS = 1e-6
SCALE = HD ** -0.5
NEGV = -30000.0
B_DIL = (1, 4, 16)


class Cfg:
    def __init__(self, KS=4, QS=(0, 1, 2, 3), BS=((0, None), (1, 0), (2, 1), (3, 2)),
                 stages=("p1", "p2", "m0", "p5", "p5b", "p6", "m1"), expose=(), inject=(), n_cores=4, masks=False):
        self.KS = KS
        self.QS = tuple(QS)
        self.BS = tuple(BS)
        self.SK = KS * SPAN
        self.NQ = len(QS) * SPAN
        self.NB = len(BS) * SPAN
        self.stages = tuple(stages)
        self.expose = set(expose)
        self.inject = set(inject)
        self.n_cores = n_cores
        self.masks = masks


class Sem:
    def __init__(self, nc, es, name):
        self.h = es.enter_context(nc.semaphore(name))
        self.n = 0
        self.name = name
        self.kind = None

    def mark(self, kind):
        assert self.kind in (None, kind), (self.name, self.kind, kind)
        self.kind = kind


class FW:
    def __init__(self, nc, es):
        self.nc = nc
        self.es = es
        self.eng = dict(pe=nc.tensor, act=nc.scalar, dve=nc.vector, pool=nc.gpsimd, sp=nc.sync)
        self.seen = {k: {} for k in self.eng}
        self.nsem = 0
        self.rings = {}
        self.ring_i = {}

    def sem(self, name):
        self.nsem += 1
        return Sem(self.nc, self.es, f"{name}_{self.nsem}")

    def _waits(self, e, waits):
        for w in waits:
            if w is None:
                continue
            if isinstance(w, list):
                self._waits(e, w)
                continue
            s, v = w
            if v is None or v <= 0:
                continue
            if self.seen[e].get(s.name, 0) >= v:
                continue
            self.seen[e][s.name] = v
            self.eng[e].wait_ge(s.h, v)

    def op(self, e, fn, waits=(), inc=None):
        self._waits(e, waits)
        ins = fn(self.eng[e])
        if inc is not None:
            inc.mark('eng')
            inc.n += 1
            ins.then_inc(inc.h, 1)
            return (inc, inc.n)
        return None

    def _ring_next(self, e):
        if e not in self.rings:
            self.rings[e] = [self.sem(f"dq{e}{i}") for i in range(8)]
            self.ring_i[e] = 0
        s = self.rings[e][self.ring_i[e] % len(self.rings[e])]
        self.ring_i[e] += 1
        self._waits(e, [(s, s.n)])
        return s

    def group(self, e):
        return DmaGroup(self, e)

    def dma(self, e, out, in_, waits=(), inc=None):
        if isinstance(inc, DmaGroup):
            assert inc.e == e
            s = inc.sem
        else:
            s = self._ring_next(e)
        self._waits(e, waits)
        ins = self.eng[e].dma_start(out=out, in_=in_)
        s.n += 16
        ins.then_inc(s.h, 16)
        return (s, s.n)

    def dma_all(self):
        return [(s, s.n) for r in self.rings.values() for s in r]


class DmaGroup:
    def __init__(self, fw, e):
        self.e = e
        self.sem = fw._ring_next(e)

    def token(self):
        return (self.sem, self.sem.n)


def _t5_bucket_np(dist):
    dist = np.asarray(dist, np.int64)
    max_exact = 16
    d_f = np.maximum(dist, max_exact).astype(np.float32)
    large = max_exact + (np.log(d_f / np.float32(max_exact)) / np.float32(math.log(2048 / max_exact))
                         * np.float32(32 - max_exact)).astype(np.int32)
    large = np.minimum(large, 31)
    return np.where(dist < max_exact, dist, large)


def make_consts():
    c = {}
    c["c_ident"] = np.eye(128, dtype=np.float32)
    k = np.arange(128)[:, None]
    q = np.arange(128)[None, :]
    c["c_blk"] = ((k // 64) == (q // 64)).astype(np.float32)
    c["c_triu"] = np.where(k <= q, -1.0, 0.0).astype(np.float32)
    qq = np.arange(512)[None, None, :]
    jj = np.arange(4)[None, :, None]
    kk = np.arange(128)[:, None, None]
    c["c_negm"] = np.where(qq < 128 * jj + kk, NEGV, 0.0).astype(np.float32).reshape(128, 2048)
    c["c_anti"] = np.eye(128, dtype=np.float32)[::-1].copy()
    oh = np.zeros((3, 32, 384), np.float32)
    ng = np.zeros((3, 1, 384), np.float32)
    for g, d in enumerate(B_DIL):
        for m in range(384):
            n = m - 127
            if 0 <= n <= 128:
                oh[g, int(_t5_bucket_np(n * d)), m] = 1.0
            else:
                ng[g, 0, m] = NEGV
    c["c_oh"] = oh
    c["c_stri"] = (k < q).astype(np.float32)
    ee = np.arange(16)
    c["c_lt"] = (ee[None, :] < ee[:, None]).astype(np.float32).reshape(256)
    c["c_ng"] = ng
    return c


def build(cfg):
    nc = bass.Bass("TRN2", target_bir_lowering=False)
    es = ExitStack()
    fw = FW(nc, es)
    SK, NQ, NB = cfg.SK, cfg.NQ, cfg.NB

    def ext_in(name, shape, dtype=F32):
        return nc.dram_tensor(name, list(shape), dtype, kind="ExternalInput").ap()

    def scratch(name, shape, dtype):
        if name in cfg.inject:
            kind = "ExternalInput"
        elif name in cfg.expose:
            kind = "ExternalOutput"
        else:
            kind = "Internal"
        return nc.dram_tensor(name, list(shape), dtype, kind=kind).ap()

    SHAPES = dict([("x", [SK, D]), ("a_norm", [D]), ("a_w_in", [D, 3088]), ("a_b_f", [16]), ("a_q_gain", [64]), ("a_k_gain", [64]),
                   ("a_w_out", [D, D]), ("kv_norm", [D]), ("kv_w", [D, 3072]), ("kv_k_gain", [64]),
                   ("rel_bias", [32, 24]), ("b_norm", [D]), ("b_w_q", [D, 1536]), ("b_q_gain", [64]),
                   ("b_w_out", [512, D]), ("ffn_norm", [2, D]), ("moe_w_group", [2, D, 4]), ("moe_b_group", [2, 4]),
                   ("moe_w_expert", [2, D, 16]), ("moe_b_expert", [2, 16]), ("moe_w_gate", [2, NE, D, DE]),
                   ("moe_w_up", [2, NE, D, DE]), ("moe_w_down", [2, NE, DE, D]),
                   ("c_ident", [128, 128]), ("c_blk", [128, 128]), ("c_triu", [128, 128]), ("c_negm", [128, 2048]),
                   ("c_anti", [128, 128]), ("c_oh", [3, 32, 384]), ("c_ng", [3, 1, 384]), ("c_stri", [128, 128]), ("c_lt", [256]), ("c_big", [SK]), ("c_kbias", [len(cfg.QS)])])

    class LazyIn(dict):
        def __missing__(self, k):
            v = ext_in(k, SHAPES[k])
            self[k] = v
            return v
    I = LazyIn()
    nc._used_inputs = I

    QT = scratch("QT", [NH_A, 70, NQ], BF16)
    KT = scratch("KT", [NH_A, 70, SK], BF16)
    VP = scratch("VP", [NH_A, 128, SK // 128, 65], BF16)
    AT0 = scratch("AT0", [D, NQ], BF16)
    H1 = scratch("H1", [NQ, D], F32)
    KB = scratch("KB", [1536, NQ], BF16)
    VB = scratch("VB", [3, NQ, 8 * 65], BF16)
    QB = scratch("QB", [1536, NB], BF16)
    FV = scratch("FV", [24, 384], F32)
    AT1 = scratch("AT1", [512, NB], BF16)
    Y = nc.dram_tensor("y", [NB, D], F32, kind="ExternalOutput").ap()

    s_fin = fw.sem("fin")
    fin_waits = []

    cst = es.enter_context(nc.sbuf_tensor("cst_ident_bf", [128, 128], BF16))
    ident_bf = cst
    ident_f = es.enter_context(nc.sbuf_tensor("cst_ident_f", [128, 128], F32))
    blk_bf = es.enter_context(nc.sbuf_tensor("cst_blk", [128, 128], BF16))
    triu_f = es.enter_context(nc.sbuf_tensor("cst_triu", [128, 128], F32))
    negm_bf = es.enter_context(nc.sbuf_tensor("cst_negm", [128, 2048], BF16))
    ones_f = es.enter_context(nc.sbuf_tensor("cst_ones", [128, 128], F32))
    s_c = fw.group('sp')
    fw.dma('sp', ident_f[:], I["c_ident"][:, :], inc=s_c)
    fw.dma('sp', triu_f[:], I["c_triu"][:, :], inc=s_c)
    s_cp = fw.group('pool')
    fw.dma('pool', ident_bf[:], I["c_ident"][:, :], inc=s_cp)
    fw.dma('pool', blk_bf[:], I["c_blk"][:, :], inc=s_cp)
    fw.dma('pool', negm_bf[:], I["c_negm"][:, :], inc=s_cp)
    s_c1 = fw.sem("cst1")
    w_ones = fw.op('pool', lambda e: e.memset(ones_f[:], 1.0), inc=s_c1)
    W_CST = [s_c.token(), s_cp.token(), w_ones]

    def rms_stats(xs_ap, junk_ap, st_tile, col, n, wait_in, s_act):
        t1 = fw.op('act', lambda e: e.activation(out=junk_ap, in_=xs_ap, func=AF.Square,
                                                  accum_out=st_tile[:, col:col + 1]), waits=wait_in, inc=s_act)
        t2 = fw.op('act', lambda e: e.activation(out=st_tile[:, col + 1:col + 2], in_=st_tile[:, col:col + 1],
                                                  func=AF.Ln, scale=1.0 / n, bias=EPS), waits=[t1], inc=s_act)
        t3 = fw.op('act', lambda e: e.activation(out=st_tile[:, col + 2:col + 3], in_=st_tile[:, col + 1:col + 2],
                                                  func=AF.Exp, scale=-0.5), waits=[t2], inc=s_act)
        return t3

    def phase1():
        NT = SK // 128
        NBLK = SK // 512
        def qcol_of_block(tb):
            span = (tb * 512) // SPAN
            if span in cfg.QS:
                return cfg.QS.index(span) * SPAN + (tb * 512) % SPAN
            return None
        with ExitStack() as ps:
            sb = lambda name, shape, dt: ps.enter_context(nc.sbuf_tensor("p1_" + name, shape, dt))
            pm = lambda name, shape, dt: ps.enter_context(nc.psum_tensor("p1P_" + name, shape, dt))
            w_in = sb("w_in", [128, 8, 3088], BF16)
            g_bc = sb("g_bc", [128, D], F32)
            bf_bc = sb("bf_bc", [128, 16], F32)
            gq = sb("gq", [128, 2], F32)
            xs = [sb(f"xs{i}", [128, D], F32) for i in range(2)]
            junk = sb("junk", [128, D], BF16)
            xn = [sb(f"xn{i}", [128, D], BF16) for i in range(2)]
            xnT = [sb(f"xnT{i}", [128, 8, 512], BF16) for i in range(2)]
            st = sb("st", [128, 3 * NT], F32)
            sqk = [sb(f"sqk{i}", [128, 512], BF16) for i in range(2)]
            lnv = [sb(f"lnv{i}", [128, 512], F32) for i in range(2)]
            rstd = [sb(f"rstd{i}", [128, 512], F32) for i in range(2)]
            kst = [sb(f"kst{i}", [128, 512], BF16) for i in range(3)]
            vst = [sb(f"vst{i}", [128, 16, 4, 65], BF16) for i in range(2)]
            fl = [sb(f"fl{i}", [128, 16], F32) for i in range(2)]
            spv = [sb(f"spv{i}", [128, 16], F32) for i in range(2)]
            cum = sb("cum", [16, SK], F32)
            psA = [pm(f"psA{i}", [128, 512], F32) for i in range(3)]
            ps2 = [pm(f"ps2{i}", [128, 512], F32) for i in range(2)]
            pt = [pm("pt0", [128, 8, 128], BF16)] * 2
            psf = pm("psf", [128, 512], F32)
            pscum = pm("pscum", [128, 512], F32)

            s_w = fw.group('sp'); s_x = None; s_a = fw.sem("p1a"); s_v = fw.sem("p1v")
            s_p = fw.sem("p1p"); s_g = fw.sem("p1g"); s_st = None

            s_wp = fw.group('pool')
            for c in range(8):
                fw.dma('pool', w_in[:, c, :], I["a_w_in"][c * 128:(c + 1) * 128, :], inc=s_wp)
            fw.dma('sp', g_bc[:], I["a_norm"].partition_broadcast(128), inc=s_w)
            fw.dma('sp', bf_bc[:], I["a_b_f"].partition_broadcast(128), inc=s_w)
            for hh in range(2):
                fw.dma('sp', gq[hh * 64:(hh + 1) * 64, 0:1], I["a_q_gain"].rearrange("(d o) -> d o", o=1), inc=s_w)
                fw.dma('sp', gq[hh * 64:(hh + 1) * 64, 1:2], I["a_k_gain"].rearrange("(d o) -> d o", o=1), inc=s_w)
            W_W = [s_w.token(), s_wp.token()]
            t = fw.op('pool', lambda e: e.memset(st[:], 0.0), inc=s_g)
            for i in range(2):
                t = fw.op('pool', lambda e: e.memset(vst[i][:], 1.0), inc=s_g)
            W_INIT = t
            t_gq = fw.op('dve', lambda e: e.tensor_scalar(out=gq[:, 0:1], in0=gq[:, 0:1], scalar1=SCALE, scalar2=None,
                                                            op0=ALU.mult), waits=[W_W], inc=s_v)

            xs_free = [[], []]
            xn_free = [[], []]
            pt_free_l = [[]]
            xnT_free = [[], []]
            psA_free = [[], [], []]
            ps2_free = [[], []]
            sqk_free = [[], []]
            lnv_free = [[], []]
            rstd_free = [[], []]
            kst_free = [[], [], []]
            vst_free = [[], []]
            fl_free = [[], []]
            spv_free = [[], []]
            psf_free = []
            pscum_free = []
            cnt = dict(a=0, k=0, j=0)
            last_cum = [None]

            def emit_tile_front(tb, ti):
                tg = tb * 4 + ti
                b = tg % 2
                bb = tb % 2
                tx = fw.dma('sp', xs[b][:], I["x"][tg * 128:(tg + 1) * 128, :], waits=xs_free[b], inc=s_x)
                t3 = rms_stats(xs[b][:], junk[:], st, 3 * tg, D, [tx, W_INIT], s_a)
                tn = fw.op('dve', lambda e: e.scalar_tensor_tensor(out=xn[b][:], in0=xs[b][:], scalar=st[:, 3 * tg + 2:3 * tg + 3],
                                                                    in1=g_bc[:], op0=ALU.mult, op1=ALU.mult),
                           waits=[t3, W_W] + xn_free[b], inc=s_v)
                xs_free[b] = [tn]
                tt = None
                for c in range(8):
                    tt = fw.op('pe', lambda e: e.transpose(pt[b][:, c, :], xn[b][:, c * 128:(c + 1) * 128], ident_bf[:]),
                               waits=[tn] + W_CST + pt_free_l[0], inc=(s_p if c == 7 else None))
                xn_free[b] = [tt]
                te = fw.op('dve', lambda e: e.tensor_copy(xnT[bb][:, :, ti * 128:(ti + 1) * 128], pt[b][:]),
                           waits=[tt] + xnT_free[bb], inc=s_v)
                pt_free_l[0] = [te]
                return te

            def emit_block(tb):
                bb = tb % 2
                tes = [emit_tile_front(tb, ti) for ti in range(4)]
                t_xnT = tes[-1]
                qcol = qcol_of_block(tb)
                jobs = []
                for kc in range(8):
                    jobs.append(("k", kc))
                if qcol is not None:
                    for qc in range(8):
                        jobs.append(("q", qc))
                for ti in range(4):
                    for half in range(2):
                        jobs.append(("v", ti, half))
                for ti in range(4):
                    jobs.append(("f", ti))
                vb = tb % 2
                pend = []
                last_pe = [None]

                def stage1(job):
                    kind = job[0]
                    if kind in ("k", "q"):
                        ch = job[1]
                        a = cnt['a'] % 3; cnt['a'] += 1
                        col0 = (1024 if kind == "k" else 0) + ch * 128
                        tm = None
                        for c in range(8):
                            tm = fw.op('pe', lambda e: e.matmul(psA[a][:], w_in[:, c, col0:col0 + 128], xnT[bb][:, c, :],
                                                                start=(c == 0), stop=(c == 7)),
                                       waits=[t_xnT, W_W] + psA_free[a], inc=(s_p if c == 7 else None))
                        last_pe[0] = tm
                        k2 = cnt['k'] % 2; cnt['k'] += 1
                        tsq = fw.op('act', lambda e: e.activation(out=sqk[k2][:], in_=psA[a][:], func=AF.Square),
                                    waits=[tm] + sqk_free[k2], inc=s_a)
                        return ("norm", kind, ch, a, k2, tm, tsq)
                    if kind == "v":
                        ti, half = job[1], job[2]
                        a = cnt['a'] % 3; cnt['a'] += 1
                        tm = None
                        for c in range(8):
                            tm = fw.op('pe', lambda e: e.matmul(psA[a][:], xnT[bb][:, c, ti * 128:(ti + 1) * 128],
                                                                w_in[:, c, 2048 + half * 512:2048 + (half + 1) * 512],
                                                                start=(c == 0), stop=(c == 7)),
                                       waits=[t_xnT, W_W] + psA_free[a], inc=(s_p if c == 7 else None))
                        last_pe[0] = tm
                        tev = fw.op('act', lambda e: e.copy(vst[vb][:, half * 8:(half + 1) * 8, ti, 0:64],
                                                            psA[a][:].rearrange("p (h d) -> p h d", d=64)),
                                    waits=[tm, W_INIT] + vst_free[vb], inc=s_a)
                        psA_free[a] = [tev]
                        return ("vdone", tev)
                    if kind == "f":
                        ti = job[1]
                        tg = tb * 4 + ti
                        f2 = tg % 2
                        tm = None
                        for c in range(8):
                            tm = fw.op('pe', lambda e: e.matmul(psf[:, 0:16], xnT[bb][:, c, ti * 128:(ti + 1) * 128],
                                                                w_in[:, c, 3072:3088], start=(c == 0), stop=(c == 7)),
                                       waits=[t_xnT, W_W] + psf_free, inc=(s_p if c == 7 else None))
                        last_pe[0] = tm
                        t1 = fw.op('dve', lambda e: e.tensor_tensor(out=fl[f2][:], in0=psf[:, 0:16], in1=bf_bc[:], op=ALU.add),
                                   waits=[tm] + fl_free[f2], inc=s_v)
                        psf_free[:] = [t1]
                        t2 = fw.op('act', lambda e: e.activation(out=fl[f2][:], in_=fl[f2][:], func=AF.Exp, scale=-1.0),
                                   waits=[t1], inc=s_a)
                        t3 = fw.op('act', lambda e: e.activation(out=spv[f2][:], in_=fl[f2][:], func=AF.Ln, bias=1.0, scale=1.0),
                                   waits=[t2] + spv_free[f2], inc=s_a)
                        fl_free[f2] = [t3]
                        return ("cum", ti, tg, f2, t3)
                    raise ValueError

                def stage2(rec):
                    if rec[0] == "norm":
                        _, kind, ch, a, k2, tm, tsq = rec
                        j = cnt['j'] % 2; cnt['j'] += 1
                        t2 = fw.op('pe', lambda e: e.matmul(ps2[j][:], blk_bf[:], sqk[k2][:], start=True, stop=True),
                                   waits=[tsq] + W_CST + ps2_free[j], inc=s_p)
                        sqk_free[k2] = [t2]
                        tl = fw.op('act', lambda e: e.activation(out=lnv[j][:], in_=ps2[j][:], func=AF.Ln, scale=1.0 / 64, bias=EPS),
                                   waits=[t2] + lnv_free[j], inc=s_a)
                        ps2_free[j] = [tl]
                        tr = fw.op('act', lambda e: e.activation(out=rstd[j][:], in_=lnv[j][:], func=AF.Exp, scale=-0.5),
                                   waits=[tl] + rstd_free[j], inc=s_a)
                        lnv_free[j] = [tr]
                        ks = cnt.setdefault('ks', 0) % 3; cnt['ks'] = cnt.get('ks', 0) + 1
                        gcol = 1 if kind == "k" else 0
                        tk = fw.op('dve', lambda e: e.scalar_tensor_tensor(out=kst[ks][:], in0=psA[a][:], scalar=gq[:, gcol:gcol + 1],
                                                                            in1=rstd[j][:], op0=ALU.mult, op1=ALU.mult),
                                   waits=[tr, t_gq, tm] + kst_free[ks], inc=s_v)
                        psA_free[a] = [tk]
                        rstd_free[j] = [tk]
                        tdl = []
                        for hh in range(2):
                            h = 2 * ch + hh
                            if kind == "k":
                                dst = KT[h, 0:64, tb * 512:(tb + 1) * 512]
                            else:
                                dst = QT[h, 0:64, qcol:qcol + 512]
                            tdl.append(fw.dma('pool', dst, kst[ks][hh * 64:(hh + 1) * 64, :], waits=[tk], inc=s_st))
                        kst_free[ks] = tdl
                    elif rec[0] == "cum":
                        _, ti, tg, f2, t3 = rec
                        tc = fw.op('pe', lambda e: e.matmul(pscum[0:16, 0:128], spv[f2][:], triu_f[:], start=True, stop=True),
                                   waits=[t3] + W_CST + pscum_free, inc=s_p)
                        spv_free[f2] = [tc]
                        if tg == 0:
                            td = fw.op('dve', lambda e: e.tensor_copy(cum[0:16, 0:128], pscum[0:16, 0:128]), waits=[tc], inc=s_v)
                        else:
                            td = fw.op('dve', lambda e: e.tensor_scalar(out=cum[0:16, tg * 128:(tg + 1) * 128], in0=pscum[0:16, 0:128],
                                                                         scalar1=cum[0:16, tg * 128 - 1:tg * 128], scalar2=None, op0=ALU.add),
                                       waits=[tc, last_cum[0]], inc=s_v)
                        last_cum[0] = td
                        pscum_free[:] = [td]

                prev = None
                for job in jobs:
                    rec = stage1(job)
                    if prev is not None:
                        stage2(prev)
                    prev = rec if rec[0] in ("norm", "cum") else None
                    if rec[0] == "vdone":
                        pass
                if prev is not None:
                    stage2(prev)
                tv = (s_a, s_a.n)
                td = fw.dma('pool', VP.rearrange("h p t c -> p h t c")[:, :, tb * 4:(tb + 1) * 4, :], vst[vb][:], waits=[tv], inc=s_st)
                vst_free[vb] = [td]
                xnT_free[bb] = [(s_p, s_p.n)]

            if "wci" in cfg.stages:
                weight_convert_prepare()
            for tb in range(NBLK):
                emit_block(tb)

            t_cum = last_cum[0]
            with ExitStack() as ps2c:
                sb2 = lambda name, shape, dt: ps2c.enter_context(nc.sbuf_tensor("p1c_" + name, shape, dt))
                cq = [sb2("cq0", [16, 6, 1024], BF16)]
                ck = [sb2("ck0", [16, 6, 1024], BF16)]
                r1 = sb2("r1", [16, 1024], F32)
                r2 = sb2("r2", [16, 1024], F32)
                csum = sb2("csum", [16, 1024], F32)
                bigb = [sb2("bigb0", [16, 1024], F32)]
                csum_free = []
                tinit = None
                for i in range(1):
                    fw.op('pool', lambda e: e.memset(cq[i][:], 1.0), inc=s_g)
                    tinit = fw.op('pool', lambda e: e.memset(ck[i][:], 1.0), inc=s_g)
                c_free = [[], []]
                for cc in range(SK // 1024):
                    b = 0
                    src = cum[0:16, cc * 1024:(cc + 1) * 1024]
                    w0 = [t_cum, tinit] + c_free[b]
                    if cfg.masks:
                        tbb = fw.dma('sp', bigb[b][:], I["c_big"][cc * 1024:(cc + 1) * 1024].partition_broadcast(16), waits=c_free[b])
                        tsum = fw.op('dve', lambda e: e.tensor_tensor(out=csum[:], in0=src, in1=bigb[b][:], op=ALU.add), waits=[t_cum, tbb] + csum_free, inc=s_v)
                        src = csum[:]
                        w0 = [tsum, tinit] + c_free[b]
                    t = fw.op('dve', lambda e: e.tensor_copy(cq[b][:, 0, :], src), waits=w0, inc=s_v)
                    t = fw.op('dve', lambda e: e.tensor_tensor(out=r1[:], in0=src, in1=cq[b][:, 0, :], op=ALU.subtract), waits=[t], inc=s_v)
                    t = fw.op('dve', lambda e: e.tensor_copy(cq[b][:, 1, :], r1[:]), waits=[t], inc=s_v)
                    t = fw.op('dve', lambda e: e.tensor_tensor(out=r2[:], in0=r1[:], in1=cq[b][:, 1, :], op=ALU.subtract), waits=[t], inc=s_v)
                    t = fw.op('dve', lambda e: e.tensor_copy(cq[b][:, 2, :], r2[:]), waits=[t], inc=s_v)
                    t = fw.op('dve', lambda e: e.tensor_scalar(out=ck[b][:, 3:6, :], in0=cq[b][:, 0:3, :], scalar1=-1.0, scalar2=None,
                                                                op0=ALU.mult), waits=[t], inc=s_v)
                    csum_free = [t]
                    tdl = [fw.dma('pool', KT[:, 64:70, cc * 1024:(cc + 1) * 1024], ck[b][:], waits=[t], inc=s_st)]
                    span = (cc * 1024) // SPAN
                    if span in cfg.QS:
                        qc0 = cfg.QS.index(span) * SPAN + (cc * 1024) % SPAN
                        tdl.append(fw.dma('pool', QT[:, 64:70, qc0:qc0 + 1024], cq[b][:], waits=[t], inc=s_st))
                    c_free[b] = tdl
                done = fw.dma_all()
                for e in ('pe', 'act', 'dve', 'pool', 'sp'):
                    fw._waits(e, [done, (s_p, s_p.n), (s_a, s_a.n), (s_v, s_v.n), (s_g, s_g.n)])
            return done

    def phase2():
        with ExitStack() as ps:
            sb = lambda name, shape, dt: ps.enter_context(nc.sbuf_tensor("p2_" + name, shape, dt))
            pm = lambda name, shape, dt: ps.enter_context(nc.psum_tensor("p2P_" + name, shape, dt))
            kT = [sb(f"kT{i}", [70, SK], BF16) for i in range(2)]
            qT = [sb(f"qT{i}", [70, NQ], BF16) for i in range(2)]
            vS = [sb(f"vS{i}", [128, SK // 128, 65], BF16) for i in range(2)]
            NPB = 4
            pT = [sb(f"pT{i}", [128, 512], BF16) for i in range(NPB)]
            osb = [sb(f"osb{i}", [65, 512], F32) for i in range(2)]
            rc = [sb(f"rc{i}", [65, 512], F32) for i in range(2)]
            ost = [sb(f"ost{i}", [64, 512], BF16) for i in range(2)]
            ps_s = [pm(f"s{i}", [128, 512], F32) for i in range(NPB)]
            ps_o = [pm(f"o{i}", [65, 512], F32) for i in range(2)]
            ps_bc = pm("bc", [64, 512], F32)

            s_ld = None; s_S = fw.sem("p2S"); s_E = fw.sem("p2E"); s_PV = fw.sem("p2PV")
            s_oc = fw.sem("p2oc"); s_v = fw.sem("p2v"); s_bc = fw.sem("p2bc"); s_st = None

            qblocks = []
            for j, dspan in enumerate(cfg.QS):
                for qb in range(4):
                    nt = dspan * 16 + 4 * qb + 4
                    qblocks.append((j * SPAN + qb * 512, nt))

            head_free = [[], []]
            n = 0
            nqb = 0
            ld_tok = {}
            osb_free = [[], []]
            rc_free = [[], []]
            ost_free = [[], []]
            psbc_free = []
            pso_free = [[], []]

            def load_head(h):
                b = h % 2
                w = head_free[b]
                gl = fw.group('sp')
                fw.dma('sp', kT[b][:], KT[h, :, :], waits=w, inc=gl)
                fw.dma('sp', qT[b][:], QT[h, :, :], waits=w, inc=gl)
                t = fw.dma('sp', vS[b][:], VP[h, :, :, :], waits=w, inc=gl)
                ld_tok[h] = t

            load_head(0)
            for h in range(NH_A):
                b = h % 2
                if h + 1 < NH_A:
                    load_head(h + 1)
                if "wci" in cfg.stages:
                    wc_issue(6)
                W_LD = [ld_tok[h]] + W_CST
                pairs = []
                for qi, (qc0, nt) in enumerate(qblocks):
                    for kt in range(nt):
                        jj = kt - (nt - 4) if kt >= nt - 4 else None
                        pairs.append((qi, qc0, kt, jj, kt == 0, kt == nt - 1))
                NP_ = len(pairs)
                deferred = {}

                def S_job(idx):
                    nonlocal n
                    qi, qc0, kt, jj, first, last = pairs[idx]
                    g = n + idx
                    sbuf = g % NPB
                    wfree = [(s_E, g - NPB + 1)] if g - NPB + 1 > 0 else []
                    if jj is not None:
                        c0 = 128 * jj if not first else 0
                        fw.op('pe', lambda e: e.matmul(ps_s[sbuf][:, c0:512], ident_bf[:], negm_bf[:, jj * 512 + c0:(jj + 1) * 512],
                                                       start=True, stop=False), waits=W_LD + wfree)
                        fw.op('pe', lambda e: e.matmul(ps_s[sbuf][:, c0:512], kT[b][:, kt * 128:(kt + 1) * 128], qT[b][:, qc0 + c0:qc0 + 512],
                                                       start=False, stop=True), inc=s_S)
                    else:
                        fw.op('pe', lambda e: e.matmul(ps_s[sbuf][:], kT[b][:, kt * 128:(kt + 1) * 128], qT[b][:, qc0:qc0 + 512],
                                                       start=True, stop=True), waits=W_LD + wfree, inc=s_S)

                def E_job(idx):
                    g = n + idx
                    sbuf = g % NPB
                    jj_ = pairs[idx][3]
                    c0 = 128 * jj_ if (jj_ is not None and not pairs[idx][4]) else 0
                    wfree = [(s_PV, g - NPB + 1)] if g - NPB + 1 > 0 else []
                    fw.op('act', lambda e: e.activation(out=pT[sbuf][:, c0:512], in_=ps_s[sbuf][:, c0:512], func=AF.Exp),
                          waits=[(s_S, g + 1)] + wfree, inc=s_E)

                def PV_job(idx):
                    qi, qc0, kt, jj, first, last = pairs[idx]
                    g = n + idx
                    sbuf = g % NPB
                    ob = (nqb + qi) % 2
                    w = [(s_E, g + 1)]
                    if first:
                        w = w + pso_free[ob]
                    c0 = 128 * jj if (jj is not None and not first) else 0
                    fw.op('pe', lambda e: e.matmul(ps_o[ob][:, c0:512], vS[b][:, kt, :], pT[sbuf][:, c0:512], start=first, stop=last),
                          waits=w, inc=s_PV)

                def epi_act(qi, idx_last):
                    ob = (nqb + qi) % 2
                    g = n + idx_last
                    t = fw.op('dve', lambda e: e.tensor_copy(osb[ob][:], ps_o[ob][:]), waits=[(s_PV, g + 1)] + osb_free[ob], inc=s_v)
                    pso_free[ob] = [t]
                    t2 = fw.op('dve', lambda e: e.reciprocal(rc[ob][64:65, :], osb[ob][64:65, :]), waits=[t] + rc_free[ob], inc=s_v)
                    return t2

                def epi_pe(qi, t2):
                    ob = (nqb + qi) % 2
                    qc0 = qblocks[qi][0]
                    t3 = fw.op('pe', lambda e: e.matmul(ps_bc[:], ones_f[64:65, 0:64], rc[ob][64:65, :], start=True, stop=True),
                               waits=[t2] + W_CST + psbc_free, inc=s_bc)
                    rc_free[ob] = [t3]
                    t4 = fw.op('dve', lambda e: e.tensor_tensor(out=ost[ob][:], in0=osb[ob][0:64, :], in1=ps_bc[:], op=ALU.mult),
                               waits=[t3] + ost_free[ob], inc=s_v)
                    psbc_free[:] = [t4]
                    osb_free[ob] = [t4]
                    t5 = fw.dma('pool', AT0[h * 64:(h + 1) * 64, qc0:qc0 + 512], ost[ob][:], waits=[t4], inc=s_st)
                    ost_free[ob] = [t5]

                epi_state = {}
                for step in range(NP_ + 8):
                    if step < NP_:
                        S_job(step)
                    if step - 1 >= 0 and step - 1 < NP_:
                        E_job(step - 1)
                    if step - 2 >= 0 and step - 2 < NP_:
                        PV_job(step - 2)
                        qi, qc0, kt, jj, first, last = pairs[step - 2]
                        if last:
                            deferred.setdefault(step + 2, []).append(("act", qi, step - 2))
                    for item in deferred.pop(step, []):
                        if item[0] == "act":
                            t2 = epi_act(item[1], item[2])
                            deferred.setdefault(step + 3, []).append(("pe", item[1], t2))
                        else:
                            epi_pe(item[1], item[2])
                assert not deferred
                n += NP_
                nqb += len(qblocks)
                head_free[b] = [(s_PV, s_PV.n), (s_S, s_S.n)]
            done = fw.dma_all()
            for e in ('pe', 'act', 'dve', 'pool', 'sp'):
                fw._waits(e, [done, (s_PV, s_PV.n), (s_E, s_E.n), (s_v, s_v.n), (s_bc, s_bc.n), (s_oc, s_oc.n), (s_S, s_S.n)])
            return done


    def moe_phase(layer, n_pass, resid_fn, AT, w_out_name, nch, out_fn, tag):
        with ExitStack() as ps:
            sb = lambda name, shape, dt: ps.enter_context(nc.sbuf_tensor(f"{tag}_" + name, shape, dt))
            acc = sb("acc", [128, 16, D], F32)
            xnT = sb("xnT", [128, 8, SPAN], BF16)
            gates = sb("gates", [128, 16, NE], F32)
            s_ld = None; s_a = fw.sem(tag + "a"); s_v = fw.sem(tag + "v"); s_p = fw.sem(tag + "p")
            s_g = fw.sem(tag + "g"); s_st = None
            pass_free = []
            for pz in range(n_pass):
                resid = resid_fn(pz)
                outp = out_fn(pz)
                with ExitStack() as pa:
                    sba = lambda name, shape, dt: pa.enter_context(nc.sbuf_tensor(f"{tag}a{pz}_" + name, shape, dt))
                    pma = lambda name, shape, dt: pa.enter_context(nc.psum_tensor(f"{tag}aP{pz}_" + name, shape, dt))
                    w_out = sba("w_out", [128, nch, D], BF16)
                    at_sb = [sba(f"at{i}", [128, nch, 512], BF16) for i in range(2)]
                    g_bc = sba("g_bc", [128, D], F32)
                    xn = [sba(f"xn{i}", [128, D], F32) for i in range(2)]
                    junk = sba("junk", [128, D], BF16)
                    xnTf = [sba(f"xnTf{i}", [128, 8, 128], F32) for i in range(2)]
                    wr = sba("wr", [128, 8, 20], F32)
                    rb = sba("rb", [128, 20], F32)
                    st = sba("st", [128, 3 * 16], F32)
                    rt = [sba(f"rt{i}", [128, 96], F32) for i in range(2)]
                    ps_op = [pma(f"op{i}", [128, 512], F32) for i in range(2)]
                    ptfa = pma("ptfa", [128, 4, 128], F32)
                    ptfb = pma("ptfb", [128, 4, 128], F32)
                    ps_r = pma("r", [128, 512], F32)

                    s_w = fw.group('sp'); s_wp = fw.group('pool')
                    for c in range(nch):
                        fw.dma('pool', w_out[:, c, :], I[w_out_name][c * 128:(c + 1) * 128, :], waits=pass_free, inc=s_wp)
                    fw.dma('sp', g_bc[:], I["ffn_norm"][layer, :].partition_broadcast(128), waits=pass_free, inc=s_w)
                    fw.dma('sp', wr[:, :, 0:4], I["moe_w_group"][layer].rearrange("(c p) g -> p c g", p=128), inc=s_w)
                    fw.dma('sp', wr[:, :, 4:20], I["moe_w_expert"][layer].rearrange("(c p) g -> p c g", p=128), inc=s_w)
                    fw.dma('sp', rb[:, 0:4], I["moe_b_group"][layer, :].partition_broadcast(128), inc=s_w)
                    fw.dma('sp', rb[:, 4:20], I["moe_b_expert"][layer, :].partition_broadcast(128), inc=s_w)
                    W_W = [s_w.token(), s_wp.token()]
                    W_INIT = fw.op('pool', lambda e: e.memset(st[:], 0.0), waits=pass_free, inc=s_g)
                    at_free = [[], []]
                    op_free = [[], []]
                    xn_free = [[], []]
                    ptf_free = []
                    xnTf_free = [[], []]
                    psr_free = []
                    rt_free = [[], []]
                    last_gate = None
                    for blk in range(4):
                        ab = blk % 2
                        t_at = fw.dma('sp', at_sb[ab][:], AT[:, pz * SPAN + blk * 512: pz * SPAN + (blk + 1) * 512]
                                      .rearrange("(c p) n -> p c n", p=128), waits=at_free[ab] + pass_free, inc=s_ld)
                        t_rs = fw.dma('sp', acc[:, blk * 4:(blk + 1) * 4, :],
                                      resid[blk * 512:(blk + 1) * 512, :].rearrange("(t p) d -> p t d", p=128),
                                      waits=pass_free, inc=s_ld)
                        t_last_op = None
                        for ti in range(4):
                            t = blk * 4 + ti
                            xb = t % 2
                            tadd = None
                            for half in range(2):
                                tm = None
                                for c in range(nch):
                                    tm = fw.op('pe', lambda e: e.matmul(ps_op[half][:], at_sb[ab][:, c, ti * 128:(ti + 1) * 128],
                                                                        w_out[:, c, half * 512:(half + 1) * 512],
                                                                        start=(c == 0), stop=(c == nch - 1)),
                                               waits=[t_at, W_W] + op_free[half], inc=(s_p if c == nch - 1 else None))
                                t_last_op = tm
                                tadd = fw.op('dve', lambda e: e.tensor_tensor(out=acc[:, t, half * 512:(half + 1) * 512],
                                                                               in0=acc[:, t, half * 512:(half + 1) * 512],
                                                                               in1=ps_op[half][:], op=ALU.add),
                                             waits=[tm, t_rs], inc=s_v)
                                op_free[half] = [tadd]
                            KSTOP = int(os.environ.get('KSTOP', 9))
                            if KSTOP <= 1:
                                continue
                            t3 = rms_stats(acc[:, t, :], junk[:], st, 3 * t, D, [tadd, W_INIT], s_a)
                            tn = fw.op('dve', lambda e: e.scalar_tensor_tensor(out=xn[xb][:], in0=acc[:, t, :], scalar=st[:, 3 * t + 2:3 * t + 3],
                                                                                in1=g_bc[:], op0=ALU.mult, op1=ALU.mult),
                                       waits=[t3, W_W] + xn_free[xb], inc=s_v)
                            if KSTOP <= 2:
                                continue
                            tt = None
                            for c in range(8):
                                pdst = (ptfa if c < 4 else ptfb)[:, c % 4, :]
                                tt = fw.op('pe', lambda e: e.matmul(pdst, xn[xb][:, c * 128:(c + 1) * 128], ident_f[:], start=True, stop=True),
                                           waits=[tn] + W_CST + ptf_free, inc=(s_p if c == 7 else None))
                            xn_free[xb] = [tt]
                            fw.op('dve', lambda e: e.tensor_copy(xnTf[xb][:, 0:4, :], ptfa[:]), waits=[tt] + xnTf_free[xb], inc=s_v)
                            te2 = fw.op('dve', lambda e: e.tensor_copy(xnTf[xb][:, 4:8, :], ptfb[:]), inc=s_v)
                            te1 = fw.op('pool', lambda e: e.tensor_copy(xnT[:, :, t * 128:(t + 1) * 128], xnTf[xb][:]), waits=[te2] + pass_free, inc=s_g)
                            ptf_free = [te2]
                            if KSTOP <= 3:
                                continue
                            tr = None
                            for c in range(8):
                                tr = fw.op('pe', lambda e: e.matmul(ps_r[:, 0:20], xnTf[xb][:, c, :], wr[:, c, :], start=(c == 0), stop=(c == 7)),
                                           waits=[te2, W_W] + psr_free, inc=(s_p if c == 7 else None))
                            xnTf_free[xb] = [tr, te1]
                            if KSTOP <= 4:
                                continue
                            R = rt[xb]
                            lg = R[:, 0:20]; gmax = R[:, 20:21]; ngmax = R[:, 21:22]; ge = R[:, 22:26]; gsum = R[:, 26:27]
                            gval = R[:, 27:28]; goh = R[:, 28:32]; pen = R[:, 32:36]; elm = R[:, 36:52]; m1 = R[:, 52:53]
                            oh1 = R[:, 53:69]; elm2 = R[:, 69:85]; m2 = R[:, 85:86]; dm = R[:, 86:87]; ex = R[:, 87:88]
                            den = R[:, 88:89]; e1 = R[:, 89:90]; e2 = R[:, 90:91]; w1 = R[:, 91:92]; w2 = R[:, 92:93]
                            oh2 = xn[xb][:, 0:16]
                            oh2 = R[:, 93:96]
                            V = lambda fn, w: fw.op('dve', fn, waits=w, inc=s_v)
                            A = lambda fn, w: fw.op('act', fn, waits=w, inc=s_a)
                            q = V(lambda e: e.tensor_tensor(out=lg, in0=ps_r[:, 0:20], in1=rb[:], op=ALU.add), [tr, W_W] + rt_free[xb])
                            psr_free = [q]
                            q = V(lambda e: e.memset(gsum, 0.0), [q])
                            q = V(lambda e: e.reduce_max(out=gmax, in_=lg[:, 0:4], axis=AX.X), [q])
                            q = V(lambda e: e.tensor_scalar(out=ngmax, in0=gmax, scalar1=-1.0, scalar2=None, op0=ALU.mult), [q])
                            qa = A(lambda e: e.activation(out=ge, in_=lg[:, 0:4], func=AF.Exp, bias=ngmax, scale=1.0, accum_out=gsum), [q])
                            q = V(lambda e: e.reciprocal(gval, gsum), [qa])
                            q = V(lambda e: e.tensor_scalar(out=goh, in0=lg[:, 0:4], scalar1=gmax, scalar2=None, op0=ALU.is_equal), [q])
                            q = V(lambda e: e.tensor_scalar(out=pen, in0=goh, scalar1=-1.0, scalar2=-NEGV, op0=ALU.add, op1=ALU.mult), [q])
                            q = V(lambda e: e.tensor_tensor(out=elm.rearrange("p (g k) -> p g k", k=4), in0=lg[:, 4:20].rearrange("p (g k) -> p g k", k=4),
                                                            in1=pen.unsqueeze(2).to_broadcast([128, 4, 4]), op=ALU.add), [q])
                            q = V(lambda e: e.reduce_max(out=m1, in_=elm, axis=AX.X), [q])
                            q = V(lambda e: e.tensor_scalar(out=oh1, in0=elm, scalar1=m1, scalar2=None, op0=ALU.is_equal), [q])
                            q = V(lambda e: e.scalar_tensor_tensor(out=elm2, in0=oh1, scalar=NEGV, in1=elm, op0=ALU.mult, op1=ALU.add), [q])
                            q = V(lambda e: e.reduce_max(out=m2, in_=elm2, axis=AX.X), [q])
                            q = V(lambda e: e.tensor_tensor(out=dm, in0=m2, in1=m1, op=ALU.subtract), [q])
                            qa = A(lambda e: e.activation(out=ex, in_=dm, func=AF.Exp), [q])
                            q = V(lambda e: e.tensor_scalar(out=den, in0=ex, scalar1=1.0, scalar2=None, op0=ALU.add), [qa])
                            q = V(lambda e: e.reciprocal(e1, den), [q])
                            q = V(lambda e: e.tensor_tensor(out=e2, in0=ex, in1=e1, op=ALU.mult), [q])
                            q = V(lambda e: e.tensor_tensor(out=w1, in0=e1, in1=gval, op=ALU.mult), [q])
                            q = V(lambda e: e.tensor_tensor(out=w2, in0=e2, in1=gval, op=ALU.mult), [q])
                            q = V(lambda e: e.tensor_scalar(out=elm, in0=elm2, scalar1=m2, scalar2=w2, op0=ALU.is_equal, op1=ALU.mult), [q])
                            q = V(lambda e: e.scalar_tensor_tensor(out=gates[:, t, :], in0=oh1, scalar=w1, in1=elm, op0=ALU.mult, op1=ALU.add),
                                  [q] + pass_free)
                            rt_free[xb] = [q]
                            last_gate = q
                        at_free[ab] = [t_last_op]
                    W_A = [last_gate, (s_a, s_a.n), (s_p, s_p.n), (s_g, s_g.n)]
                    for e_ in ('pe', 'act', 'dve', 'pool', 'sp'):
                        fw._waits(e_, W_A + [(s_v, s_v.n), (s_g, s_g.n)] + fw.dma_all())
                with ExitStack() as pb:
                    sbb = lambda name, shape, dt: pb.enter_context(nc.sbuf_tensor(f"{tag}b{pz}_" + name, shape, dt))
                    pmb = lambda name, shape, dt: pb.enter_context(nc.psum_tensor(f"{tag}bP{pz}_" + name, shape, dt))
                    wg = [sbb(f"wg{i}", [128, 8, DE], BF16) for i in range(2)]
                    wu = [sbb(f"wu{i}", [128, 8, DE], BF16) for i in range(2)]
                    wd = [sbb(f"wd{i}", [128, 4, D], BF16) for i in range(2)]
                    hidT = [sbb(f"hid{i}", [128, 4, 512], BF16) for i in range(2)]
                    sg = [sbb(f"sg{i}", [128, 512], F32) for i in range(2)]
                    ps_g = [pmb(f"g{i}", [128, 512], F32) for i in range(2)]
                    ps_u = [pmb(f"u{i}", [128, 512], F32) for i in range(2)]
                    ps_d = [pmb(f"d{i}", [128, 512], F32) for i in range(3)]
                    w_free = [[], []]
                    w_tok = {}

                    def load_w(e_):
                        b = e_ % 2
                        s_wp = fw.group('pool')
                        for c in range(8):
                            fw.dma('pool', wg[b][:, c, :], I["moe_w_gate"][layer, e_, c * 128:(c + 1) * 128, :], waits=w_free[b], inc=s_wp)
                            fw.dma('pool', wu[b][:, c, :], I["moe_w_up"][layer, e_, c * 128:(c + 1) * 128, :], waits=w_free[b], inc=s_wp)
                        for c in range(4):
                            fw.dma('pool', wd[b][:, c, :], I["moe_w_down"][layer, e_, c * 128:(c + 1) * 128, :], waits=w_free[b], inc=s_wp)
                        w_tok[e_] = s_wp.token()

                    g_free = [[], []]; u_free = [[], []]; d_free = [[], [], []]
                    hid_free = [[], []]; sg_free = [[], []]
                    cntb = dict(gu=0, d=0, u=0)
                    units = [(e_, blk) for e_ in range(int(os.environ.get('KNEXP', NE))) for blk in range(4)]
                    hid_ready = {}

                    def stageA(ui):
                        e_, blk = units[ui]
                        b = e_ % 2
                        hb = ui % 2
                        toks = []
                        for fc in range(4):
                            gb = cntb['gu'] % 2; cntb['gu'] += 1
                            tg = None
                            for c in range(8):
                                tg = fw.op('pe', lambda e: e.matmul(ps_g[gb][:], wg[b][:, c, fc * 128:(fc + 1) * 128], xnT[:, c, blk * 512:(blk + 1) * 512],
                                                                    start=(c == 0), stop=(c == 7)),
                                           waits=[w_tok[e_]] + g_free[gb], inc=(s_p if c == 7 else None))
                            tu = None
                            for c in range(8):
                                tu = fw.op('pe', lambda e: e.matmul(ps_u[gb][:], wu[b][:, c, fc * 128:(fc + 1) * 128], xnT[:, c, blk * 512:(blk + 1) * 512],
                                                                    start=(c == 0), stop=(c == 7)),
                                           waits=u_free[gb], inc=(s_p if c == 7 else None))
                            ts = fw.op('act', lambda e: e.activation(out=sg[gb][:], in_=ps_g[gb][:], func=AF.Silu),
                                       waits=[tg] + sg_free[gb], inc=s_a)
                            g_free[gb] = [ts]
                            th = fw.op('dve', lambda e: e.tensor_tensor(out=hidT[hb][:, fc, :], in0=sg[gb][:], in1=ps_u[gb][:], op=ALU.mult),
                                       waits=[ts, tu] + (hid_free[hb] if fc == 0 else []), inc=s_v)
                            u_free[gb] = [th]
                            sg_free[gb] = [th]
                            toks.append(th)
                        hid_ready[ui] = toks[-1]

                    def stageB(ui):
                        e_, blk = units[ui]
                        b = e_ % 2
                        hb = ui % 2
                        tm = None
                        for tt in range(4):
                            t = blk * 4 + tt
                            for dh in range(2):
                                db = cntb['d'] % 3; cntb['d'] += 1
                                for fc in range(4):
                                    tm = fw.op('pe', lambda e: e.matmul(ps_d[db][:], hidT[hb][:, fc, tt * 128:(tt + 1) * 128],
                                                                        wd[b][:, fc, dh * 512:(dh + 1) * 512], start=(fc == 0), stop=(fc == 3)),
                                               waits=[hid_ready[ui]] + d_free[db], inc=(s_p if fc == 3 else None))
                                ta = fw.op('dve', lambda e: e.scalar_tensor_tensor(out=acc[:, t, dh * 512:(dh + 1) * 512], in0=ps_d[db][:],
                                                                                    scalar=gates[:, t, e_:e_ + 1], in1=acc[:, t, dh * 512:(dh + 1) * 512],
                                                                                    op0=ALU.mult, op1=ALU.add),
                                           waits=[tm], inc=s_v)
                                d_free[db] = [ta]
                        hid_free[hb] = [tm]
                        if blk == 3:
                            w_free[b] = [tm]
                            if e_ + 2 < len(units) // 4:
                                load_w(e_ + 2)

                    if len(units) > 0:
                        load_w(0)
                    if len(units) > 4:
                        load_w(1)
                    for ui in range(len(units) + 1):
                        if ui < len(units):
                            stageA(ui)
                        if ui - 1 >= 0:
                            stageB(ui - 1)
                    t_fin = (s_v, s_v.n)
                    td = fw.dma('sp', outp.rearrange("(t p) d -> p t d", p=128), acc[:], waits=[t_fin], inc=s_st)
                    pass_free = [td, (s_p, s_p.n)]
                    for e_ in ('pe', 'act', 'dve', 'pool', 'sp'):
                        fw._waits(e_, [td, (s_p, s_p.n), (s_a, s_a.n), (s_v, s_v.n)] + fw.dma_all())
            return pass_free[0]


    def proj_phase(tag, src, N, norm_ap, w_ap, WC, gain_aps, njobs, vgroups):
        NBLK = N // 512
        with ExitStack() as ps:
            sb = lambda name, shape, dt: ps.enter_context(nc.sbuf_tensor(f"{tag}_" + name, shape, dt))
            pm = lambda name, shape, dt: ps.enter_context(nc.psum_tensor(f"{tag}P_" + name, shape, dt))
            w_sb = sb("w", [128, 8, WC], BF16)
            g_bc = sb("g_bc", [128, D], F32)
            gq = sb("gq", [128, max(1, len(gain_aps))], F32)
            xs = [sb(f"xs{i}", [128, D], F32) for i in range(2)]
            junk = sb("junk", [128, D], BF16)
            xn = [sb(f"xn{i}", [128, D], BF16) for i in range(2)]
            xnT = [sb(f"xnT{i}", [128, 8, 512], BF16) for i in range(2)]
            st = sb("st", [128, 3 * (N // 128)], F32)
            sqk = [sb(f"sqk{i}", [128, 512], BF16) for i in range(2)]
            lnv = [sb(f"lnv{i}", [128, 512], F32) for i in range(2)]
            rstd = [sb(f"rstd{i}", [128, 512], F32) for i in range(2)]
            kst = [sb(f"kst{i}", [128, 512], BF16) for i in range(3)]
            NG = len(vgroups)
            vst = [sb(f"vst{i}", [128, max(1, NG), 4, 8, 65], BF16) for i in range(2)] if NG else None
            psA = [pm(f"psA{i}", [128, 512], F32) for i in range(5)]
            ps2 = [pm(f"ps2{i}", [128, 512], F32) for i in range(2)]
            pt = pm("pt0", [128, 8, 128], BF16)
            s_a = fw.sem(tag + "a"); s_v = fw.sem(tag + "v"); s_p = fw.sem(tag + "p"); s_g = fw.sem(tag + "g")
            gwp = fw.group('pool')
            for c in range(8):
                fw.dma('pool', w_sb[:, c, :], w_ap[c * 128:(c + 1) * 128, :], inc=gwp)
            gws = fw.group('sp')
            fw.dma('sp', g_bc[:], norm_ap.partition_broadcast(128), inc=gws)
            for gi, (gap, gscale) in enumerate(gain_aps):
                for hh in range(2):
                    fw.dma('sp', gq[hh * 64:(hh + 1) * 64, gi:gi + 1], gap.rearrange("(d o) -> d o", o=1), inc=gws)
            W_W = [gwp.token(), gws.token()]
            t = fw.op('pool', lambda e: e.memset(st[:], 0.0), inc=s_g)
            if NG:
                for i in range(2):
                    t = fw.op('pool', lambda e: e.memset(vst[i][:], 1.0), inc=s_g)
            W_INIT = t
            t_gq = None
            for gi, (gap, gscale) in enumerate(gain_aps):
                if gscale != 1.0:
                    t_gq = fw.op('dve', lambda e: e.tensor_scalar(out=gq[:, gi:gi + 1], in0=gq[:, gi:gi + 1], scalar1=gscale, scalar2=None,
                                                                    op0=ALU.mult), waits=[W_W], inc=s_v)
            xs_free = [[], []]; xn_free = [[], []]; pt_free = [[]]; xnT_free = [[], []]
            psA_free = [[] for _ in range(5)]; ps2_free = [[], []]; sqk_free = [[], []]; lnv_free = [[], []]; rstd_free = [[], []]
            kst_free = [[], [], []]; vst_free = [[], []]
            cnt = dict(a=0, k=0, j=0, ks=0)

            def tile_front(tb, ti):
                tg = tb * 4 + ti
                b = tg % 2; bb = tb % 2
                tx = fw.dma('sp', xs[b][:], src[tg * 128:(tg + 1) * 128, :], waits=xs_free[b])
                t3 = rms_stats(xs[b][:], junk[:], st, 3 * tg, D, [tx, W_INIT], s_a)
                tn = fw.op('dve', lambda e: e.scalar_tensor_tensor(out=xn[b][:], in0=xs[b][:], scalar=st[:, 3 * tg + 2:3 * tg + 3],
                                                                    in1=g_bc[:], op0=ALU.mult, op1=ALU.mult),
                           waits=[t3, W_W] + xn_free[b], inc=s_v)
                xs_free[b] = [tn]
                tt = None
                for c in range(8):
                    tt = fw.op('pe', lambda e: e.transpose(pt[:, c, :], xn[b][:, c * 128:(c + 1) * 128], ident_bf[:]),
                               waits=[tn] + W_CST + pt_free[0], inc=(s_p if c == 7 else None))
                xn_free[b] = [tt]
                te = fw.op('dve', lambda e: e.tensor_copy(xnT[bb][:, :, ti * 128:(ti + 1) * 128], pt[:]),
                           waits=[tt] + xnT_free[bb], inc=s_v)
                pt_free[0] = [te]
                return te

            for tb in range(NBLK):
                bb = tb % 2
                t_xnT = None
                for ti in range(4):
                    t_xnT = tile_front(tb, ti)
                vb = tb % 2
                jobs = [("n",) + j for j in njobs]
                for ti in range(4):
                    for gi, vcol in enumerate(vgroups):
                        jobs.append(("v", ti, gi, vcol))

                def stage1(job):
                    a = cnt['a'] % 5; cnt['a'] += 1
                    if job[0] == "n":
                        _, col0, gcol, dst = job
                        tm = None
                        for c in range(8):
                            tm = fw.op('pe', lambda e: e.matmul(psA[a][:], w_sb[:, c, col0:col0 + 128], xnT[bb][:, c, :],
                                                                start=(c == 0), stop=(c == 7)),
                                       waits=[t_xnT, W_W] + psA_free[a], inc=(s_p if c == 7 else None))
                        k2 = cnt['k'] % 2; cnt['k'] += 1
                        tsq = fw.op('act', lambda e: e.activation(out=sqk[k2][:], in_=psA[a][:], func=AF.Square),
                                    waits=[tm] + sqk_free[k2], inc=s_a)
                        return ("norm", gcol, dst, a, k2, tm, tsq)
                    _, ti, gi, vcol = job
                    tm = None
                    for c in range(8):
                        tm = fw.op('pe', lambda e: e.matmul(psA[a][:], xnT[bb][:, c, ti * 128:(ti + 1) * 128], w_sb[:, c, vcol:vcol + 512],
                                                            start=(c == 0), stop=(c == 7)),
                                   waits=[t_xnT, W_W] + psA_free[a], inc=(s_p if c == 7 else None))
                    tev = fw.op('act', lambda e: e.copy(vst[vb][:, gi, ti, :, 0:64], psA[a][:].rearrange("p (h d) -> p h d", d=64)),
                                waits=[tm, W_INIT] + vst_free[vb], inc=s_a)
                    psA_free[a] = [tev]
                    return None

                def stage2(rec):
                    _, gcol, dst, a, k2, tm, tsq = rec
                    j = cnt['j'] % 2; cnt['j'] += 1
                    t2 = fw.op('pe', lambda e: e.matmul(ps2[j][:], blk_bf[:], sqk[k2][:], start=True, stop=True),
                               waits=[tsq] + W_CST + ps2_free[j], inc=s_p)
                    sqk_free[k2] = [t2]
                    tl = fw.op('act', lambda e: e.activation(out=lnv[j][:], in_=ps2[j][:], func=AF.Ln, scale=1.0 / 64, bias=EPS),
                               waits=[t2] + lnv_free[j], inc=s_a)
                    ps2_free[j] = [tl]
                    tr = fw.op('act', lambda e: e.activation(out=rstd[j][:], in_=lnv[j][:], func=AF.Exp, scale=-0.5),
                               waits=[tl] + rstd_free[j], inc=s_a)
                    lnv_free[j] = [tr]
                    ks = cnt['ks'] % 3; cnt['ks'] += 1
                    tk = fw.op('dve', lambda e: e.scalar_tensor_tensor(out=kst[ks][:], in0=psA[a][:], scalar=gq[:, gcol:gcol + 1],
                                                                        in1=rstd[j][:], op0=ALU.mult, op1=ALU.mult),
                               waits=[tr, t_gq, tm] + kst_free[ks], inc=s_v)
                    psA_free[a] = [tk]
                    rstd_free[j] = [tk]
                    td = fw.dma('pool', dst[:, tb * 512:(tb + 1) * 512], kst[ks][:], waits=[tk])
                    kst_free[ks] = [td]

                prev = None
                for job in jobs:
                    rec = stage1(job)
                    if prev is not None:
                        stage2(prev)
                    prev = rec
                if prev is not None:
                    stage2(prev)
                if NG:
                    tv = (s_a, s_a.n)
                    td = None
                    for gi in range(NG):
                        td = fw.dma('pool', VB[gi, tb * 512:(tb + 1) * 512, :].rearrange("(t p) c -> p t c", p=128),
                                    vst[vb][:, gi, :, :, :].rearrange("p t h c -> p t (h c)"), waits=[tv])
                    vst_free[vb] = [td] + fw.dma_all()
                xnT_free[bb] = [(s_p, s_p.n)]
            done = fw.dma_all()
            for e in ('pe', 'act', 'dve', 'pool', 'sp'):
                fw._waits(e, [done, (s_p, s_p.n), (s_a, s_a.n), (s_v, s_v.n), (s_g, s_g.n)])

    def phase5():
        njobs = [(ch * 128, 0, KB[ch * 128:(ch + 1) * 128, :]) for ch in range(12)]
        proj_phase("p5", H1, NQ, I["kv_norm"], I["kv_w"], 3072, [(I["kv_k_gain"], 1.0)], njobs, [1536, 2048, 2560])

    def phase5b():
        for bi, (qs, pv) in enumerate(cfg.BS):
            njobs = [(ch * 128, 0, QB[ch * 128:(ch + 1) * 128, bi * SPAN:(bi + 1) * SPAN]) for ch in range(12)]
            proj_phase(f"p5b{bi}", H1[qs * SPAN:(qs + 1) * SPAN, :], SPAN, I["b_norm"][0] if False else I["b_norm"], I["b_w_q"], 1536,
                       [(I["b_q_gain"], SCALE)], njobs, [])

    def phase6():
        with ExitStack() as ps:
            sb = lambda name, shape, dt: ps.enter_context(nc.sbuf_tensor("p6_" + name, shape, dt))
            pm = lambda name, shape, dt: ps.enter_context(nc.psum_tensor("p6P_" + name, shape, dt))
            expB = sb("expB", [128, 3, 8, 2, 128], BF16)
            kbias = sb("kbias", [128, len(cfg.QS)], F32)
            ps_s = [pm(f"s{i}", [128, 512], F32) for i in range(4)]
            ps_o = [pm(f"o{i}", [65, 512], F32) for i in range(2)]
            ps_bc = pm("bc", [128, 512], F32)
            s_a = fw.sem("p6a"); s_v = fw.sem("p6v"); s_p = fw.sem("p6p"); s_g = fw.sem("p6g")

            with ExitStack() as pb:
                sbb = lambda name, shape, dt: pb.enter_context(nc.sbuf_tensor("p6b_" + name, shape, dt))
                biasT = sbb("biasT", [128, 3, 8, 2, 128], F32)
                rb_sb = sbb("rb", [32, 24], F32)
                oh_sb = sbb("oh", [32, 3, 384], F32)
                ng_sb = sbb("ng", [1, 3, 384], F32)
                anti = sbb("anti", [128, 128], F32)
                fvs = sbb("fvs", [24, 3, 384], F32)
                hank = [sbb(f"hank{i}", [128, 128], F32) for i in range(2)]
                g0 = fw.group('sp')
                fw.dma('sp', rb_sb[:], I["rel_bias"][:, :], inc=g0)
                fw.dma('sp', oh_sb[:], I["c_oh"].rearrange("g k m -> k g m"), inc=g0)
                fw.dma('sp', ng_sb[:], I["c_ng"].rearrange("g o m -> o g m"), inc=g0)
                fw.dma('sp', anti[:], I["c_anti"][:, :], inc=g0)
                if cfg.masks:
                    fw.dma('sp', kbias[:], I["c_kbias"].partition_broadcast(128), inc=g0)
                tk0 = g0.token()
                tprev = []
                tds = []
                for g in range(3):
                    fw.op('pe', lambda e: e.matmul(ps_bc[0:24, 0:384], rb_sb[:], oh_sb[:, g, :], start=True, stop=False),
                          waits=[tk0] + W_CST + tprev)
                    tm = fw.op('pe', lambda e: e.matmul(ps_bc[0:24, 0:384], ones_f[0:1, 0:24], ng_sb[0:1, g, :], start=False, stop=True), inc=s_p)
                    tc = fw.op('dve', lambda e: e.tensor_copy(fvs[:, g, :], ps_bc[0:24, 0:384]), waits=[tm], inc=s_v)
                    tprev = [tc]
                    tds.append(fw.dma('sp', FV[g * 8:(g + 1) * 8, :], fvs[g * 8:(g + 1) * 8, g, :], waits=[tc]))
                hk_free = [[], []]
                ci = 0
                for g in range(3):
                    for h in range(8):
                        for pc in range(2):
                            hb = ci % 2; ci += 1
                            head = g * 8 + h
                            off = head * 384 + (128 if pc == 0 else 0)
                            srcap = bass.AP(FV.tensor, off, [[1, 128], [1, 128]])
                            tl = fw.dma('sp', hank[hb][:], srcap, waits=tds + hk_free[hb])
                            tm = fw.op('pe', lambda e: e.matmul(ps_bc[:, 0:128], anti[:], hank[hb][:], start=True, stop=True),
                                       waits=[tl] + tprev, inc=s_p)
                            hk_free[hb] = [tm]
                            tc = fw.op('dve', lambda e: e.tensor_copy(biasT[:, g, h, pc, :], ps_bc[:, 0:128]), waits=[tm], inc=s_v)
                            tprev = [tc]
                t_eb = fw.op('act', lambda e: e.activation(out=expB[:].rearrange("p g h c a -> p (g h c a)"),
                                                             in_=biasT[:].rearrange("p g h c a -> p (g h c a)"), func=AF.Exp), waits=tprev, inc=s_a)
                W_BIAS = [t_eb]
                for e in ('pe', 'act', 'dve', 'pool', 'sp'):
                    fw._waits(e, W_BIAS + [(s_p, s_p.n)] + fw.dma_all())

            k_sb = sb("k", [128, 4, 2 * SPAN], BF16)
            q_sb = sb("q", [128, 4, SPAN], BF16)
            v_sb = sb("v", [128, 2, 16, 520], BF16)
            oacc = sb("oacc", [65, 8, SPAN], F32)
            p_sb = [sb(f"p{i}", [128, 512], BF16) for i in range(4)]
            rc = [sb(f"rc{i}", [65, 512], F32) for i in range(2)]
            ost = [sb(f"ost{i}", [64, 512], BF16) for i in range(2)]
            ld_free = []
            oacc_free = []
            st_s = dict(a=0, o=0)
            psS_free = [[], [], [], []]
            pS_free = [[], [], [], []]
            pso_free = [[], []]
            bc_free = list(W_BIAS)
            rc_free = [[], []]; ost_free = [[], []]
            for bi, (qs, pv) in enumerate(cfg.BS):
                last_acc_tok = None
                for g, d in enumerate(B_DIL):
                    gl = fw.group('sp')
                    for c in range(4):
                        row0 = (4 * g + c) * 128
                        if pv is not None:
                            fw.dma('sp', k_sb[:, c, 0:SPAN], KB[row0:row0 + 128, pv * SPAN:(pv + 1) * SPAN], waits=ld_free, inc=gl)
                        fw.dma('sp', k_sb[:, c, SPAN:2 * SPAN], KB[row0:row0 + 128, qs * SPAN:(qs + 1) * SPAN], waits=ld_free, inc=gl)
                        fw.dma('sp', q_sb[:, c, :], QB[row0:row0 + 128, bi * SPAN:(bi + 1) * SPAN], waits=ld_free, inc=gl)

                    def vsrc(slot):
                        rows = VB[g, slot * SPAN:(slot + 1) * SPAN, :]
                        if d == 1:
                            return rows.rearrange("(b a) c -> a b c", a=128)
                        if d == 4:
                            return rows.rearrange("(m a r) c -> a m r c", m=4, a=128, r=4)
                        return rows.rearrange("(a r) c -> a r c", r=16)

                    def vdst(pc):
                        if d == 4:
                            return v_sb[:, pc, :, :].rearrange("a (m r) c -> a m r c", r=4)
                        return v_sb[:, pc, :, :]
                    if pv is not None:
                        fw.dma('sp', vdst(0), vsrc(pv), waits=ld_free, inc=gl)
                    fw.dma('sp', vdst(1), vsrc(qs), waits=ld_free, inc=gl)
                    W_LD = [gl.token()]

                    def blk_start(beta):
                        if d == 1:
                            return beta * 128
                        if d == 4:
                            return (beta // 4) * 512 + (beta % 4)
                        return beta

                    def prev_info(beta):
                        st0 = blk_start(beta) - 128 * d
                        if st0 >= 0:
                            return (1, beta - (1 if d == 1 else 4))
                        if pv is None:
                            return None
                        return (0, 15 if d == 1 else (12 + beta % 4 if d == 4 else beta))

                    units = [(h, qd) for h in range(8) for qd in range(4)]
                    urec = {}

                    def stageA(ui):
                        h, qd = units[ui]
                        c = h // 2; hh = h % 2
                        prs = [prev_info(4 * qd + i) for i in range(4)]
                        npv = sum(1 for p_ in prs if p_ is None)
                        c0 = 128 * npv
                        a0 = st_s['a'] % 4; a1 = (st_s['a'] + 1) % 4; st_s['a'] += 2
                        tp = None
                        for i in range(4):
                            if prs[i] is None:
                                continue
                            s0 = blk_start(4 * qd + i)
                            kcols = slice(SPAN + s0 - 128 * d, SPAN + s0 - 128 * d + 127 * d + 1, d)
                            qcols = slice(s0, s0 + 127 * d + 1, d)
                            tp = fw.op('pe', lambda e: e.matmul(ps_s[a0][:, i * 128:(i + 1) * 128], k_sb[hh * 64:(hh + 1) * 64, c, kcols],
                                                                q_sb[hh * 64:(hh + 1) * 64, c, qcols], start=True, stop=True),
                                       waits=W_LD + psS_free[a0], inc=s_p)
                        tcur = None
                        for i in range(4):
                            s0 = blk_start(4 * qd + i)
                            kcols = slice(SPAN + s0, SPAN + s0 + 127 * d + 1, d)
                            qcols = slice(s0, s0 + 127 * d + 1, d)
                            tcur = fw.op('pe', lambda e: e.matmul(ps_s[a1][:, i * 128:(i + 1) * 128], k_sb[hh * 64:(hh + 1) * 64, c, kcols],
                                                                  q_sb[hh * 64:(hh + 1) * 64, c, qcols], start=True, stop=True),
                                         waits=W_LD + psS_free[a1], inc=s_p)
                        tE0 = None
                        if tp is not None:
                            nb_ = 4 - npv
                            te0 = None
                            i0 = npv
                            while i0 < 4:
                                i1 = i0
                                while i1 < 4 and (prs[i1][0] == 0) == (prs[i0][0] == 0):
                                    i1 += 1
                                use_kb = cfg.masks and prs[i0][0] == 0
                                bias_arg = kbias[:, pv:pv + 1] if use_kb else 0.0
                                te0 = fw.op('act', lambda e: e.activation(out=p_sb[a0][:, i0 * 128:i1 * 128], in_=ps_s[a0][:, i0 * 128:i1 * 128],
                                                                          func=AF.Exp, bias=bias_arg, scale=1.0),
                                            waits=[tp, tk0] + pS_free[a0], inc=s_a)
                                i0 = i1
                            psS_free[a0] = [te0]
                            tE0 = fw.op('dve', lambda e: e.tensor_tensor(out=p_sb[a0][:, c0:512].rearrange("p (i a) -> p i a", a=128),
                                                                          in0=p_sb[a0][:, c0:512].rearrange("p (i a) -> p i a", a=128),
                                                                          in1=expB[:, g, h, 0, :].unsqueeze(1).to_broadcast([128, nb_, 128]), op=ALU.mult),
                                        waits=[te0] + W_BIAS, inc=s_v)
                        te1 = fw.op('act', lambda e: e.activation(out=p_sb[a1][:], in_=ps_s[a1][:], func=AF.Exp),
                                    waits=[tcur] + pS_free[a1], inc=s_a)
                        psS_free[a1] = [te1]
                        tE1 = fw.op('dve', lambda e: e.tensor_tensor(out=p_sb[a1][:].rearrange("p (i a) -> p i a", a=128),
                                                                      in0=p_sb[a1][:].rearrange("p (i a) -> p i a", a=128),
                                                                      in1=expB[:, g, h, 1, :].unsqueeze(1).to_broadcast([128, 4, 128]), op=ALU.mult),
                                    waits=[te1] + W_BIAS, inc=s_v)
                        urec[ui] = (prs, a0, a1, tE0, tE1)

                    def stageB(ui):
                        nonlocal last_acc_tok
                        h, qd = units[ui]
                        prs, a0, a1, tE0, tE1 = urec.pop(ui)
                        ob = st_s['o'] % 2; st_s['o'] += 1
                        tm = None
                        for i in range(4):
                            beta = 4 * qd + i
                            if prs[i] is not None:
                                vbuf, vblk = prs[i]
                                fw.op('pe', lambda e: e.matmul(ps_o[ob][:, i * 128:(i + 1) * 128], v_sb[:, vbuf, vblk, h * 65:(h + 1) * 65],
                                                               p_sb[a0][:, i * 128:(i + 1) * 128], start=True, stop=False),
                                      waits=[tE0, tE1] + pso_free[ob])
                            tm = fw.op('pe', lambda e: e.matmul(ps_o[ob][:, i * 128:(i + 1) * 128], v_sb[:, 1, beta, h * 65:(h + 1) * 65],
                                                                p_sb[a1][:, i * 128:(i + 1) * 128], start=(prs[i] is None), stop=True),
                                       waits=[tE0, tE1] + pso_free[ob], inc=(s_p if i == 3 else None))
                        pS_free[a0] = [tm]; pS_free[a1] = [tm]
                        if d == 1:
                            dst = oacc[0:65, h, qd * 512:(qd + 1) * 512].rearrange("p (i a) -> p i a", a=128)
                        elif d == 4:
                            dst = oacc[0:65, h, qd * 512:(qd + 1) * 512].rearrange("p (a r) -> p r a", r=4)
                        else:
                            dst = oacc[0:65, h, :].rearrange("p (a r) -> p r a", r=16)[:, 4 * qd:4 * qd + 4, :]
                        srcp = ps_o[ob][:].rearrange("p (i a) -> p i a", a=128)
                        if g == 0:
                            ta = fw.op('dve', lambda e: e.tensor_copy(dst, srcp), waits=[tm] + oacc_free, inc=s_v)
                        else:
                            ta = fw.op('dve', lambda e: e.tensor_tensor(out=dst, in0=dst, in1=srcp, op=ALU.add), waits=[tm, last_acc_tok], inc=s_v)
                        last_acc_tok = ta
                        pso_free[ob] = [ta]

                    for ui in range(len(units) + 1):
                        if ui < len(units):
                            stageA(ui)
                        if ui >= 1:
                            stageB(ui - 1)
                    ld_free = [(s_p, s_p.n)]
                t_last = None
                ei = 0
                for h in range(8):
                    for cb in range(4):
                        eb = ei % 2; ei += 1
                        cols = slice(cb * 512, (cb + 1) * 512)
                        t1 = fw.op('dve', lambda e: e.reciprocal(rc[eb][64:65, :], oacc[64:65, h, cols]), waits=[last_acc_tok] + rc_free[eb], inc=s_v)
                        t2 = fw.op('pe', lambda e: e.matmul(ps_bc[0:64, :], ones_f[64:65, 0:64], rc[eb][64:65, :], start=True, stop=True),
                                   waits=[t1] + W_CST + bc_free, inc=s_p)
                        rc_free[eb] = [t2]
                        t3 = fw.op('dve', lambda e: e.tensor_tensor(out=ost[eb][:], in0=oacc[0:64, h, cols], in1=ps_bc[0:64, :], op=ALU.mult),
                                   waits=[t2] + ost_free[eb], inc=s_v)
                        bc_free = [t3]
                        t4 = fw.dma('pool', AT1[h * 64:(h + 1) * 64, bi * SPAN + cb * 512: bi * SPAN + (cb + 1) * 512], ost[eb][:], waits=[t3])
                        ost_free[eb] = [t4]
                        t_last = t3
                oacc_free = [t_last]
            done = fw.dma_all()
            for e in ('pe', 'act', 'dve', 'pool', 'sp'):
                fw._waits(e, [done, (s_p, s_p.n), (s_a, s_a.n), (s_v, s_v.n), (s_g, s_g.n)])


    def moe_sparse(layer, N, resid_rows_fn, AT, w_out_name, nch, out_rows_fn, tag, wc_tok):
        NTT = N // 128
        G = 2
        SUP = 128 * G
        STOT = 2 * N + NE * SUP
        NTL = STOT // 128
        NSUP = STOT // SUP
        DUM = 2 * N
        OOBV = DUM
        XN = scratch(tag + "XN", [2 * N + 128, D], BF16)
        HM = scratch(tag + "HM", [N, D], F32)
        OUT2 = scratch(tag + "OUT2", [2 * N + 128, D], F32)
        TOKIDX = scratch(tag + "TOKIDX", [STOT, 1], mybir.dt.int32)
        I32 = mybir.dt.int32
        with ExitStack() as ps:
            sb = lambda name, shape, dt: ps.enter_context(nc.sbuf_tensor(f"{tag}_" + name, shape, dt))
            OH = sb("OH", [128, NTT, 2, 16], F32)
            OHb = sb("OHb", [128, NTT, 2, 16], BF16)
            W12 = sb("W12", [128, NTT, 2], F32)
            RANK = sb("RANK", [128, NTT, 2], F32)
            CNT = sb("CNT", [128, 16], F32)
            SLOTI = sb("SLOTI", [128, NTT, 2], I32)
            WIDX = sb("WIDX", [128, NSUP], I32)
            TOKID = sb("TOKID", [128, 2, NTT], I32)
            stri = sb("stri", [128, 128], BF16)
            ones_b = sb("ones_b", [128, 128], BF16)
            s_a = fw.sem(tag + "a"); s_v = fw.sem(tag + "v"); s_p = fw.sem(tag + "p"); s_g = fw.sem(tag + "g")
            gk = fw.group('pool')
            fw.dma('pool', stri[:], I["c_stri"][:, :], inc=gk)
            t_ob = fw.op('pool', lambda e: e.memset(ones_b[:], 1.0), inc=s_g)
            t_c0 = fw.op('pool', lambda e: e.memset(CNT[:], 0.0), inc=s_g)
            fw.op('pool', lambda e: e.iota(TOKID[:, 0, :], pattern=[[128, NTT]], base=0, channel_multiplier=1), inc=s_g)
            t_tid = fw.op('pool', lambda e: e.iota(TOKID[:, 1, :], pattern=[[128, NTT]], base=N, channel_multiplier=1), inc=s_g)
            W_K = [gk.token(), t_ob, t_c0]
            with ExitStack() as pa:
                sba = lambda name, shape, dt: pa.enter_context(nc.sbuf_tensor(f"{tag}a_" + name, shape, dt))
                pma = lambda name, shape, dt: pa.enter_context(nc.psum_tensor(f"{tag}aP_" + name, shape, dt))
                w_out = sba("w_out", [128, nch, D], BF16)
                at_sb = [sba(f"at{i}", [128, nch, 512], BF16) for i in range(2)]
                g_bc = sba("g_bc", [128, D], F32)
                hb_ = [sba(f"h{i}", [128, 4, D], F32) for i in range(2)]
                xn = [sba(f"xn{i}", [128, D], F32) for i in range(2)]
                xnb = [sba(f"xnb{i}", [128, D], BF16) for i in range(2)]
                junk = sba("junk", [128, D], BF16)
                xnTf = [sba(f"xnTf{i}", [128, 8, 128], F32) for i in range(2)]
                wr = sba("wr", [128, 8, 20], F32)
                rb = sba("rb", [128, 20], F32)
                st = sba("st", [128, 3 * NTT], F32)
                rt = [sba(f"rt{i}", [128, 640], F32) for i in range(2)]
                ps_op = [pma(f"op{i}", [128, 512], F32) for i in range(2)]
                ptfa = pma("ptfa", [128, 4, 128], F32)
                ptfb = pma("ptfb", [128, 4, 128], F32)
                ps_r = [pma(f"r{i}", [128, 512], F32) for i in range(2)]
                ps_k = pma("k", [128, 512], F32)

                s_w = fw.group('sp'); s_wp = fw.group('pool')
                for c in range(nch):
                    fw.dma('pool', w_out[:, c, :], I[w_out_name][c * 128:(c + 1) * 128, :], inc=s_wp)
                fw.dma('sp', g_bc[:], I["ffn_norm"][layer, :].partition_broadcast(128), inc=s_w)
                fw.dma('sp', wr[:, :, 0:4], I["moe_w_group"][layer].rearrange("(c p) g -> p c g", p=128), inc=s_w)
                fw.dma('sp', wr[:, :, 4:20], I["moe_w_expert"][layer].rearrange("(c p) g -> p c g", p=128), inc=s_w)
                fw.dma('sp', rb[:, 0:4], I["moe_b_group"][layer, :].partition_broadcast(128), inc=s_w)
                fw.dma('sp', rb[:, 4:20], I["moe_b_expert"][layer, :].partition_broadcast(128), inc=s_w)
                W_W = [s_w.token(), s_wp.token()]
                W_INIT = fw.op('pool', lambda e: e.memset(st[:], 0.0), inc=s_g)
                at_free = [[], []]; op_free = [[], []]; xn_free = [[], []]; ptf_free = []; xnTf_free = [[], []]
                psr_free = [[], []]; rt_free = [[], []]; h_free = [[], []]; xnb_free = [[], []]; psk_free = []
                last_cnt = t_c0
                pending_rank = []
                pipe = []
                stage_f2 = []
                rblocks = {}

                def advance_pipe(flush):
                    while stage_f2 and (flush or len(stage_f2) > 1 or True):
                        it = stage_f2.pop(0)
                        tr_ = it['F3'](it['te2'])
                        if it['ti'] == 3:
                            pending_rank.append((it['blk'], tr_))
                        break
                    while pipe and (flush or len(pipe) > 1):
                        it = pipe.pop(0)
                        it['te2'] = it['F2']()
                        stage_f2.append(it)
                        if not flush:
                            break
                    if flush:
                        while stage_f2:
                            it = stage_f2.pop(0)
                            tr_ = it['F3'](it['te2'])
                            if it['ti'] == 3:
                                pending_rank.append((it['blk'], tr_))

                def run_router_blocks(flush):
                    nonlocal rank_fn
                    while pending_rank and pending_rank[0][0] in rblocks:
                        bk, tr_ = pending_rank.pop(0)
                        fn = rblocks.pop(bk)(tr_)
                        if rank_fn is not None:
                            rank_fn()
                        rank_fn = fn
                    if flush and rank_fn is not None:
                        rank_fn()
                        rank_fn = None
                rank_fn = None
                for blk in range(N // 512):
                    ab = blk % 2
                    t_at = fw.dma('sp', at_sb[ab][:], AT[:, blk * 512:(blk + 1) * 512].rearrange("(c p) n -> p c n", p=128),
                                  waits=at_free[ab])
                    t_rs = fw.dma('sp', hb_[ab][:], resid_rows_fn(blk).rearrange("(t p) d -> p t d", p=128), waits=h_free[ab])
                    t_last_op = None
                    t_hdone = []
                    for ti in range(4):
                        t = blk * 4 + ti
                        xb = t % 2
                        hT = hb_[ab][:, ti, :]
                        tadd = None
                        for half in range(2):
                            tm = None
                            for c in range(nch):
                                tm = fw.op('pe', lambda e: e.matmul(ps_op[half][:], at_sb[ab][:, c, ti * 128:(ti + 1) * 128],
                                                                    w_out[:, c, half * 512:(half + 1) * 512],
                                                                    start=(c == 0), stop=(c == nch - 1)),
                                           waits=[t_at, W_W] + op_free[half], inc=(s_p if c == nch - 1 else None))
                            t_last_op = tm
                            tadd = fw.op('dve', lambda e: e.tensor_tensor(out=hT[:, half * 512:(half + 1) * 512],
                                                                           in0=hT[:, half * 512:(half + 1) * 512],
                                                                           in1=ps_op[half][:], op=ALU.add),
                                         waits=[tm, t_rs], inc=s_v)
                            op_free[half] = [tadd]
                        t3 = rms_stats(hT, junk[:], st, 3 * t, D, [tadd, W_INIT], s_a)
                        tn = fw.op('dve', lambda e: e.scalar_tensor_tensor(out=xn[xb][:], in0=hT, scalar=st[:, 3 * t + 2:3 * t + 3],
                                                                            in1=g_bc[:], op0=ALU.mult, op1=ALU.mult),
                                   waits=[t3, W_W] + xn_free[xb], inc=s_v)
                        t_hdone.append(tn)
                        tcb = fw.op('pool', lambda e: e.tensor_copy(xnb[xb][:], xn[xb][:]), waits=[tn] + xnb_free[xb], inc=s_g)
                        txs = fw.dma('sp', XN[t * 128:(t + 1) * 128, :], xnb[xb][:], waits=[tcb])
                        txs2 = fw.dma('sp', XN[N + t * 128:N + (t + 1) * 128, :], xnb[xb][:], waits=[tcb])
                        xnb_free[xb] = [txs, txs2]
                        def F2(t=t, xb=xb, tn=tn, tcb=tcb):
                            nonlocal ptf_free
                            tt = None
                            for c in range(8):
                                pdst = (ptfa if c < 4 else ptfb)[:, c % 4, :]
                                tt = fw.op('pe', lambda e: e.matmul(pdst, xn[xb][:, c * 128:(c + 1) * 128], ident_f[:], start=True, stop=True),
                                           waits=[tn] + W_CST + ptf_free, inc=(s_p if c == 7 else None))
                            xn_free[xb] = [tt, tcb]
                            fw.op('dve', lambda e: e.tensor_copy(xnTf[xb][:, 0:4, :], ptfa[:]), waits=[tt] + xnTf_free[xb], inc=s_v)
                            te2 = fw.op('dve', lambda e: e.tensor_copy(xnTf[xb][:, 4:8, :], ptfb[:]), inc=s_v)
                            ptf_free = [te2]
                            return te2

                        def F3(te2, t=t, xb=xb, blk=blk, ti=ti):
                            tr = None
                            for c in range(8):
                                tr = fw.op('pe', lambda e: e.matmul(ps_r[blk % 2][:, ti * 20:(ti + 1) * 20], xnTf[xb][:, c, :], wr[:, c, :], start=(c == 0), stop=(c == 7)),
                                           waits=[te2, W_W] + psr_free[blk % 2], inc=(s_p if c == 7 else None))
                            xnTf_free[xb] = [tr]
                            return tr
                        pipe.append(dict(F2=F2, F3=F3, blk=blk, ti=ti))
                        advance_pipe(False)
                    def router_block(tr_last, blk=blk, ab=ab):
                        nonlocal last_cnt, psk_free
                        rb_i = blk % 2
                        R = rt[rb_i]
                        t0 = blk * 4
                        off = [0]

                        def alloc(n):
                            v = R[:, off[0]:off[0] + n]
                            off[0] += n
                            return v
                        LG = alloc(80).rearrange("p (t c) -> p t c", c=20)
                        gmax = alloc(4); gsh = alloc(16).rearrange("p (t g) -> p t g", g=4); gsum = alloc(4); gval = alloc(4)
                        goh = alloc(16).rearrange("p (t g) -> p t g", g=4); pen = alloc(16).rearrange("p (t g) -> p t g", g=4)
                        elm = alloc(64).rearrange("p (t e) -> p t e", e=16); m1 = alloc(4)
                        elm2 = alloc(64).rearrange("p (t e) -> p t e", e=16); m2 = alloc(4); dm = alloc(4); ex = alloc(4)
                        den = alloc(4); e1 = alloc(4); e2 = alloc(4)
                        cntp = alloc(128).rearrange("p (t c) -> p t c", c=32)
                        rk = alloc(128).rearrange("p (t c) -> p t c", c=32)
                        V = lambda fn, w: fw.op('dve', fn, waits=w, inc=s_v)
                        A = lambda fn, w: fw.op('act', fn, waits=w, inc=s_a)
                        PR = ps_r[rb_i]
                        oh1 = OH[:, t0:t0 + 4, 0, :]; oh2 = OH[:, t0:t0 + 4, 1, :]
                        q = V(lambda e: e.tensor_tensor(out=LG, in0=PR[:, 0:80].rearrange("p (t c) -> p t c", c=20),
                                                        in1=rb[:].unsqueeze(1).to_broadcast([128, 4, 20]), op=ALU.add), [tr_last, W_W] + rt_free[rb_i])
                        psr_free[rb_i] = [q]
                        q = V(lambda e: e.reduce_max(out=gmax, in_=LG[:, :, 0:4], axis=AX.X), [q])
                        q = V(lambda e: e.tensor_tensor(out=gsh, in0=LG[:, :, 0:4], in1=gmax.unsqueeze(2).to_broadcast([128, 4, 4]), op=ALU.subtract), [q])
                        qa = A(lambda e: e.activation(out=gsh, in_=gsh, func=AF.Exp), [q])
                        q = V(lambda e: e.reduce_sum(out=gsum, in_=gsh, axis=AX.X), [qa])
                        q = V(lambda e: e.reciprocal(gval, gsum), [q])
                        q = V(lambda e: e.tensor_tensor(out=goh, in0=LG[:, :, 0:4], in1=gmax.unsqueeze(2).to_broadcast([128, 4, 4]), op=ALU.is_equal), [q])
                        q = V(lambda e: e.tensor_scalar(out=pen, in0=goh, scalar1=-1.0, scalar2=-NEGV, op0=ALU.add, op1=ALU.mult), [q])
                        q = V(lambda e: e.tensor_tensor(out=elm.rearrange("p t (g k) -> p t g k", k=4),
                                                        in0=LG[:, :, 4:20].rearrange("p t (g k) -> p t g k", k=4),
                                                        in1=pen.unsqueeze(3).to_broadcast([128, 4, 4, 4]), op=ALU.add), [q])
                        q = V(lambda e: e.reduce_max(out=m1, in_=elm, axis=AX.X), [q])
                        q = V(lambda e: e.tensor_tensor(out=oh1, in0=elm, in1=m1.unsqueeze(2).to_broadcast([128, 4, 16]), op=ALU.is_equal), [q, W_K])
                        q = V(lambda e: e.scalar_tensor_tensor(out=elm2, in0=oh1, scalar=NEGV, in1=elm, op0=ALU.mult, op1=ALU.add), [q])
                        q = V(lambda e: e.reduce_max(out=m2, in_=elm2, axis=AX.X), [q])
                        q = V(lambda e: e.tensor_tensor(out=oh2, in0=elm2, in1=m2.unsqueeze(2).to_broadcast([128, 4, 16]), op=ALU.is_equal), [q])
                        q = V(lambda e: e.tensor_tensor(out=dm, in0=m2, in1=m1, op=ALU.subtract), [q])
                        qa = A(lambda e: e.activation(out=ex, in_=dm, func=AF.Exp), [q])
                        q = V(lambda e: e.tensor_scalar(out=den, in0=ex, scalar1=1.0, scalar2=None, op0=ALU.add), [qa])
                        q = V(lambda e: e.reciprocal(e1, den), [q])
                        q = V(lambda e: e.tensor_tensor(out=e2, in0=ex, in1=e1, op=ALU.mult), [q])
                        q = V(lambda e: e.tensor_tensor(out=W12[:, t0:t0 + 4, 0], in0=e1, in1=gval, op=ALU.mult), [q])
                        q = V(lambda e: e.tensor_tensor(out=W12[:, t0:t0 + 4, 1], in0=e2, in1=gval, op=ALU.mult), [q])
                        ohf = OH[:, t0:t0 + 4, :, :].rearrange("p t k e -> p t (k e)")
                        ohb = OHb[:, t0:t0 + 4, :, :].rearrange("p t k e -> p t (k e)")
                        q_oh = V(lambda e: e.tensor_copy(ohb, ohf), [q])

                        def rank_part():
                            nonlocal last_cnt, psk_free
                            tk = None
                            for ti in range(4):
                                fw.op('pe', lambda e: e.matmul(ps_k[:, ti * 64:ti * 64 + 32], stri[:], OHb[:, t0 + ti, :, :].rearrange("p k e -> p (k e)"),
                                                               start=True, stop=True), waits=[q_oh] + W_K + psk_free)
                                tk = fw.op('pe', lambda e: e.matmul(ps_k[:, ti * 64 + 32:ti * 64 + 64], ones_b[:], OHb[:, t0 + ti, :, :].rearrange("p k e -> p (k e)"),
                                                                    start=True, stop=True), inc=s_p)
                            PK = ps_k[:, 0:256].rearrange("p (t c) -> p t c", c=64)
                            cntf = CNT[:]
                            qq = V(lambda e: e.tensor_copy(cntp[:, 0, 0:16], cntf), [tk, last_cnt])
                            for ti in range(4):
                                qq = V(lambda e: e.tensor_tensor(out=cntp[:, ti, 16:32], in0=cntp[:, ti, 0:16], in1=PK[:, ti, 32:48], op=ALU.add), [qq])
                                dst_ = cntp[:, ti + 1, 0:16] if ti < 3 else cntf
                                qq = V(lambda e: e.tensor_tensor(out=dst_, in0=cntp[:, ti, 16:32], in1=PK[:, ti, 48:64], op=ALU.add), [qq])
                            qc = qq
                            qq = V(lambda e: e.tensor_tensor(out=rk, in0=PK[:, :, 0:32], in1=cntp, op=ALU.add), [qc])
                            qq = V(lambda e: e.tensor_tensor(out=rk, in0=rk, in1=ohf, op=ALU.mult), [qq])
                            qq = V(lambda e: e.reduce_sum(out=RANK[:, t0:t0 + 4, :], in_=rk.rearrange("p t (k e) -> p t k e", e=16), axis=AX.X), [qq])
                            last_cnt = qq
                            psk_free = [qq]
                            rt_free[rb_i] = [qq]
                        return rank_part
                    rblocks[blk] = router_block
                    run_router_blocks(False)
                    at_free[ab] = [t_last_op]
                    ths = fw.dma('sp', HM[blk * 512:(blk + 1) * 512, :].rearrange("(t p) d -> p t d", p=128), hb_[ab][:], waits=t_hdone)
                    h_free[ab] = [ths]
                advance_pipe(True)
                run_router_blocks(True)
                assert not pipe and not stage_f2 and not pending_rank and not rblocks
                for e_ in ('pe', 'act', 'dve', 'pool', 'sp'):
                    fw._waits(e_, [last_cnt, (s_a, s_a.n), (s_p, s_p.n), (s_v, s_v.n), (s_g, s_g.n)] + fw.dma_all())
            with ExitStack() as pbx:
                sbb = lambda name, shape, dt: pbx.enter_context(nc.sbuf_tensor(f"{tag}B_" + name, shape, dt))
                MT = (2 * N) // SUP
                THR = sbb("THR", [128, MT], F32)
                THRJ = sbb("THRJ", [128, NSUP], F32)
                IOP = sbb("IOP", [128, 1], F32)
                LT = sbb("LT", [128, 16, 16], F32)
                big = sbb("big", [128, 16 * max(MT, NSUP)], F32)
                NTI = sbb("NTI", [128, 16], F32)
                PAD = sbb("PAD", [128, 16], F32)
                BASE = sbb("BASE", [128, 16], F32)
                END = sbb("END", [128, 16], F32)
                tmp2 = sbb("tmp2", [128, 16, 16], F32)
                SLT = sbb("SLT", [128, NTT, 2, 16], F32)
                SLF = sbb("SLF", [128, NTT, 2], F32)
                EBF = sbb("EBF", [128, NSUP], F32)
                FILL = sbb("FILL", [128, STOT // 128], I32)
                g1 = fw.group('sp')
                fw.dma('sp', LT[:].rearrange("p a b -> p (a b)"), I["c_lt"].partition_broadcast(128), inc=g1)
                fw.op('pool', lambda e: e.iota(THR[:], pattern=[[SUP, MT]], base=0, channel_multiplier=0, allow_small_or_imprecise_dtypes=True), inc=s_g)
                fw.op('pool', lambda e: e.iota(THRJ[:], pattern=[[SUP, NSUP]], base=0, channel_multiplier=0, allow_small_or_imprecise_dtypes=True), inc=s_g)
                fw.op('pool', lambda e: e.iota(IOP[:], pattern=[[0, 1]], base=0, channel_multiplier=1, allow_small_or_imprecise_dtypes=True), inc=s_g)
                t_fill = fw.op('pool', lambda e: e.iota(FILL[:], pattern=[[1, STOT // 128]], base=OOBV, channel_multiplier=0), inc=s_g)
                t_tf = fw.dma('pool', TOKIDX.rearrange("(p a) o -> p (a o)", p=128), FILL[:], waits=[t_fill])
                V = lambda fn, w: fw.op('dve', fn, waits=w, inc=s_v)
                q = V(lambda e: e.tensor_tensor(out=big[:, 0:16 * MT].rearrange("p (a m) -> p a m", m=MT),
                                                in0=CNT[:].unsqueeze(2).to_broadcast([128, 16, MT]),
                                                in1=THR[:].unsqueeze(1).to_broadcast([128, 16, MT]), op=ALU.is_gt), [(s_g, s_g.n), g1.token()])
                q = V(lambda e: e.reduce_sum(out=NTI[:], in_=big[:, 0:16 * MT].rearrange("p (a m) -> p a m", m=MT), axis=AX.X), [q])
                q = V(lambda e: e.tensor_scalar(out=PAD[:], in0=NTI[:], scalar1=float(SUP), scalar2=None, op0=ALU.mult), [q])
                q = V(lambda e: e.tensor_tensor(out=tmp2[:], in0=PAD[:].unsqueeze(1).to_broadcast([128, 16, 16]), in1=LT[:], op=ALU.mult), [q])
                q = V(lambda e: e.reduce_sum(out=BASE[:], in_=tmp2[:], axis=AX.X), [q])
                q = V(lambda e: e.tensor_tensor(out=END[:], in0=BASE[:], in1=PAD[:], op=ALU.add), [q])
                q = V(lambda e: e.tensor_tensor(out=SLT[:], in0=OH[:], in1=BASE[:].unsqueeze(1).unsqueeze(1).to_broadcast([128, NTT, 2, 16]), op=ALU.mult), [q])
                q = V(lambda e: e.reduce_sum(out=SLF[:], in_=SLT[:], axis=AX.X), [q])
                q = V(lambda e: e.tensor_tensor(out=SLF[:], in0=SLF[:], in1=RANK[:], op=ALU.add), [q])
                q = V(lambda e: e.tensor_copy(SLOTI[:], SLF[:]), [q])
                t_slot = q
                q = V(lambda e: e.tensor_tensor(out=big[:, 0:NSUP * 16].rearrange("p (j e) -> p j e", e=16),
                                                in0=END[:].unsqueeze(1).to_broadcast([128, NSUP, 16]),
                                                in1=THRJ[:].unsqueeze(2).to_broadcast([128, NSUP, 16]), op=ALU.is_le), [q])
                q = V(lambda e: e.reduce_sum(out=EBF[:], in_=big[:, 0:NSUP * 16].rearrange("p (j e) -> p j e", e=16), axis=AX.X), [q])
                q = V(lambda e: e.tensor_scalar(out=EBF[:], in0=EBF[:], scalar1=15.0, scalar2=128.0, op0=ALU.min, op1=ALU.mult), [q])
                q = V(lambda e: e.tensor_scalar(out=EBF[:], in0=EBF[:], scalar1=IOP[:, 0:1], scalar2=None, op0=ALU.add), [q])
                q = V(lambda e: e.tensor_copy(WIDX[:], EBF[:]), [q])
                t_widx = q
                tsc = []
                SSTOP = os.environ.get('SSTOP', '')
                for t in range(NTT if SSTOP != 'B' else 0):
                    for k in range(2):
                        fw._waits('pool', [t_slot, t_tf, t_tid])
                        sring = fw._ring_next('pool')
                        ins = nc.gpsimd.indirect_dma_start(out=TOKIDX[:, :], out_offset=bass.IndirectOffsetOnAxis(ap=SLOTI[:, t, k:k + 1], axis=0),
                                                           in_=TOKID[:, k, t:t + 1], in_offset=None)
                        sring.n += 16
                        ins.then_inc(sring.h, 16)
                for e_ in ('pe', 'act', 'dve', 'pool', 'sp'):
                    fw._waits(e_, [t_widx, (s_v, s_v.n), (s_g, s_g.n)] + fw.dma_all())
            if SSTOP in ('B', 'C'):
                return
            with ExitStack() as pd:
                sbd = lambda name, shape, dt: pd.enter_context(nc.sbuf_tensor(f"{tag}D_" + name, shape, dt))
                pmd = lambda name, shape, dt: pd.enter_context(nc.psum_tensor(f"{tag}DP_" + name, shape, dt))
                wall = [sbd(f"wall{i}", [128, 12288], BF16) for i in range(3)]
                NXG = 5
                xg = [sbd(f"xg{i}", [128, D], BF16) for i in range(NXG)]
                NID = 10
                idt = [sbd(f"idt{i}", [128, 1], I32) for i in range(NID)]
                xgT = [sbd(f"xgT{i}", [128, 8, 128], BF16) for i in range(2)]
                sg = [sbd(f"sg{i}", [128, 512], F32) for i in range(2)]
                hid = [sbd(f"hid{i}", [128, 512], BF16) for i in range(2)]
                hidT = [sbd(f"hidT{i}", [128, 4, 128], BF16) for i in range(2)]
                NOD = 4
                od = [sbd(f"od{i}", [128, D], F32) for i in range(NOD)]
                ptx = pmd("ptx", [128, 8, 128], BF16)
                pth = pmd("pth", [128, 8, 128], BF16)
                ps_g = [pmd(f"g{i}", [128, 512], F32) for i in range(1)]
                ps_u = [pmd(f"u{i}", [128, 512], F32) for i in range(1)]
                ps_d = [pmd(f"d{i}", [128, 512], F32) for i in range(2)]
                t_z = None
                for i in range(NXG):
                    t_z = fw.op('pool', lambda e: e.memset(xg[i][:], 0.0), inc=s_g)
                t_zr = fw.dma('pool', XN[DUM:DUM + 128, :], xg[0][:, :], waits=[t_z])
                t_z = [t_z, t_zr]
                wall_free = [[], [], []]; xg_free = [[] for _ in range(NXG)]; idt_free = [[] for _ in range(NID)]; xgT_free = [[], []]
                sg_free = [[], []]; hid_free = [[], []]; hidT_free = [[], []]; od_free = [[] for _ in range(NOD)]
                ptx_free = []; pth_free = []; g_free = []; u_free = []; d_free = [[], []]
                rec = {}
                sc_pend = {}
                wtok = {}
                last_sc = [[], []]

                def ind_dma(out, out_off, in_, in_off, bound, waits):
                    fw._waits('pool', waits)
                    sring = fw._ring_next('pool')
                    ins = nc.gpsimd.indirect_dma_start(out=out, out_offset=out_off, in_=in_, in_offset=in_off)
                    sring.n += 16
                    ins.then_inc(sring.h, 16)
                    return (sring, sring.n)

                idtok = {}

                def load_idt(j):
                    ib = j % NID
                    idtok[j] = fw.dma('sp', idt[ib][:], TOKIDX[j * 128:(j + 1) * 128, :], waits=idt_free[ib])

                def loads(j):
                    ib = j % NID; xb = j % NXG
                    J = j // G; wb = J % 3
                    t_i = idtok.pop(j)
                    if j % G == 0:
                        wtok[J] = ind_dma(wall[wb][:], None, WALL[layer][:, :], bass.IndirectOffsetOnAxis(ap=WIDX[:, J:J + 1], axis=0), NE * 128 - 1,
                                          [wc_tok] + wall_free[wb])
                    t_x = ind_dma(xg[xb][:], None, XN[:, :], bass.IndirectOffsetOnAxis(ap=idt[ib][:, :], axis=0), N - 1, [t_i, t_z] + xg_free[xb])
                    rec[j] = dict(t_x=t_x, t_w=wtok[J], t_i=t_i)

                def stage1(j):
                    xb = j % 2; xgb = j % NXG; wb = (j // G) % 3; r = rec[j]
                    tt = None
                    for c in range(8):
                        tt = fw.op('pe', lambda e: e.transpose(ptx[:, c, :], xg[xgb][:, c * 128:(c + 1) * 128], ident_bf[:]),
                                   waits=[r['t_x']] + W_CST + ptx_free, inc=(s_p if c == 7 else None))
                    xg_free[xgb] = [tt]
                    te = fw.op('dve', lambda e: e.tensor_copy(xgT[xb][:], ptx[:]), waits=[tt] + xgT_free[xb], inc=s_v)
                    ptx_free[:] = [te]
                    tg = tu = None
                    for c in range(8):
                        tg = fw.op('pe', lambda e: e.matmul(ps_g[0][:], xgT[xb][:, c, :], wall[wb][:, c * 512:(c + 1) * 512], start=(c == 0), stop=(c == 7)),
                                   waits=[te, r['t_w']] + g_free, inc=(s_p if c == 7 else None))
                        tu = fw.op('pe', lambda e: e.matmul(ps_u[0][:], xgT[xb][:, c, :], wall[wb][:, 4096 + c * 512:4096 + (c + 1) * 512],
                                                            start=(c == 0), stop=(c == 7)),
                                   waits=u_free, inc=(s_p if c == 7 else None))
                    xgT_free[xb] = [tu]
                    ts = fw.op('act', lambda e: e.activation(out=sg[xb][:], in_=ps_g[0][:], func=AF.Silu), waits=[tg] + sg_free[xb], inc=s_a)
                    g_free[:] = [ts]
                    th = fw.op('dve', lambda e: e.tensor_tensor(out=hid[xb][:], in0=sg[xb][:], in1=ps_u[0][:], op=ALU.mult),
                               waits=[ts, tu] + hid_free[xb], inc=s_v)
                    u_free[:] = [th]
                    sg_free[xb] = [th]
                    r['th'] = th

                def stage2(j):
                    xb = j % 2; wb = (j // G) % 3; ib = j % NID; ob = j % NOD; r = rec.pop(j)
                    tt = None
                    for fc in range(4):
                        tt = fw.op('pe', lambda e: e.transpose(pth[:, fc, :], hid[xb][:, fc * 128:(fc + 1) * 128], ident_bf[:]),
                                   waits=[r['th']] + pth_free, inc=(s_p if fc == 3 else None))
                    hid_free[xb] = [tt]
                    te = fw.op('dve', lambda e: e.tensor_copy(hidT[xb][:], pth[:, 0:4, :]), waits=[tt] + hidT_free[xb], inc=s_v)
                    pth_free[:] = [te]
                    tds = []
                    tm = None
                    for dh in range(2):
                        for fc in range(4):
                            tm = fw.op('pe', lambda e: e.matmul(ps_d[dh][:], hidT[xb][:, fc, :],
                                                                wall[wb][:, 8192 + fc * 1024 + dh * 512:8192 + fc * 1024 + (dh + 1) * 512],
                                                                start=(fc == 0), stop=(fc == 3)),
                                       waits=[te] + d_free[dh], inc=(s_p if fc == 3 else None))
                        tv = fw.op('dve', lambda e: e.tensor_copy(od[ob][:, dh * 512:(dh + 1) * 512], ps_d[dh][:]), waits=[tm] + od_free[ob], inc=s_v)
                        d_free[dh] = [tv]
                        tds.append(tv)
                    hidT_free[xb] = [tm]
                    if j % G == G - 1:
                        wall_free[wb] = [tm]
                    sc_pend[j] = (0, ib, ob, tds[-1])

                def scatter(j):
                    k, ib, ob, tdone = sc_pend.pop(j)
                    t_o = ind_dma(OUT2[:, :], bass.IndirectOffsetOnAxis(ap=idt[ib][:, :], axis=0), od[ob][:], None, N - 1, [tdone] + last_sc[k])
                    last_sc[k] = [t_o]
                    od_free[ob] = [t_o]
                    idt_free[ib] = [t_o]

                LA_I = 6; LA_X = 3
                for j in range(min(LA_I, NTL)):
                    load_idt(j)
                for j in range(min(LA_X, NTL)):
                    loads(j)
                for j in range(NTL + 3):
                    if j + LA_I < NTL:
                        load_idt(j + LA_I)
                    if j + LA_X < NTL:
                        loads(j + LA_X)
                    if j < NTL:
                        stage1(j)
                    if 1 <= j <= NTL:
                        stage2(j - 1)
                    if 2 <= j - 0 and (j - 2) in sc_pend:
                        scatter(j - 2)
                assert not sc_pend and not rec
                for e_ in ('pe', 'act', 'dve', 'pool', 'sp'):
                    fw._waits(e_, [(s_a, s_a.n), (s_p, s_p.n), (s_v, s_v.n), (s_g, s_g.n)] + fw.dma_all())
            with ExitStack() as pe_:
                sbe = lambda name, shape, dt: pe_.enter_context(nc.sbuf_tensor(f"{tag}E_" + name, shape, dt))
                hE = [sbe(f"h{i}", [128, D], F32) for i in range(2)]
                o1 = [sbe(f"o1{i}", [128, D], F32) for i in range(2)]
                o2 = [sbe(f"o2{i}", [128, D], F32) for i in range(2)]
                e_free = [[], []]
                for t in range(NTT):
                    b = t % 2
                    ge_ = fw.group('sp')
                    fw.dma('sp', hE[b][:], HM[t * 128:(t + 1) * 128, :], waits=e_free[b], inc=ge_)
                    fw.dma('sp', o1[b][:], OUT2[t * 128:(t + 1) * 128, :], waits=e_free[b], inc=ge_)
                    fw.dma('sp', o2[b][:], OUT2[N + t * 128:N + (t + 1) * 128, :], waits=e_free[b], inc=ge_)
                    q = fw.op('dve', lambda e: e.scalar_tensor_tensor(out=hE[b][:], in0=o1[b][:], scalar=W12[:, t, 0:1], in1=hE[b][:],
                                                                       op0=ALU.mult, op1=ALU.add), waits=[ge_.token()], inc=s_v)
                    q = fw.op('dve', lambda e: e.scalar_tensor_tensor(out=hE[b][:], in0=o2[b][:], scalar=W12[:, t, 1:2], in1=hE[b][:],
                                                                       op0=ALU.mult, op1=ALU.add), waits=[q], inc=s_v)
                    td = fw.dma('pool', out_rows_fn(t), hE[b][:], waits=[q])
                    e_free[b] = [td]
                for e_ in ('pe', 'act', 'dve', 'pool', 'sp'):
                    fw._waits(e_, [(s_v, s_v.n)] + fw.dma_all())

    WALL = [scratch(f"WALL{l}", [NE * 128, 12288], BF16) for l in range(2)]
    wc_state = dict(tok=None)

    wc_jobs = []

    def weight_convert_prepare():
        for l in range(2):
            for e_ in range(NE):
                wc_jobs.append((WALL[l][e_ * 128:(e_ + 1) * 128, 0:4096].rearrange("p (c f) -> p c f", c=8),
                                I["moe_w_gate"][l, e_].rearrange("(c p) f -> p c f", p=128)))
                wc_jobs.append((WALL[l][e_ * 128:(e_ + 1) * 128, 4096:8192].rearrange("p (c f) -> p c f", c=8),
                                I["moe_w_up"][l, e_].rearrange("(c p) f -> p c f", p=128)))
                wc_jobs.append((WALL[l][e_ * 128:(e_ + 1) * 128, 8192:12288].rearrange("p (c f) -> p c f", c=4),
                                I["moe_w_down"][l, e_].rearrange("(c p) f -> p c f", p=128)))

    def wc_issue(n):
        for _ in range(n):
            if not wc_jobs:
                break
            o_, i_ = wc_jobs.pop(0)
            fw.dma('pool', o_, i_)
        if not wc_jobs:
            wc_state['tok'] = [(s_, s_.n) for s_ in fw.rings['pool']]

    def weight_convert():
        weight_convert_prepare()
        wc_issue(10 ** 6)

    def moe0s():
        def rr(blk):
            slot = (blk * 512) // SPAN
            sp = cfg.QS[slot]
            off = (blk * 512) % SPAN
            return I["x"][sp * SPAN + off: sp * SPAN + off + 512, :]
        wc_issue(10 ** 6)
        moe_sparse(0, NQ, rr, AT0, "a_w_out", 8, lambda t: H1[t * 128:(t + 1) * 128, :], "s0", wc_state['tok'])

    def moe1s():
        def rr(blk):
            slot = (blk * 512) // SPAN
            qs = cfg.BS[slot][0]
            off = (blk * 512) % SPAN
            return H1[qs * SPAN + off: qs * SPAN + off + 512, :]
        moe_sparse(1, NB, rr, AT1, "b_w_out", 4, lambda t: Y[t * 128:(t + 1) * 128, :], "s1", wc_state['tok'])

    def moe0():
        return moe_phase(0, len(cfg.QS), lambda pz: I["x"][cfg.QS[pz] * SPAN:(cfg.QS[pz] + 1) * SPAN, :], AT0, "a_w_out", 8,
                         lambda pz: H1[pz * SPAN:(pz + 1) * SPAN, :], "m0")

    def moe1():
        return moe_phase(1, len(cfg.BS), lambda pz: H1[cfg.BS[pz][0] * SPAN:(cfg.BS[pz][0] + 1) * SPAN, :], AT1, "b_w_out", 4,
                         lambda pz: Y[pz * SPAN:(pz + 1) * SPAN, :], "m1")

    stages = {"wc": weight_convert, "wci": (lambda: None), "m0s": moe0s, "m1s": moe1s, "p1": phase1, "p2": phase2, "m0": moe0, "m1": moe1, "p5": phase5, "p5b": phase5b, "p6": phase6}
    last = None
    for sname in cfg.stages:
        last = stages[sname]()
    es.close()
    return nc


_SQUEEZE = ("a_norm", "a_w_in", "a_b_f", "a_q_gain", "a_k_gain", "a_w_out", "b_norm", "b_w_q", "b_q_gain", "b_w_out")
_NC_CACHE = {}


def _get_nc(key, cfg):
    if key not in _NC_CACHE:
        _NC_CACHE[key] = build(cfg)
    return _NC_CACHE[key]


def run_cfg(cfg, inputs, n_batch):
    nc = build(cfg)
    used = nc._used_inputs
    shared = {}
    for k, v in inputs.items():
        if k == "x":
            continue
        a = np.ascontiguousarray(np.asarray(v, dtype=np.float32))
        if k in _SQUEEZE:
            a = a.reshape(a.shape[1:])
        if k in used:
            shared[k] = a
    for k, v in make_consts().items():
        if k in used:
            shared[k] = v
    x = np.asarray(inputs["x"], dtype=np.float32)
    in_maps = []
    for b in range(n_batch):
        m = dict(shared)
        m["x"] = np.ascontiguousarray(x[b])
        in_maps.append(m)
    res = run_bass_kernel_spmd(nc, in_maps, core_ids=list(range(n_batch)))
    return res


def kernel(**inputs):
    x = np.asarray(inputs["x"], dtype=np.float32)
    B, S, _ = x.shape
    KS = S // SPAN
    half = KS // 2
    QS = tuple(range(half - 1, KS))
    BS = tuple((i, i - 1) for i in range(1, half + 1))
    cfg = Cfg(KS=KS, QS=QS, BS=BS, stages=("wci", "p1", "p2", "m0s", "p5", "p5b", "p6", "m1s"), masks=True, n_cores=2 * B)
    nc = build(cfg)
    used = nc._used_inputs
    shared = {}
    for k, v in inputs.items():
        if k == "x":
            continue
        a_ = np.ascontiguousarray(np.asarray(v, dtype=np.float32))
        if k in _SQUEEZE:
            a_ = a_.reshape(a_.shape[1:])
        if k in used:
            shared[k] = a_
    for k, v in make_consts().items():
        if k in used:
            shared[k] = v
    in_maps = []
    for b in range(B):
        for r in range(2):
            m = dict(shared)
            big = np.zeros(S, np.float32)
            kb = np.zeros(len(QS), np.float32)
            if r == 1:
                xs = x[b]
            else:
                xs = np.concatenate([x[b, 0:SPAN]] * half + [x[b, 0:half * SPAN]], axis=0)
                big[0:half * SPAN] = -NEGV
                kb[0] = NEGV
            m["x"] = np.ascontiguousarray(xs)
            m["c_big"] = big
            m["c_kbias"] = kb
            in_maps.append(m)
    res = run_bass_kernel_spmd(nc, in_maps, core_ids=list(range(2 * B)))
    out = np.empty((B, S, D), np.float32)
    for b in range(B):
        out[b, 0:half * SPAN] = np.asarray(res.results[2 * b]["y"])
        out[b, half * SPAN:] = np.asarray(res.results[2 * b + 1]["y"])
    return out
```
